# Optimizing a Trainium2 kernel written in Bass

```python
import jax, jax.numpy as jnp
from jax import lax
import numpy as np

D_MODEL = 2048
BATCH = 4
SEQ = 2048
DEPTH = 4

GRID_W = 64
CTX_LEN = 256
NA_HEADS = 8
NA_HEAD_DIM = 64
NA_WIN_ROWS = 8
NA_WIN_COLS = 16
MLA_HEADS = 8
MLA_Q_RANK = 512
MLA_KV_RANK = 256
MLA_NOPE_DIM = 64
MLA_ROPE_DIM = 32
MLA_V_DIM = 64
GQA_Q_HEADS = 8
GQA_KV_HEADS = 2
GQA_HEAD_DIM = 64
N_EXPERTS = 16
EXPERT_FF = 1024
CAPACITY_FACTOR = 2
N_BRANCHES = 3
Q_BLOCK = 128
ROPE_THETA = 10000.0
EPS = 1e-6

NA_W = NA_HEADS * NA_HEAD_DIM
MLA_QK_DIM = MLA_NOPE_DIM + MLA_ROPE_DIM
MLA_OUT = MLA_HEADS * MLA_V_DIM
GQA_Q_W = GQA_Q_HEADS * GQA_HEAD_DIM
GQA_KV_W = GQA_KV_HEADS * GQA_HEAD_DIM
IN_SIZES = [NA_W, NA_W, NA_W,
            MLA_Q_RANK, MLA_KV_RANK, MLA_ROPE_DIM,
            GQA_Q_W, GQA_KV_W, GQA_KV_W,
            N_BRANCHES * D_MODEL]
IN_WIDTH = int(sum(IN_SIZES))
IN_SPLITS = [int(v) for v in np.cumsum(IN_SIZES)[:-1]]

kernel_name = 'hybrid_na_mla_gqa_ecmoe_dit'


def rms_norm(x, g):
    xf = x.astype(jnp.float32)
    y = xf * lax.rsqrt(jnp.mean(xf * xf, axis=-1, keepdims=True) + EPS)
    return (y * g.astype(jnp.float32)).astype(x.dtype)


def heads(t, n):
    return t.reshape(t.shape[0], t.shape[1], n, -1)


def axial_angles(n_tok, rot_dim):
    t = jnp.arange(n_tok)
    row = (t // GRID_W).astype(jnp.float32)
    col = (t % GRID_W).astype(jnp.float32)
    n_freq = rot_dim // 4
    inv = ROPE_THETA ** (-jnp.arange(n_freq, dtype=jnp.float32) / n_freq)
    return row[:, None] * inv, col[:, None] * inv


def rotate_section(x, ang):
    m = ang.shape[-1]
    x1, x2 = x[..., :m], x[..., m:]
    cos = jnp.cos(ang)[:, None, :].astype(x.dtype)
    sin = jnp.sin(ang)[:, None, :].astype(x.dtype)
    return jnp.concatenate([x1 * cos - x2 * sin, x1 * sin + x2 * cos], axis=-1)


def axial_rope(x, ang_r, ang_c):
    h = x.shape[-1] // 2
    return jnp.concatenate([rotate_section(x[..., :h], ang_r),
                            rotate_section(x[..., h:], ang_c)], axis=-1)


def rope_tail(t, n_rot, ang_r, ang_c):
    return jnp.concatenate([t[..., :-n_rot], axial_rope(t[..., -n_rot:], ang_r, ang_c)], axis=-1)


def blocked_attention(q, k, v, scale):
    B, S, Hq, d = q.shape
    Hk = k.shape[2]
    G = Hq // Hk
    dv = v.shape[-1]
    nb = S // Q_BLOCK
    qb = q.reshape(B, nb, Q_BLOCK, Hk, G, d).transpose(1, 0, 2, 3, 4, 5)

    def one_block(q_blk):
        s = jnp.einsum('bqhgd,bkhd->bhgqk', q_blk, k).astype(jnp.float32) * scale
        p = jax.nn.softmax(s, axis=-1).astype(v.dtype)
        return jnp.einsum('bhgqk,bkhe->bqhge', p, v)

    o = lax.map(one_block, qb)
    return o.transpose(1, 0, 2, 3, 4, 5).reshape(B, S, Hq * dv)


def neighbourhood_attention(q, k, v, k_ctx, v_ctx, rel_bias, rows):
    B, S, H, d = q.shape
    wr = min(NA_WIN_ROWS, rows)
    wc = NA_WIN_COLS
    n_win = wr * wc
    scale = NA_HEAD_DIM ** -0.5
    qg = q.reshape(B, rows, GRID_W, H, d)
    kg = k.reshape(B, rows, GRID_W, H, d)
    vg = v.reshape(B, rows, GRID_W, H, d)
    cols = np.arange(GRID_W)
    col_start = np.clip(cols - wc // 2, 0, GRID_W - wc)
    col_idx = col_start[:, None] + np.arange(wc)[None, :]
    dc = col_idx - cols[:, None] + (NA_WIN_COLS - 1)

    def one_row(r):
        rs = jnp.clip(r - wr // 2, 0, rows - wr)
        kb = lax.dynamic_slice_in_dim(kg, rs, wr, axis=1)
        vb = lax.dynamic_slice_in_dim(vg, rs, wr, axis=1)
        kw = kb[:, :, col_idx]
        vw = vb[:, :, col_idx]
        q_r = lax.dynamic_index_in_dim(qg, r, axis=1, keepdims=False)
        dr = rs + jnp.arange(wr) - r + (NA_WIN_ROWS - 1)
        bias = rel_bias[:, dr[:, None, None], dc[None]]
        bias = bias.transpose(0, 2, 1, 3).astype(jnp.float32)
        s_win = jnp.einsum('bqhd,brqchd->bhqrc', q_r, kw).astype(jnp.float32) * scale + bias
        s_ctx = jnp.einsum('bqhd,bkhd->bhqk', q_r, k_ctx).astype(jnp.float32) * scale
        s = jnp.concatenate([s_win.reshape(B, H, GRID_W, n_win), s_ctx], axis=-1)
        p = jax.nn.softmax(s, axis=-1).astype(v.dtype)
        p_win = p[..., :n_win].reshape(B, H, GRID_W, wr, wc)
        p_ctx = p[..., n_win:]
        return (jnp.einsum('bhqrc,brqchd->bqhd', p_win, vw)
                + jnp.einsum('bhqk,bkhd->bqhd', p_ctx, v_ctx))

    o = lax.map(one_row, jnp.arange(rows))
    return o.transpose(1, 0, 2, 3, 4).reshape(B, S, H * d)


def token_mixer(h_lat, h_ctx, ang64, ang32, w_in, na_rel_bias, na_q_norm, na_k_norm,
                mla_q_a_norm, mla_w_q_b, mla_kv_a_norm, mla_w_kv_b, mla_q_norm, mla_k_norm,
                gqa_q_norm, gqa_k_norm, w_branch_a, w_branch_b, w_branch_c, w_out, with_ctx_out):
    B, S, _ = h_lat.shape
    L = h_ctx.shape[1]
    T = L + S
    rows = S // GRID_W
    proj = jnp.concatenate([h_ctx, h_lat], axis=1) @ w_in
    qa, ka, va, cq, ckv, kpe, qc, kc, vc, gates = jnp.split(proj, IN_SPLITS, axis=-1)

    qa = rms_norm(heads(qa, NA_HEADS), na_q_norm)
    ka = rms_norm(heads(ka, NA_HEADS), na_k_norm)
    va = heads(va, NA_HEADS)
    o_a_lat = neighbourhood_attention(qa[:, L:], ka[:, L:], va[:, L:], ka[:, :L], va[:, :L],
                                      na_rel_bias, rows)

    q_b = heads(rms_norm(cq, mla_q_a_norm) @ mla_w_q_b, MLA_HEADS)
    kv_b = heads(rms_norm(ckv, mla_kv_a_norm) @ mla_w_kv_b, MLA_HEADS)
    k_nope, v_b = kv_b[..., :MLA_NOPE_DIM], kv_b[..., MLA_NOPE_DIM:]
    k_pe = jnp.broadcast_to(kpe[:, :, None, :], (B, T, MLA_HEADS, MLA_ROPE_DIM))
    q_b = rms_norm(q_b, mla_q_norm)
    k_b = rms_norm(jnp.concatenate([k_nope, k_pe], axis=-1), mla_k_norm)
    q_b_lat = rope_tail(q_b[:, L:], MLA_ROPE_DIM, ang32[0], ang32[1])
    k_b_all = jnp.concatenate([k_b[:, :L], rope_tail(k_b[:, L:], MLA_ROPE_DIM, ang32[0], ang32[1])], axis=1)
    o_b_lat = blocked_attention(q_b_lat, k_b_all, v_b, MLA_QK_DIM ** -0.5)

    q_c = rms_norm(heads(qc, GQA_Q_HEADS), gqa_q_norm)
    k_c = rms_norm(heads(kc, GQA_KV_HEADS), gqa_k_norm)
    v_c = heads(vc, GQA_KV_HEADS)
    q_c_lat = axial_rope(q_c[:, L:], ang64[0], ang64[1])
    k_c_all = jnp.concatenate([k_c[:, :L], axial_rope(k_c[:, L:], ang64[0], ang64[1])], axis=1)
    o_c_lat = blocked_attention(q_c_lat, k_c_all, v_c, GQA_HEAD_DIM ** -0.5)

    def merge(o_a, o_b, o_c, g):
        g_a, g_b, g_c = jnp.split(jax.nn.sigmoid(g), N_BRANCHES, axis=-1)
        y = g_a * (o_a @ w_branch_a) + g_b * (o_b @ w_branch_b) + g_c * (o_c @ w_branch_c)
        return y @ w_out

    out_lat = merge(o_a_lat, o_b_lat, o_c_lat, gates[:, L:])
    if not with_ctx_out:
        return out_lat, None
    o_a_ctx = blocked_attention(qa[:, :L], ka[:, :L], va[:, :L], NA_HEAD_DIM ** -0.5)
    o_b_ctx = blocked_attention(q_b[:, :L], k_b[:, :L], v_b[:, :L], MLA_QK_DIM ** -0.5)
    o_c_ctx = blocked_attention(q_c[:, :L], k_c[:, :L], v_c[:, :L], GQA_HEAD_DIM ** -0.5)
    out_ctx = merge(o_a_ctx, o_b_ctx, o_c_ctx, gates[:, :L])
    return out_lat, out_ctx


def expert_choice_ffn(h, w_router, w_gate, w_up, w_down):
    B, N, _ = h.shape
    cap = max(1, CAPACITY_FACTOR * N // N_EXPERTS)
    aff = jax.nn.softmax((h @ w_router).astype(jnp.float32), axis=-1)
    top_val, top_idx = lax.top_k(aff.transpose(0, 2, 1), cap)
    b_idx = jnp.arange(B)[:, None, None]
    xe = h[b_idx, top_idx]
    hid = jax.nn.silu(jnp.einsum('becd,edf->becf', xe, w_gate)) * jnp.einsum('becd,edf->becf', xe, w_up)
    ye = jnp.einsum('becf,efd->becd', hid, w_down) * top_val[..., None].astype(h.dtype)
    return jnp.zeros_like(h).at[b_idx, top_idx].add(ye)


def setup_inputs(seed: int = 0) -> dict:
    key = jax.random.key(seed)
    ks = iter(jax.random.split(key, 40))
    D = D_MODEL
    NL = DEPTH

    def nrm(shape, scale):
        return jax.random.normal(next(ks), shape, jnp.float32) * scale

    def gain(shape):
        return 1.0 + 0.05 * jax.random.normal(next(ks), shape, jnp.float32)

    return dict(
        x=nrm((BATCH, SEQ, D), 1.0),
        c=nrm((BATCH, D), 1.0),
        ctx=nrm((BATCH, CTX_LEN, D), 1.0),
        c_ctx=nrm((D,), 1.0),
        w_mod=nrm((NL, D, 6 * D), 0.5 * D ** -0.5),
        b_mod=nrm((NL, 6 * D), 0.02),
        norm1=gain((NL, D)),
        w_in=nrm((NL, D, IN_WIDTH), D ** -0.5),
        na_rel_bias=nrm((NL, NA_HEADS, 2 * NA_WIN_ROWS - 1, 2 * NA_WIN_COLS - 1), 0.5),
        na_q_norm=gain((NL, NA_HEAD_DIM)),
        na_k_norm=gain((NL, NA_HEAD_DIM)),
        mla_q_a_norm=gain((NL, MLA_Q_RANK)),
        mla_w_q_b=nrm((NL, MLA_Q_RANK, MLA_HEADS * MLA_QK_DIM), MLA_Q_RANK ** -0.5),
        mla_kv_a_norm=gain((NL, MLA_KV_RANK)),
        mla_w_kv_b=nrm((NL, MLA_KV_RANK, MLA_HEADS * (MLA_NOPE_DIM + MLA_V_DIM)), MLA_KV_RANK ** -0.5),
        mla_q_norm=gain((NL, MLA_QK_DIM)),
        mla_k_norm=gain((NL, MLA_QK_DIM)),
        gqa_q_norm=gain((NL, GQA_HEAD_DIM)),
        gqa_k_norm=gain((NL, GQA_HEAD_DIM)),
        w_branch_a=nrm((NL, NA_W, D), NA_W ** -0.5),
        w_branch_b=nrm((NL, MLA_OUT, D), MLA_OUT ** -0.5),
        w_branch_c=nrm((NL, GQA_Q_W, D), GQA_Q_W ** -0.5),
        w_out=nrm((NL, D, D), D ** -0.5),
        norm2=gain((NL, D)),
        w_router=nrm((NL, D, N_EXPERTS), D ** -0.5),
        w_expert_gate=nrm((NL, N_EXPERTS, D, EXPERT_FF), D ** -0.5),
        w_expert_up=nrm((NL, N_EXPERTS, D, EXPERT_FF), D ** -0.5),
        w_expert_down=nrm((NL, N_EXPERTS, EXPERT_FF, D), EXPERT_FF ** -0.5),
    )


def reference(x, c, ctx, c_ctx, w_mod, b_mod, norm1, w_in, na_rel_bias, na_q_norm, na_k_norm,
              mla_q_a_norm, mla_w_q_b, mla_kv_a_norm, mla_w_kv_b, mla_q_norm, mla_k_norm,
              gqa_q_norm, gqa_k_norm, w_branch_a, w_branch_b, w_branch_c, w_out, norm2,
              w_router, w_expert_gate, w_expert_up, w_expert_down):
    S = x.shape[1]
    ang64 = axial_angles(S, GQA_HEAD_DIM)
    ang32 = axial_angles(S, MLA_ROPE_DIM)
    for i in range(DEPTH):
        last = i == DEPTH - 1
        mod_lat = jnp.split((jax.nn.silu(c) @ w_mod[i] + b_mod[i])[:, None, :], 6, axis=-1)
        mod_ctx = jnp.split(jax.nn.silu(c_ctx) @ w_mod[i] + b_mod[i], 6, axis=-1)
        sh1, sc1, g1, sh2, sc2, g2 = mod_lat
        csh1, csc1, cg1, csh2, csc2, cg2 = mod_ctx
        h_lat = rms_norm(x, norm1[i]) * (1.0 + sc1) + sh1
        h_ctx = rms_norm(ctx, norm1[i]) * (1.0 + csc1) + csh1
        o_lat, o_ctx = token_mixer(h_lat, h_ctx, ang64, ang32, w_in[i], na_rel_bias[i],
                                   na_q_norm[i], na_k_norm[i], mla_q_a_norm[i], mla_w_q_b[i],
                                   mla_kv_a_norm[i], mla_w_kv_b[i], mla_q_norm[i], mla_k_norm[i],
                                   gqa_q_norm[i], gqa_k_norm[i], w_branch_a[i], w_branch_b[i],
                                   w_branch_c[i], w_out[i], not last)
        x = x + g1 * o_lat
        h2 = rms_norm(x, norm2[i]) * (1.0 + sc2) + sh2
        x = x + g2 * expert_choice_ffn(h2, w_router[i], w_expert_gate[i], w_expert_up[i], w_expert_down[i])
        if not last:
            ctx = ctx + cg1 * o_ctx
            h2c = rms_norm(ctx, norm2[i]) * (1.0 + csc2) + csh2
            ctx = ctx + cg2 * expert_choice_ffn(h2c, w_router[i], w_expert_gate[i], w_expert_up[i], w_expert_down[i])
    return x
```

```python
import numpy as np
from contextlib import ExitStack
import concourse.bass as bass
import concourse.mybir as mybir
from concourse.bass_utils import run_bass_kernel_spmd

F32 = mybir.dt.float32
BF16 = mybir.dt.bfloat16
AF = mybir.ActivationFunctionType
ALU = mybir.AluOpType
AX = mybir.AxisListType

D = 2048
KD = 16
L = 256
S = 2048
T = L + S
NT = T // 128
DEPTH = 4
GRID_W = 64
INW = 9248
E = 16
FF = 1024
CAP_LAT = 256
CAP_CTX = 32
EPS = 1e-6
NCORES = 4
NEG = -30000.0


class Trk:
    __slots__ = ("w", "r")

    def __init__(self):
        self.w = {}
        self.r = {}


ENGS = ("pe", "act", "dve", "pool", "sp")
NRING = 12
EPOCH = 30000


class Sch:
    def __init__(self, nc, es):
        self.nc = nc
        self.es = es
        self.sems = []
        self.owner = []
        self.stream = {e: [] for e in ENGS}
        self.cnt = {e: 0 for e in ENGS}
        self.csem = {e: None for e in ENGS}
        self.waited = {e: {} for e in ENGS}
        self.ring = {}
        self.dman = {}
        self.dtok = {}
        for q in ("sp", "pool", "act"):
            self.ring[q] = [self._newsem("d%s%d" % (q, i), "dma") for i in range(NRING)]
            self.dman[q] = 0
            self.dtok[q] = [None] * NRING

    def _newsem(self, name, owner):
        h = self.es.enter_context(self.nc.semaphore(name))
        self.sems.append(h)
        self.owner.append(owner)
        return len(self.sems) - 1

    def _need(self, e, si, v, waits):
        if e == "pe" and self.owner[si] == "pe":
            return
        if self.waited[e].get(si, 0) >= v:
            return
        if waits.get(si, 0) < v:
            waits[si] = v

    def _deps(self, e, r, w):
        waits = {}
        for t in r:
            for si, v in t.w.items():
                self._need(e, si, v, waits)
        for t in w:
            for si, v in t.w.items():
                self._need(e, si, v, waits)
            for si, v in t.r.items():
                self._need(e, si, v, waits)
        return waits

    def _commit(self, e, waits, tok, r, w):
        for si, v in waits.items():
            self.waited[e][si] = v
        si, v = tok
        for t in r:
            if t.r.get(si, 0) < v:
                t.r[si] = v
        for t in w:
            t.w = {si: v}
            t.r = {}

    def op(self, e, fn, r=(), w=()):
        waits = self._deps(e, r, w)
        if self.csem[e] is None or self.cnt[e] >= EPOCH:
            self.csem[e] = self._newsem("c%s%d" % (e, len(self.sems)), e)
            self.cnt[e] = 0
        self.cnt[e] += 1
        tok = (self.csem[e], self.cnt[e])
        self.stream[e].append((list(waits.items()), fn, tok, 1))
        self._commit(e, waits, tok, r, w)
        return tok

    def dma(self, q, fn, r=(), w=()):
        waits = self._deps(q, r, w)
        n = self.dman[q]
        slot = n % NRING
        prev = self.dtok[q][slot]
        if prev is not None:
            self._need(q, prev[0], prev[1], waits)
        tok = (self.ring[q][slot], 16 * (n // NRING + 1))
        self.dman[q] = n + 1
        self.dtok[q][slot] = tok
        self.stream[q].append((list(waits.items()), fn, tok, 16))
        self._commit(q, waits, tok, r, w)
        return tok

    def wait_all(self, e, trks):
        waits = {}
        for t in trks:
            for si, v in t.w.items():
                self._need(e, si, v, waits)
        for si, v in waits.items():
            self.waited[e][si] = v
        self.stream[e].append((list(waits.items()), None, None, 0))

    def replay(self, e, eng):
        for waits, fn, tok, inc in self.stream[e]:
            for si, v in waits:
                eng.wait_ge(self.sems[si], v)
            if fn is not None:
                fn(eng).then_inc(self.sems[tok[0]], inc)


def _rope_tables(rot_dim):
    t = np.arange(S)
    row = (t // GRID_W).astype(np.float32)
    col = (t % GRID_W).astype(np.float32)
    nf = rot_dim // 4
    inv = (10000.0 ** (-np.arange(nf, dtype=np.float32) / nf)).astype(np.float32)
    ar = row[:, None] * inv
    ac = col[:, None] * inv
    C = np.concatenate([np.cos(ar), np.cos(ar), np.cos(ac), np.cos(ac)], axis=1)
    Sg = np.concatenate([-np.sin(ar), np.sin(ar), -np.sin(ac), np.sin(ac)], axis=1)
    return C.astype(np.float32), Sg.astype(np.float32)


def _na_plan():
    plan = []
    for qb in range(4):
        rows = range(8 * qb, 8 * qb + 8)
        rs = [min(max(r - 4, 0), 24) for r in rows]
        lo, hi = min(rs), max(rs) + 7
        plan.append(list(range(lo // 2, hi // 2 + 1)))
    return plan


NA_PLAN = _na_plan()
NA_NTILES = sum(len(p) for p in NA_PLAN)


_NA_IDX = None


def _na_idx():
    global _NA_IDX
    if _NA_IDX is None:
        kk = np.arange(128)[:, None]
        qq = np.arange(512)[None, :]
        idx = np.full((NA_NTILES, 128, 512), -1, np.int64)
        ti = 0
        for qb in range(4):
            for kc in NA_PLAN[qb]:
                kt = kc * 128 + kk
                kr, kcol = kt // 64, kt % 64
                qt = qb * 512 + qq
                r, c = qt // 64, qt % 64
                rs = np.clip(r - 4, 0, 24)
                cs = np.clip(c - 8, 0, 48)
                ok = (kr >= rs) & (kr < rs + 8) & (kcol >= cs) & (kcol < cs + 16)
                v = (kr - r + 7) * 31 + (kcol - c + 15)
                idx[ti] = np.where(ok, v, -1)
                ti += 1
        _NA_IDX = idx
    return _NA_IDX


NA_TILE_ID = []
_uid = 0
for _qb in range(4):
    if _qb == 2:
        NA_TILE_ID.append(list(NA_TILE_ID[1]))
        continue
    NA_TILE_ID.append(list(range(_uid, _uid + len(NA_PLAN[_qb]))))
    _uid += len(NA_PLAN[_qb])
NA_NUNIQ = _uid


def _na_uniq_idx():
    idx = _na_idx()
    out = np.zeros((NA_NUNIQ, 128, 512), np.int64)
    ti = 0
    for qb in range(4):
        for j in range(len(NA_PLAN[qb])):
            out[NA_TILE_ID[qb][j]] = idx[ti]
            ti += 1
    return out


class Arena:
    def __init__(self, t, n):
        self.t = t
        self.n = n
        self.off = 0

    def reset(self):
        self.off = 0

    def get(self, n, shape=None):
        n_al = (n + 15) // 16 * 16
        assert self.off + n_al <= self.n, ("arena overflow", self.off, n_al, self.n)
        v = self.t[:, self.off:self.off + n]
        self.off += n_al
        if shape is not None and len(shape) == 2:
            v = v.rearrange("p (a b) -> p a b", a=shape[0])
        elif shape is not None and len(shape) == 3:
            v = v.rearrange("p (a b c) -> p a b c", a=shape[0], b=shape[1])
        return v


def build(nl=DEPTH, dbg=None, nlw=DEPTH, ne=E):
    dbg = dbg or {}
    nc = bass.Bass("TRN2", target_bir_lowering=False)

    def din(name, shape, dt=F32):
        return nc.dram_tensor(name, list(shape), dt, kind="ExternalInput").ap()

    def dscr(name, shape, dt):
        kind = "ExternalOutput" if dbg.get(name) else "Internal"
        return nc.dram_tensor(name, list(shape), dt, kind=kind).ap()

    x_in = din("x", [S, D])
    ctx_in = din("ctx", [L, D])
    cT_in = din("cT", [128, KD * 2])
    w_mod = din("w_mod", [nlw, D, 6 * D])
    b_mod = din("b_mod", [nlw, 6 * D])
    b_modT = din("b_modT", [nlw, 128, 96])
    norm1T = din("norm1T", [nlw, 128, KD])
    norm2T = din("norm2T", [nlw, 128, KD])
    w_in = din("w_in", [nlw, D, INW])
    na_bias = din("na_bias", [nlw, 8, NA_NUNIQ, 128, 512])
    gains = {}
    for nm, n in (("na_q_norm", 64), ("na_k_norm", 64), ("mla_q_a_norm", 512), ("mla_kv_a_norm", 256),
                  ("mla_q_norm", 96), ("mla_k_norm", 96), ("gqa_q_norm", 64), ("gqa_k_norm", 64)):
        gains[nm] = din(nm, [nlw, n])
    w_q_b = din("mla_w_q_b", [nlw, 512, 768])
    w_kv_b = din("mla_w_kv_b", [nlw, 256, 1024])
    w_br = [din("w_branch_a", [nlw, 512, D]), din("w_branch_b", [nlw, 512, D]), din("w_branch_c", [nlw, 512, D])]
    w_out = din("w_out", [nlw, D, D])
    w_router = din("w_router", [nlw, D, E])
    w_eg = din("w_expert_gate", [nlw, ne, D, FF])
    w_eu = din("w_expert_up", [nlw, ne, D, FF])
    w_ed = din("w_expert_down", [nlw, ne, FF, D])
    rc64 = din("ropeC64", [S, 64])
    rs64 = din("ropeS64", [S, 64])
    rc32 = din("ropeC32", [S, 32])
    rs32 = din("ropeS32", [S, 32])
    ident_in = din("ident", [128, 128])
    iota_in = din("iota", [128, 256])
    sel64_in = din("sel64", [128, 128])
    out = nc.dram_tensor("out", [S, D], F32, kind="ExternalOutput").ap()

    X = dscr("X", [T, D], F32)
    QaT = dscr("QaT", [512, T], BF16)
    KaT = dscr("KaT", [512, T], BF16)
    QcT = dscr("QcT", [512, T], BF16)
    KcT = dscr("KcT", [128, T], BF16)
    QbT = dscr("QbT", [8, 96, T], BF16)
    KbT = dscr("KbT", [8, 96, T], BF16)
    Va = dscr("Va", [T, 512], BF16)
    Vb = dscr("Vb", [T, 512], BF16)
    Vc = dscr("Vc", [T, 128], BF16)
    GT = dscr("GT", [3 * D, T], BF16)
    OT = dscr("OT", [3, 512, T], BF16)
    YT = dscr("YT", [D, T], BF16)
    GREP = dscr("GREP", [2, 2, 128, D], F32)
    HID = dscr("HID", [E, 128, 8, 288], BF16)
    SWTL = dscr("SWTL", [16, 128, E, 2, 128], BF16)
    SWTC = dscr("SWTC", [E, 32, L], BF16)

    es = ExitStack()
    abf_t = es.enter_context(nc.sbuf_tensor("abf", [128, 68 * 1024], BF16))
    af_t = es.enter_context(nc.sbuf_tensor("af", [128, 11 * 1024], F32))
    cst_t = es.enter_context(nc.sbuf_tensor("cst", [128, 4 * 1024], F32))
    pps = [es.enter_context(nc.psum_tensor("pp%d" % i, [128, 1024], F32)) for i in range(4)]
    sch = Sch(nc, es)
    ABF = Arena(abf_t, 68 * 1024)
    AFF = Arena(af_t, 11 * 1024)
    CST = Arena(cst_t, 4 * 1024)

    def bank(b):
        return pps[b // 2][:, (b % 2) * 512:(b % 2 + 1) * 512]

    def bank_bf(b):
        return bank(b).bitcast(BF16)

    ptrk = [Trk() for _ in range(8)]

    def barrier():
        toks = []
        for f in ENGS:
            if sch.csem[f] is not None:
                toks.append((sch.csem[f], sch.cnt[f]))
        for q in ("sp", "pool", "act"):
            for tk in sch.dtok[q]:
                if tk is not None:
                    toks.append(tk)
        for e in ENGS:
            waits = {}
            for si, v in toks:
                sch._need(e, si, v, waits)
            for si, v in waits.items():
                sch.waited[e][si] = v
            if waits:
                sch.stream[e].append((list(waits.items()), None, None, 0))

    def new_phase():
        barrier()
        ABF.reset()
        AFF.reset()

    ident_f = CST.get(128)
    iota_f = CST.get(256)
    sel64_f = CST.get(128)
    ident_bf_t = es.enter_context(nc.sbuf_tensor("identbf", [128, 128], BF16))
    ident_bf = ident_bf_t[:]
    rC64 = CST.get(16 * 64, (16, 64))
    rS64 = CST.get(16 * 64, (16, 64))
    rC32 = CST.get(16 * 32, (16, 32))
    rS32 = CST.get(16 * 32, (16, 32))
    scT_f = CST.get(32)
    modc = CST.get(4 * 32, (4, 32))
    modT = CST.get(192, (96, 2))
    bmT = CST.get(96)
    n1T = CST.get(16)
    n2T = CST.get(16)
    cT_sb = CST.get(32)
    scT_bf_t = es.enter_context(nc.sbuf_tensor("scTbf", [128, 32], BF16))
    scRep_t = es.enter_context(nc.sbuf_tensor("scRep", [128, 32 * 128], BF16))
    scT_bf = scT_bf_t[:]
    scRep = scRep_t[:].rearrange("p (a b) -> p a b", a=32)
    k_cst = Trk()

    def ld(q, out_ap, in_ap, r=(), w=()):
        return sch.dma(q, lambda e, o=out_ap, i=in_ap: e.dma_start(out=o, in_=i), r=r, w=w)

    ld("sp", ident_f, ident_in, w=[k_cst])
    ld("sp", iota_f, iota_in, w=[k_cst])
    ld("sp", sel64_f, sel64_in, w=[k_cst])
    ld("sp", cT_sb, cT_in, w=[k_cst])
    for (dst, src, n) in ((rC64, rc64, 64), (rS64, rs64, 64), (rC32, rc32, 32), (rS32, rs32, 32)):
        ld("sp", dst, src.rearrange("(t p) d -> p t d", p=128), w=[k_cst])
    sch.op("dve", lambda e: e.tensor_copy(out=ident_bf, in_=ident_f), r=[k_cst], w=[k_cst])
    sch.op("act", lambda e: e.activation(out=scT_f, in_=cT_sb, func=AF.Silu), r=[k_cst], w=[k_cst])
    sch.op("dve", lambda e: e.tensor_copy(out=scT_bf, in_=scT_f), r=[k_cst], w=[k_cst])
    sch.op("dve", lambda e: e.tensor_copy(out=scRep, in_=scT_f.unsqueeze(2).to_broadcast([128, 32, 128])),
           r=[k_cst], w=[k_cst])

    k_X = [Trk() for _ in range(NT)]
    ld("sp", X[0:L, :], ctx_in, w=k_X[0:2])
    for j in range(4):
        ld("sp", X[L + j * 512:L + (j + 1) * 512, :], x_in[j * 512:(j + 1) * 512, :], w=k_X[2 + 4 * j:6 + 4 * j])

    k_scr = {n: Trk() for n in ("QaT", "KaT", "QcT", "KcT", "QbT", "KbT", "Va", "Vb", "Vc", "GT", "OT", "YT",
                                "GREP", "HID", "SWTL", "SWTC")}

    def kind_of(t):
        return 1 if t < 2 else 0

    for li in range(nl):
        last = (li == DEPTH - 1) or bool(dbg.get("force_last"))

        new_phase()
        k_mod = Trk()
        ld("sp", bmT, b_modT[li], w=[k_mod])
        ld("sp", n1T, norm1T[li], w=[k_mod])
        ld("sp", n2T, norm2T[li], w=[k_mod])
        wm = [ABF.get(16 * 512, (16, 512)) for _ in range(2)]
        k_wm = [Trk(), Trk()]
        bmr = [AFF.get(512) for _ in range(2)]
        k_bmr = [Trk(), Trk()]
        grs = [AFF.get(512) for _ in range(2)]
        k_grs = [Trk(), Trk()]
        wmv = w_mod[li].rearrange("(k p) c -> p k c", p=128)
        psA = bank(0)[:, 0:192].rearrange("p (a b) -> p a b", a=96)
        ngr = 0
        for j in range(24):
            b = j % 2
            ld("pool", wm[b], wmv[:, :, j * 512:(j + 1) * 512], w=[k_wm[b]])
            for q in range(4):
                cc = j * 4 + q
                for k in range(KD):
                    sch.op("pe", lambda e, o=psA[:, cc, :], l=wm[b][:, k, q * 128:(q + 1) * 128],
                           r_=scT_bf[:, 2 * k:2 * k + 2], st=(k == 0), sp_=(k == KD - 1):
                           e.matmul(o, l, r_, start=st, stop=sp_),
                           r=[k_wm[b], k_cst], w=[ptrk[0]])
            which = {8: 0, 9: 0, 10: 0, 11: 0, 20: 1, 21: 1, 22: 1, 23: 1}.get(j)
            if which is not None:
                cb = (j - 8) if which == 0 else (j - 20)
                g0 = 2 * D if which == 0 else 5 * D
                for kind in range(2):
                    pb_ = 2 + (ngr % 4)
                    for k in range(KD):
                        sch.op("pe", lambda e, o=bank(pb_), l=scRep[:, 2 * k + kind, :], r_=wm[b][:, k, :],
                               st=(k == 0), sp_=(k == KD - 1): e.matmul(o, l, r_, start=st, stop=sp_),
                               r=[k_wm[b], k_cst], w=[ptrk[pb_]])
                    sb = ngr % 2
                    ld("sp", bmr[sb], b_mod[li, g0 + cb * 512:g0 + (cb + 1) * 512].partition_broadcast(128),
                       w=[k_bmr[sb]])
                    sch.op("dve", lambda e, o=grs[sb], a=bank(pb_), b_=bmr[sb]:
                           e.tensor_tensor(out=o, in0=a, in1=b_, op=ALU.add),
                           r=[ptrk[pb_], k_bmr[sb]], w=[k_grs[sb]])
                    ld("sp", GREP[which, kind, :, cb * 512:(cb + 1) * 512], grs[sb], r=[k_grs[sb]],
                       w=[k_scr["GREP"]])
                    ngr += 1
        sch.op("dve", lambda e: e.tensor_tensor(out=modT, in0=psA,
                                                in1=bmT.unsqueeze(2).to_broadcast([128, 96, 2]), op=ALU.add),
               r=[ptrk[0], k_mod], w=[k_mod])
        mc = modc.rearrange("p a (k c) -> p a k c", c=2)
        for (dst, sc_lo, sh_lo, nT) in ((0, 16, 0, n1T), (2, 64, 48, n2T)):
            sch.op("dve", lambda e, o=mc[:, dst], a=modT[:, sc_lo:sc_lo + 16, :], n_=nT:
                   e.scalar_tensor_tensor(out=o, in0=a, scalar=1.0,
                                          in1=n_.unsqueeze(2).to_broadcast([128, 16, 2]),
                                          op0=ALU.add, op1=ALU.mult),
                   r=[k_mod], w=[k_mod])
            sch.op("dve", lambda e, o=mc[:, dst + 1], a=modT[:, sh_lo:sh_lo + 16, :]:
                   e.tensor_copy(out=o, in_=a), r=[k_mod], w=[k_mod])

        def modcol(j, k, kind):
            return modc[:, j, 2 * k + kind:2 * k + kind + 1]

        new_phase()
        hT = ABF.get(KD * T, (KD, T))
        k_hT = [Trk() for _ in range(NT)]
        xb = [AFF.get(D) for _ in range(2)]
        k_xb = [Trk(), Trk()]
        scr6k = ABF.get(3 * D)
        junk = scr6k[:, 0:D]
        k_junk = Trk()
        xn = [scr6k[:, D:2 * D], scr6k[:, 2 * D:3 * D]]
        k_xn = [Trk(), Trk()]
        st_f = AFF.get(4 * NT, (4, NT))
        k_st = Trk()

        def norm_tile(t, xbuf, kx, xnbuf, kxn):
            sch.op("act", lambda e, o=junk, i=xbuf, a=st_f[:, 0, t:t + 1]:
                   e.activation(out=o, in_=i, func=AF.Square, accum_out=a), r=[kx], w=[k_junk, k_st])
            sch.op("act", lambda e, o=st_f[:, 1, t:t + 1], i=st_f[:, 0, t:t + 1]:
                   e.activation(out=o, in_=i, func=AF.Sqrt, scale=1.0 / D, bias=EPS), r=[k_st], w=[k_st])
            sch.op("dve", lambda e, o=st_f[:, 2, t:t + 1], i=st_f[:, 1, t:t + 1]: e.reciprocal(out=o, in_=i),
                   r=[k_st], w=[k_st])
            sch.op("dve", lambda e, o=xnbuf, i=xbuf, s_=st_f[:, 2, t:t + 1]:
                   e.tensor_scalar(out=o, in0=i, scalar1=s_, scalar2=None, op0=ALU.mult),
                   r=[kx, k_st], w=[kxn])

        def transpose_mod(t, xnbuf, kxn, dst_fn, kdst, gj):
            kind = kind_of(t)
            for g in range(4):
                pb_ = 4 + g
                for q in range(4):
                    k = g * 4 + q
                    sch.op("pe", lambda e, o=bank_bf(pb_)[:, q * 128:(q + 1) * 128],
                           i=xnbuf[:, k * 128:(k + 1) * 128]: e.transpose(out=o, in_=i, identity=ident_bf),
                           r=[kxn, k_cst], w=[ptrk[pb_]])
                for q in range(4):
                    k = g * 4 + q
                    sch.op("act", lambda e, o=dst_fn(k), i=bank_bf(pb_)[:, q * 128:(q + 1) * 128],
                           sc_=modcol(gj, k, kind), bi_=modcol(gj + 1, k, kind):
                           e.activation(out=o, in_=i, func=AF.Identity, scale=sc_, bias=bi_),
                           r=[ptrk[pb_], k_mod], w=[kdst])

        for t in range(NT):
            b = t % 2
            ld("pool", xb[b], X[t * 128:(t + 1) * 128, :], r=[k_X[t]], w=[k_xb[b]])
            norm_tile(t, xb[b], k_xb[b], xn[b], k_xn[b])
            transpose_mod(t, xn[b], k_xn[b], lambda k, t=t: hT[:, k, t * 128:(t + 1) * 128], k_hT[t], 0)
        if dbg.get("stopB"):
            break

        barrier()
        winv = w_in[li].rearrange("(k p) c -> p k c", p=128)
        gn = {}
        k_gn = Trk()
        for nm, n in (("na_q_norm", 64), ("na_k_norm", 64), ("mla_q_a_norm", 512), ("mla_kv_a_norm", 256),
                      ("mla_q_norm", 96), ("mla_k_norm", 96), ("gqa_q_norm", 64), ("gqa_k_norm", 64)):
            gn[nm] = AFF.get(n)
            ld("sp", gn[nm], gains[nm][li].partition_broadcast(128), w=[k_gn])
        sch.op("dve", lambda e, o=gn["na_q_norm"]: e.tensor_scalar(out=o, in0=o, scalar1=0.125, scalar2=None,
                                                                   op0=ALU.mult), r=[k_gn], w=[k_gn])
        wblk = [ABF.get(KD * 512, (KD, 512)) for _ in range(2)]
        k_wblk = [Trk(), Trk()]
        wqb = ABF.get(4 * 768, (4, 768))
        wkvb = ABF.get(2 * 1024, (2, 1024))
        k_w2 = Trk()
        ld("pool", wqb, w_q_b[li].rearrange("(k p) c -> p k c", p=128), w=[k_w2])
        ld("pool", wkvb, w_kv_b[li].rearrange("(k p) c -> p k c", p=128), w=[k_w2])
        sq = [AFF.get(1024) for _ in range(2)]
        k_sq = [Trk(), Trk()]
        o1 = [AFF.get(768) for _ in range(2)]
        k_o1 = [Trk(), Trk()]
        sm = AFF.get(64, (4, 16))
        k_sm = Trk()
        kpe_f = AFF.get(32)
        k_kpe = Trk()
        nb = [ABF.get(1024) for _ in range(2)]
        k_nb = [Trk(), Trk()]
        stg = [ABF.get(1024) for _ in range(2)]
        k_stg = [Trk(), Trk()]
        cqT = ABF.get(6 * 128, (6, 128))
        k_cqT = Trk()
        wctr = [0]
        uctr = [0]
        AFF_tmp = [AFF.get(768) for _ in range(2)]
        k_tmp = [Trk(), Trk()]

        def load_w(c0, n):
            b = wctr[0] % 2
            wctr[0] += 1
            ld("pool", wblk[b][:, :, 0:n], winv[:, :, c0:c0 + n], w=[k_wblk[b]])
            return wblk[b], k_wblk[b]

        def proj(t, wb, kwb, c0, n, pb_):
            for k in range(KD):
                sch.op("pe", lambda e, o=bank(pb_)[:, 0:n], l=hT[:, k, t * 128:(t + 1) * 128],
                       r_=wb[:, k, c0:c0 + n], st=(k == 0), sp_=(k == KD - 1):
                       e.matmul(o, l, r_, start=st, stop=sp_), r=[k_hT[t], kwb], w=[ptrk[pb_]])

        def normhead(src, ksrc, nh, hd, gain, dst, kdst):
            n = nh * hd
            u = uctr[0] % 2
            uctr[0] += 1
            sch.op("act", lambda e, o=sq[u][:, 0:n], i=src: e.activation(out=o, in_=i, func=AF.Square),
                   r=ksrc, w=[k_sq[u]])
            sch.op("dve", lambda e, o=sm[:, 0, 0:nh], i=sq[u][:, 0:n].rearrange("p (h d) -> p h d", h=nh):
                   e.tensor_reduce(out=o, in_=i, axis=AX.X, op=ALU.add), r=[k_sq[u]], w=[k_sm])
            sch.op("act", lambda e, o=sm[:, 1, 0:nh], i=sm[:, 0, 0:nh]:
                   e.activation(out=o, in_=i, func=AF.Sqrt, scale=1.0 / hd, bias=EPS), r=[k_sm], w=[k_sm])
            sch.op("dve", lambda e, o=sm[:, 2, 0:nh], i=sm[:, 1, 0:nh]: e.reciprocal(out=o, in_=i),
                   r=[k_sm], w=[k_sm])
            sch.op("dve", lambda e, o=sq[u][:, 0:n].rearrange("p (h d) -> p h d", h=nh),
                   i=src.rearrange("p (h d) -> p h d", h=nh),
                   s_=sm[:, 2, 0:nh].unsqueeze(2).to_broadcast([128, nh, hd]):
                   e.tensor_tensor(out=o, in0=i, in1=s_, op=ALU.mult), r=list(ksrc) + [k_sm, k_sq[u]], w=[k_sq[u]])
            sch.op("dve", lambda e, o=dst.rearrange("p (h d) -> p h d", h=nh),
                   i=sq[u][:, 0:n].rearrange("p (h d) -> p h d", h=nh),
                   g_=gain.unsqueeze(1).to_broadcast([128, nh, hd]):
                   e.tensor_tensor(out=o, in0=i, in1=g_, op=ALU.mult), r=[k_sq[u], k_gn], w=kdst)

        def rope(t, buf, kbuf, nh, hd, r0, R, tC, tS, dst, kdst):
            tt = t - 2
            m = R // 4
            u = uctr[0] % 2
            uctr[0] += 1
            bv = buf.rearrange("p (h d) -> p h d", h=nh)
            xs = sq[u][:, 0:nh * R].rearrange("p (h s a m) -> p h s a m", h=nh, s=2, a=2)
            xin = bv[:, :, r0:r0 + R].rearrange("p h (s a m) -> p h s a m", s=2, a=2)
            Sv = tS[:, tt, :].rearrange("p (s a m) -> p s a m", s=2, a=2)
            for a in range(2):
                for s_ in range(2):
                    sch.op("dve", lambda e, o=xs[:, :, s_, a, :], i=xin[:, :, s_, 1 - a, :],
                           g_=Sv[:, s_, a, :].unsqueeze(1).to_broadcast([128, nh, m]):
                           e.tensor_tensor(out=o, in0=i, in1=g_, op=ALU.mult),
                           r=[kbuf, k_cst, k_sq[u]], w=[k_sq[u]])
            t1 = o1[u][:, 0:nh * R].rearrange("p (h r) -> p h r", h=nh)
            sch.op("dve", lambda e, o=t1, i=bv[:, :, r0:r0 + R],
                   g_=tC[:, tt, :].unsqueeze(1).to_broadcast([128, nh, R]):
                   e.tensor_tensor(out=o, in0=i, in1=g_, op=ALU.mult), r=[kbuf, k_cst], w=[k_o1[u]])
            dv = dst.rearrange("p (h d) -> p h d", h=nh)
            sch.op("dve", lambda e, o=dv[:, :, r0:r0 + R], i=t1,
                   x_=sq[u][:, 0:nh * R].rearrange("p (h r) -> p h r", h=nh):
                   e.tensor_tensor(out=o, in0=i, in1=x_, op=ALU.add), r=[k_o1[u], k_sq[u]], w=kdst)
            if r0 > 0:
                sch.op("dve", lambda e, o=dv[:, :, 0:r0], i=bv[:, :, 0:r0]: e.tensor_copy(out=o, in_=i),
                       r=[kbuf], w=kdst)

        def transpose_out(t, src, ksrc, ncol_blocks, rows, dst_dram, kd):
            s = uctr[0] % 2
            uctr[0] += 1
            pb_ = 6 + s
            for q in range(ncol_blocks):
                sch.op("pe", lambda e, o=bank_bf(pb_)[0:rows, q * 128:(q + 1) * 128],
                       i=src[:, q * rows:(q + 1) * rows]: e.transpose(out=o, in_=i, identity=ident_bf),
                       r=[ksrc, k_cst], w=[ptrk[pb_]])
            n = ncol_blocks * 128
            sch.op("act", lambda e, o=stg[s][0:rows, 0:n], i=bank_bf(pb_)[0:rows, 0:n]: e.copy(out=o, in_=i),
                   r=[ptrk[pb_]], w=[k_stg[s]])
            ld("sp", dst_dram, stg[s][0:rows, 0:n].rearrange("p (q c) -> p q c", q=ncol_blocks),
               r=[k_stg[s]], w=[kd])

        def qk_group(c0, nh, gain, dst_dram_fn, kd, do_rope):
            wb, kwb = load_w(c0, nh * 64)
            for t in range(NT):
                pb_ = t % 4
                proj(t, wb, kwb, 0, nh * 64, pb_)
                u = t % 2
                if do_rope and t >= 2:
                    normhead(bank(pb_)[:, 0:nh * 64], [ptrk[pb_]], nh, 64, gain, AFF_tmp[u][:, 0:nh * 64],
                             [k_tmp[u]])
                    rope(t, AFF_tmp[u][:, 0:nh * 64], k_tmp[u], nh, 64, 0, 64, rC64, rS64,
                         nb[u][:, 0:nh * 64], [k_nb[u]])
                else:
                    normhead(bank(pb_)[:, 0:nh * 64], [ptrk[pb_]], nh, 64, gain, nb[u][:, 0:nh * 64], [k_nb[u]])
                nblk = max(1, nh * 64 // 128)
                transpose_out(t, nb[u], k_nb[u], nblk, 128 if nh >= 2 else 64, dst_dram_fn(t), kd)

        qk_group(0, 8, gn["na_q_norm"],
                 lambda t: QaT.rearrange("(q p) t -> p q t", p=128)[:, :, t * 128:(t + 1) * 128], k_scr["QaT"], False)
        qk_group(512, 8, gn["na_k_norm"],
                 lambda t: KaT.rearrange("(q p) t -> p q t", p=128)[:, :, t * 128:(t + 1) * 128], k_scr["KaT"], False)
        if dbg.get("stopC1"):
            break
        qk_group(2336, 8, gn["gqa_q_norm"],
                 lambda t: QcT.rearrange("(q p) t -> p q t", p=128)[:, :, t * 128:(t + 1) * 128], k_scr["QcT"], True)
        if dbg.get("stopC2"):
            break
        wb, kwb = load_w(2848, 256)
        for t in range(NT):
            pb_ = t % 4
            u = t % 2
            proj(t, wb, kwb, 0, 256, pb_)
            if t >= 2:
                normhead(bank(pb_)[:, 0:128], [ptrk[pb_]], 2, 64, gn["gqa_k_norm"], AFF_tmp[u][:, 0:128],
                         [k_tmp[u]])
                rope(t, AFF_tmp[u][:, 0:128], k_tmp[u], 2, 64, 0, 64, rC64, rS64, nb[u][:, 0:128], [k_nb[u]])
            else:
                normhead(bank(pb_)[:, 0:128], [ptrk[pb_]], 2, 64, gn["gqa_k_norm"], nb[u][:, 0:128], [k_nb[u]])
            sch.op("act", lambda e, o=nb[u][:, 128:256], i=bank(pb_)[:, 128:256]: e.copy(out=o, in_=i),
                   r=[ptrk[pb_]], w=[k_nb[u]])
            ld("sp", Vc[t * 128:(t + 1) * 128, :], nb[u][:, 128:256], r=[k_nb[u]], w=[k_scr["Vc"]])
            transpose_out(t, nb[u], k_nb[u], 1, 128,
                          KcT.rearrange("(q p) t -> p q t", p=128)[:, :, t * 128:(t + 1) * 128], k_scr["KcT"])
        wb, kwb = load_w(1024, 512)
        for t in range(NT):
            pb_ = t % 4
            u = t % 2
            proj(t, wb, kwb, 0, 512, pb_)
            sch.op("act", lambda e, o=nb[u][:, 0:512], i=bank(pb_): e.copy(out=o, in_=i),
                   r=[ptrk[pb_]], w=[k_nb[u]])
            ld("sp", Va[t * 128:(t + 1) * 128, :], nb[u][:, 0:512], r=[k_nb[u]], w=[k_scr["Va"]])

        if dbg.get("stopC3"):
            break
        wcq, k_wcq = load_w(1536, 512)
        wckv, k_wckv = load_w(2048, 288)
        QbTv = QbT.rearrange("h d t -> d h t")
        KbTv = KbT.rearrange("h d t -> d h t")
        for t in range(NT):
            lat = t >= 2
            proj(t, wcq, k_wcq, 0, 512, 0)
            proj(t, wckv, k_wckv, 0, 288, 1)
            normhead(bank(0), [ptrk[0]], 1, 512, gn["mla_q_a_norm"], nb[0][:, 0:512], [k_nb[0]])
            normhead(bank(1)[:, 0:256], [ptrk[1]], 1, 256, gn["mla_kv_a_norm"], nb[0][:, 512:768], [k_nb[0]])
            sch.op("act", lambda e, o=kpe_f, i=bank(1)[:, 256:288]: e.copy(out=o, in_=i), r=[ptrk[1]], w=[k_kpe])
            for q in range(6):
                sch.op("pe", lambda e, o=bank_bf(6)[:, q * 128:(q + 1) * 128], i=nb[0][:, q * 128:(q + 1) * 128]:
                       e.transpose(out=o, in_=i, identity=ident_bf), r=[k_nb[0], k_cst], w=[ptrk[6]])
            sch.op("act", lambda e, o=cqT.rearrange("p a b -> p (a b)"), i=bank_bf(6)[:, 0:768]: e.copy(out=o, in_=i),
                   r=[ptrk[6]], w=[k_cqT])
            if dbg.get("mla_stop") == 1:
                continue
            for (cb, n, pb_) in ((0, 512, 2), (512, 256, 3)):
                for k in range(4):
                    sch.op("pe", lambda e, o=bank(pb_)[:, 0:n], l=cqT[:, k, :], r_=wqb[:, k, cb:cb + n],
                           st=(k == 0), sp_=(k == 3): e.matmul(o, l, r_, start=st, stop=sp_),
                           r=[k_cqT, k_w2], w=[ptrk[pb_]])
            normhead(pps[1][:, 0:768], [ptrk[2], ptrk[3]], 8, 96, gn["mla_q_norm"], AFF_tmp[0][:, 0:768], [k_tmp[0]])
            if lat:
                rope(t, AFF_tmp[0][:, 0:768], k_tmp[0], 8, 96, 64, 32, rC32, rS32, nb[0][:, 0:768], [k_nb[0]])
            else:
                sch.op("dve", lambda e, o=nb[0][:, 0:768], i=AFF_tmp[0][:, 0:768]: e.tensor_copy(out=o, in_=i),
                       r=[k_tmp[0]], w=[k_nb[0]])
            transpose_out(t, nb[0], k_nb[0], 8, 96, QbTv[:, :, t * 128:(t + 1) * 128], k_scr["QbT"])
            if dbg.get("mla_stop") == 2:
                continue
            for (cb, pb_) in ((0, 4), (512, 5)):
                for k in range(2):
                    sch.op("pe", lambda e, o=bank(pb_), l=cqT[:, 4 + k, :], r_=wkvb[:, k, cb:cb + 512],
                           st=(k == 0), sp_=(k == 1): e.matmul(o, l, r_, start=st, stop=sp_),
                           r=[k_cqT, k_w2], w=[ptrk[pb_]])
            kbf = AFF_tmp[1][:, 0:768].rearrange("p (h d) -> p h d", h=8)
            if dbg.get("mla_stop") == 31:
                continue
            for hb2 in range(2):
                kvb = bank(4 + hb2).rearrange("p (h d) -> p h d", h=4)
                sch.op("dve", lambda e, o=nb[1][:, hb2 * 256:(hb2 + 1) * 256].rearrange("p (h d) -> p h d", h=4),
                       i=kvb[:, :, 64:128]: e.tensor_copy(out=o, in_=i), r=[ptrk[4 + hb2]], w=[k_nb[1]])
                if dbg.get("mla_stop") == 32:
                    continue
                sch.op("dve", lambda e, o=kbf[:, hb2 * 4:(hb2 + 1) * 4, 0:64], i=kvb[:, :, 0:64]:
                       e.tensor_copy(out=o, in_=i), r=[ptrk[4 + hb2]], w=[k_tmp[1]])
            if dbg.get("mla_stop") in (32, 33):
                continue
            ld("sp", Vb[t * 128:(t + 1) * 128, :], nb[1][:, 0:512], r=[k_nb[1]], w=[k_scr["Vb"]])
            sch.op("dve", lambda e, o=kbf[:, :, 64:96], i=kpe_f.unsqueeze(1).to_broadcast([128, 8, 32]):
                   e.tensor_copy(out=o, in_=i), r=[k_kpe], w=[k_tmp[1]])
            if dbg.get("mla_stop") == 3:
                continue
            normhead(AFF_tmp[1][:, 0:768], [k_tmp[1]], 8, 96, gn["mla_k_norm"], AFF_tmp[1][:, 0:768], [k_tmp[1]])
            if dbg.get("mla_stop") == 4:
                continue
            if lat:
                rope(t, AFF_tmp[1][:, 0:768], k_tmp[1], 8, 96, 64, 32, rC32, rS32, nb[1][:, 0:768], [k_nb[1]])
            else:
                sch.op("dve", lambda e, o=nb[1][:, 0:768], i=AFF_tmp[1][:, 0:768]: e.tensor_copy(out=o, in_=i),
                       r=[k_tmp[1]], w=[k_nb[1]])
            transpose_out(t, nb[1], k_nb[1], 8, 96, KbTv[:, :, t * 128:(t + 1) * 128], k_scr["KbT"])

        if dbg.get("stopC4"):
            break
        barrier()
        gst = [scr6k[:, 0:T], scr6k[:, T:2 * T]]
        k_gst = [Trk(), Trk()]
        tblocks = [(0, 256)] + [(L + 512 * j, 512) for j in range(4)]
        if last:
            tblocks = tblocks[1:]
        t_lo = tblocks[0][0]
        gctr = 0
        for blk in range(12):
            wb, kwb = load_w(3104 + blk * 512, 512)
            for q in range(4):
                gc = blk * 4 + q
                sg = gc % 2
                for (q0, nq) in tblocks:
                    pb_ = gctr % 4
                    gctr += 1
                    tl = list(range(q0 // 128, (q0 + nq) // 128))
                    for k in range(KD):
                        sch.op("pe", lambda e, o=bank(pb_)[:, 0:nq], l=wb[:, k, q * 128:(q + 1) * 128],
                               r_=hT[:, k, q0:q0 + nq], st=(k == 0), sp_=(k == KD - 1):
                               e.matmul(o, l, r_, start=st, stop=sp_),
                               r=[kwb] + [k_hT[t] for t in tl], w=[ptrk[pb_]])
                    sch.op("act", lambda e, o=gst[sg][:, q0:q0 + nq], i=bank(pb_)[:, 0:nq]:
                           e.activation(out=o, in_=i, func=AF.Sigmoid), r=[ptrk[pb_]], w=[k_gst[sg]])
                ld("sp", GT[gc * 128:(gc + 1) * 128, t_lo:T], gst[sg][:, t_lo:T], r=[k_gst[sg]], w=[k_scr["GT"]])
        if dbg.get("stopC"):
            break
        new_phase()
        qt = [ABF.get(T) for _ in range(2)]
        kt = [ABF.get(T) for _ in range(2)]
        vt = [ABF.get(NT * 128, (NT, 128)) for _ in range(2)]
        k_q = [Trk(), Trk()]
        k_k = [Trk(), Trk()]
        k_v = [Trk(), Trk()]
        pT = [ABF.get(512) for _ in range(4)]
        k_pT = [Trk() for _ in range(4)]
        pending = [None]
        bias_bfs = [ABF.get(NA_NUNIQ * 512, (NA_NUNIQ, 512)) for _ in range(2)]
        k_biass = [Trk(), Trk()]
        bstage = [AFF.get(512) for _ in range(2)]
        k_bst = [Trk(), Trk()]
        posb = [AFF.get(512) for _ in range(2)]
        lnb = [AFF.get(512) for _ in range(2)]
        rbb = [AFF.get(512) for _ in range(2)]
        k_fin = [Trk(), Trk()]
        oTb = [ABF.get(512) for _ in range(2)]
        k_oT = [Trk(), Trk()]
        for b in range(2):
            sch.op("dve", lambda e, o=vt[b]: e.memset(o, 0.0), w=[k_v[b]])
            sch.op("dve", lambda e, o=vt[b][:, :, 64:65]: e.memset(o, 1.0), w=[k_v[b]])
            sch.op("dve", lambda e, o=qt[b]: e.memset(o, 0.0), w=[k_q[b]])
            sch.op("dve", lambda e, o=kt[b]: e.memset(o, 0.0), w=[k_k[b]])
        jobs = []
        for h in range(8):
            jobs.append((0, h, QaT[h * 64:(h + 1) * 64, :], KaT[h * 64:(h + 1) * 64, :], Va[:, h * 64:(h + 1) * 64],
                         64, 1.0))
        for h in range(8):
            jobs.append((1, h, QbT[h], KbT[h], Vb[:, h * 64:(h + 1) * 64], 96, 96 ** -0.5))
        for h in range(8):
            g = h // 4
            jobs.append((2, h, QcT[h * 64:(h + 1) * 64, :], KcT[g * 64:(g + 1) * 64, :], Vc[:, g * 64:(g + 1) * 64],
                         64, 0.125))
        sctr = 0
        pctr = 0
        fctr = 0
        srcname = {0: ("QaT", "KaT", "Va"), 1: ("QbT", "KbT", "Vb"), 2: ("QcT", "KcT", "Vc")}
        for j, (mix, h, Qs, Ks, Vs, dk, scale) in enumerate(jobs):
            buf = j % 2
            nq_, nk_, nv_ = srcname[mix]
            sch.op("dve", lambda e, o=qt[buf][64:128, :]: e.memset(o, 0.0), w=[k_q[buf]])
            sch.op("dve", lambda e, o=kt[buf][64:128, :]: e.memset(o, 0.0), w=[k_k[buf]])
            ld("pool", qt[buf][0:dk, :], Qs, r=[k_scr[nq_]], w=[k_q[buf]])
            ld("pool", kt[buf][0:dk, :], Ks, r=[k_scr[nk_]], w=[k_k[buf]])
            ld("pool", vt[buf][:, :, 0:64], Vs.rearrange("(n p) d -> p n d", p=128), r=[k_scr[nv_]], w=[k_v[buf]])
            bias_bf = bias_bfs[h % 2]
            k_bias = k_biass[h % 2]
            if mix == 0:
                for tg in range(2):
                    ld("pool", bias_bf[:, tg * 10:(tg + 1) * 10, :],
                       na_bias[li, h, tg * 10:(tg + 1) * 10].rearrange("t p c -> p t c"), w=[k_bias])
            blocks = [(L + 512 * qb, 512, qb) for qb in range(4)]
            if not last:
                blocks.append((0, 256, None))
            LA = 2

            def finalize(fin):
                (q0f, nqf, s2f, pobf, mixf, hf) = fin
                pbb = 4 + s2f
                sch.op("dve", lambda e, o=posb[s2f][:, 0:nqf], i=bank(pobf)[:, 0:nqf]: e.tensor_copy(out=o, in_=i),
                       r=[ptrk[pobf]], w=[k_fin[s2f]])
                sch.op("pe", lambda e, o=bank(pbb)[:, 0:nqf], r_=posb[s2f][:, 0:nqf]:
                       e.matmul(o, sel64_f, r_, start=True, stop=True),
                       r=[k_fin[s2f], k_cst], w=[ptrk[pbb]])
                sch.op("dve", lambda e, o=rbb[s2f][0:64, 0:nqf], i=bank(pbb)[0:64, 0:nqf]: e.reciprocal(out=o, in_=i),
                       r=[ptrk[pbb]], w=[k_fin[s2f]])
                sch.op("dve", lambda e, o=oTb[s2f][0:64, 0:nqf], a=posb[s2f][0:64, 0:nqf], b_=rbb[s2f][0:64, 0:nqf]:
                       e.tensor_tensor(out=o, in0=a, in1=b_, op=ALU.mult), r=[k_fin[s2f]], w=[k_oT[s2f]])
                ld("sp", OT[mixf, hf * 64:(hf + 1) * 64, q0f:q0f + nqf], oTb[s2f][0:64, 0:nqf], r=[k_oT[s2f]],
                   w=[k_scr["OT"]])

            for (q0, nq, qb) in blocks:
                if qb is None:
                    kch = [(0, None), (1, None)]
                elif mix == 0:
                    kch = [(0, None), (1, None)] + [(2 + kc, NA_TILE_ID[qb][jj]) for jj, kc in enumerate(NA_PLAN[qb])]
                else:
                    kch = [(kc, None) for kc in range(NT)]
                s2 = fctr % 2
                fctr += 1
                pob = 6 + s2
                nk = len(kch)
                pslots = [None] * nk
                for idx in range(nk + LA):
                    if idx < nk:
                        kc, bi = kch[idx]
                        psb = sctr % 4
                        sctr += 1
                        sch.op("pe", lambda e, o=bank(psb)[:, 0:nq], l=kt[buf][:, kc * 128:(kc + 1) * 128],
                               r_=qt[buf][:, q0:q0 + nq], sp_=(bi is None): e.matmul(o, l, r_, start=True, stop=sp_),
                               r=[k_k[buf], k_q[buf]], w=[ptrk[psb]])
                        if bi is not None:
                            sch.op("pe", lambda e, o=bank(psb)[:, 0:nq], r_=bias_bf[:, bi, 0:nq]:
                                   e.matmul(o, ident_bf, r_, start=False, stop=True), r=[k_bias, k_cst], w=[ptrk[psb]])
                        p_ = pctr % 4
                        pctr += 1
                        pslots[idx] = p_
                        sch.op("act", lambda e, o=pT[p_][:, 0:nq], i=bank(psb)[:, 0:nq], sc_=scale:
                               e.activation(out=o, in_=i, func=AF.Exp, scale=sc_), r=[ptrk[psb]], w=[k_pT[p_]])
                    j2 = idx - LA
                    if j2 >= 0:
                        kc2 = kch[j2][0]
                        p2 = pslots[j2]
                        sch.op("pe", lambda e, o=bank(pob)[:, 0:nq], l=vt[buf][:, kc2, :], r_=pT[p2][:, 0:nq],
                               st=(j2 == 0), sp_=(j2 == nk - 1): e.matmul(o, l, r_, start=st, stop=sp_),
                               r=[k_v[buf], k_pT[p2]], w=[ptrk[pob]])
                    if idx == LA and pending[0] is not None:
                        finalize(pending[0])
                        pending[0] = None
                if pending[0] is not None:
                    finalize(pending[0])
                pending[0] = (q0, nq, s2, pob, mix, h)
        if pending[0] is not None:
            finalize(pending[0])
            pending[0] = None
        if dbg.get("stopD"):
            break

        new_phase()
        wbr = [ABF.get(4 * D, (4, D)) for _ in range(3)]
        k_wbr = Trk()
        for br in range(3):
            ld("pool", wbr[br], w_br[br][li].rearrange("(k p) c -> p k c", p=128), w=[k_wbr])
        otb = [[ABF.get(4 * 512, (4, 512)) for _ in range(3)] for _ in range(2)]
        k_otb = [Trk(), Trk()]
        gtb = [[ABF.get(512) for _ in range(3)] for _ in range(2)]
        k_gtb = [Trk(), Trk()]
        ytb = [ABF.get(KD * 512, (KD, 512)) for _ in range(2)]
        k_ytb = [Trk(), Trk()]
        ef = [[AFF.get(512) for _ in range(3)] for _ in range(2)]
        k_ef = [Trk(), Trk()]
        OTv = OT.rearrange("m (k p) t -> m p k t", p=128)
        ectr = 0
        for bi_, (q0, nq) in enumerate(tblocks):
            ob = bi_ % 2
            for br in range(3):
                ld("pool", otb[ob][br][:, :, 0:nq], OTv[br][:, :, q0:q0 + nq], r=[k_scr["OT"]], w=[k_otb[ob]])
            for dc in range(KD):
                gb = ectr % 2
                ectr += 1
                for br in range(3):
                    ld("pool", gtb[gb][br][:, 0:nq], GT[br * D + dc * 128:br * D + (dc + 1) * 128, q0:q0 + nq],
                       r=[k_scr["GT"]], w=[k_gtb[gb]])
                pbs = [(3 * gb + br) for br in range(3)]
                for br in range(3):
                    for k in range(4):
                        sch.op("pe", lambda e, o=bank(pbs[br])[:, 0:nq], l=wbr[br][:, k, dc * 128:(dc + 1) * 128],
                               r_=otb[ob][br][:, k, 0:nq], st=(k == 0), sp_=(k == 3):
                               e.matmul(o, l, r_, start=st, stop=sp_), r=[k_wbr, k_otb[ob]], w=[ptrk[pbs[br]]])
                for br in range(3):
                    sch.op("dve", lambda e, o=ef[gb][br][:, 0:nq], a=bank(pbs[br])[:, 0:nq], b_=gtb[gb][br][:, 0:nq]:
                           e.tensor_tensor(out=o, in0=a, in1=b_, op=ALU.mult),
                           r=[ptrk[pbs[br]], k_gtb[gb]], w=[k_ef[gb]])
                sch.op("dve", lambda e, o=ef[gb][0][:, 0:nq], a=ef[gb][0][:, 0:nq], b_=ef[gb][1][:, 0:nq]:
                       e.tensor_tensor(out=o, in0=a, in1=b_, op=ALU.add), r=[k_ef[gb]], w=[k_ef[gb]])
                sch.op("dve", lambda e, o=ytb[ob][:, dc, 0:nq], a=ef[gb][0][:, 0:nq], b_=ef[gb][2][:, 0:nq]:
                       e.tensor_tensor(out=o, in0=a, in1=b_, op=ALU.add), r=[k_ef[gb]], w=[k_ytb[ob]])
            ld("sp", YT.rearrange("(k p) t -> p k t", p=128)[:, :, q0:q0 + nq], ytb[ob][:, :, 0:nq],
               r=[k_ytb[ob]], w=[k_scr["YT"]])

        new_phase()
        wo = ABF.get(KD * D, (KD, D))
        k_wo = Trk()
        for j4 in range(4):
            ld("pool", wo[:, :, j4 * 512:(j4 + 1) * 512],
               w_out[li].rearrange("(k p) c -> p k c", p=128)[:, :, j4 * 512:(j4 + 1) * 512], w=[k_wo])
        grep1 = AFF.get(2 * D, (2, D))
        k_gr = Trk()
        for kind in range(2):
            ld("sp", grep1[:, kind, :], GREP[0, kind], r=[k_scr["GREP"]], w=[k_gr])
        yti = [ABF.get(KD * 128, (KD, 128)) for _ in range(2)]
        k_yti = [Trk(), Trk()]
        xe2 = [AFF.get(D) for _ in range(2)]
        k_xe2 = [Trk(), Trk()]
        tmpf = [AFF.get(512) for _ in range(2)]
        k_tmpf = [Trk(), Trk()]
        YTv = YT.rearrange("(k p) t -> p k t", p=128)
        octr = 0
        for t in range(2 if last else 0, NT):
            b = t % 2
            kind = kind_of(t)
            ld("pool", yti[b], YTv[:, :, t * 128:(t + 1) * 128], r=[k_scr["YT"]], w=[k_yti[b]])
            ld("pool", xe2[b], X[t * 128:(t + 1) * 128, :], r=[k_X[t]], w=[k_xe2[b]])
            for j4 in range(4):
                pb_ = octr % 4
                tb_ = octr % 2
                octr += 1
                for k in range(KD):
                    sch.op("pe", lambda e, o=bank(pb_), l=yti[b][:, k, :], r_=wo[:, k, j4 * 512:(j4 + 1) * 512],
                           st=(k == 0), sp_=(k == KD - 1): e.matmul(o, l, r_, start=st, stop=sp_),
                           r=[k_yti[b], k_wo], w=[ptrk[pb_]])
                sch.op("dve", lambda e, o=tmpf[tb_], a=bank(pb_), g_=grep1[:, kind, j4 * 512:(j4 + 1) * 512]:
                       e.tensor_tensor(out=o, in0=a, in1=g_, op=ALU.mult), r=[ptrk[pb_], k_gr], w=[k_tmpf[tb_]])
                sch.op("dve", lambda e, o=xe2[b][:, j4 * 512:(j4 + 1) * 512], a=xe2[b][:, j4 * 512:(j4 + 1) * 512],
                       t_=tmpf[tb_]: e.tensor_tensor(out=o, in0=a, in1=t_, op=ALU.add),
                       r=[k_tmpf[tb_]], w=[k_xe2[b]])
            ld("sp", X[t * 128:(t + 1) * 128, :], xe2[b], r=[k_xe2[b]], w=[k_X[t]])
        if dbg.get("stopE"):
            break
        new_phase()
        xn2 = ABF.get(NT * D, (NT, D))
        k_xn2 = [Trk() for _ in range(NT)]
        wr = ABF.get(KD * E, (KD, E))
        k_wr = Trk()
        ld("pool", wr, w_router[li].rearrange("(k p) e -> p k e", p=128), w=[k_wr])
        sel = ABF.get(16 * 256, (16, 256))
        selc = ABF.get(2 * 32, (2, 32))
        selw = [ABF.get(256) for _ in range(2)]
        swst = ABF.get(2 * S, (2, S))
        swsc = ABF.get(256)
        xeT = ABF.get(KD * 288, (KD, 288))
        wgu_r = ABF.get(16384)
        hid1 = ABF.get(8 * 288, (8, 288))
        hid = [hid1, hid1]
        wgu = [[wgu_r[:, (2 * b2 + kk) * 4096:(2 * b2 + kk + 1) * 4096].rearrange("p (k f) -> p k f", k=KD)
                for kk in range(2)] for b2 in range(2)]
        junk = wgu_r[:, 0:D]
        k_junk = Trk()
        h2T = [wgu_r[:, D + b2 * 2048:D + (b2 + 1) * 2048].rearrange("p (k f) -> p k f", k=KD) for b2 in range(2)]
        k_h2T = [Trk(), Trk()]
        xreg = AFF.get(2 * T)
        xb = [xreg[:, 0:D], xreg[:, T:T + D]]
        k_xb = [Trk(), Trk()]
        st_f = AFF.get(4 * NT, (4, NT))
        k_st = Trk()
        aff_tm = AFF.get(NT * E, (NT, E))
        posm_tm = AFF.get(NT * E, (NT, E))
        affT = AFF.get(T)
        reg2 = AFF.get(T)
        rs_f = AFF.get(16, (4, 4))
        k_aff = Trk()
        k_affT = Trk()
        for t in range(NT):
            b = t % 2
            kind = kind_of(t)
            ld("pool", xb[b], X[t * 128:(t + 1) * 128, :], r=[k_X[t]], w=[k_xb[b]])
            norm_tile(t, xb[b], k_xb[b], xn2[:, t, :], k_xn2[t])
            transpose_mod(t, xn2[:, t, :], k_xn2[t], lambda k, b=b: h2T[b][:, k, :], k_h2T[b], 2)
            pb_ = t % 2
            for k in range(KD):
                sch.op("pe", lambda e, o=bank(pb_)[:, 0:E], l=h2T[b][:, k, :], r_=wr[:, k, :],
                       st=(k == 0), sp_=(k == KD - 1): e.matmul(o, l, r_, start=st, stop=sp_),
                       r=[k_h2T[b], k_wr], w=[ptrk[pb_]])
            sch.op("dve", lambda e, o=rs_f[:, 0, 0:1], i=bank(pb_)[:, 0:E]:
                   e.tensor_reduce(out=o, in_=i, axis=AX.X, op=ALU.max), r=[ptrk[pb_]], w=[k_aff])
            sch.op("dve", lambda e, o=rs_f[:, 1, 0:1], i=rs_f[:, 0, 0:1]:
                   e.tensor_scalar(out=o, in0=i, scalar1=-1.0, scalar2=None, op0=ALU.mult), r=[k_aff], w=[k_aff])
            sch.op("act", lambda e, o=aff_tm[:, t, :], i=bank(pb_)[:, 0:E], b_=rs_f[:, 1, 0:1], a=rs_f[:, 2, 0:1]:
                   e.activation(out=o, in_=i, func=AF.Exp, bias=b_, scale=1.0, accum_out=a),
                   r=[ptrk[pb_], k_aff], w=[k_aff])
            sch.op("dve", lambda e, o=rs_f[:, 3, 0:1], i=rs_f[:, 2, 0:1]: e.reciprocal(out=o, in_=i),
                   r=[k_aff], w=[k_aff])
            sch.op("dve", lambda e, o=aff_tm[:, t, :], i=aff_tm[:, t, :], s_=rs_f[:, 3, 0:1]:
                   e.tensor_scalar(out=o, in0=i, scalar1=s_, scalar2=None, op0=ALU.mult), r=[k_aff], w=[k_aff])
            pb2 = 2 + t % 2
            sch.op("pe", lambda e, o=bank(pb2)[0:E, 0:128], i=aff_tm[:, t, :]:
                   e.transpose(out=o, in_=i, identity=ident_f), r=[k_aff, k_cst], w=[ptrk[pb2]])
            sch.op("act", lambda e, o=affT[0:E, t * 128:(t + 1) * 128], i=bank(pb2)[0:E, 0:128]: e.copy(out=o, in_=i),
                   r=[ptrk[pb2]], w=[k_affT])

        barrier()
        work = xreg[:, 0:T]
        cs = xreg[:, T:2 * T]
        m8 = rs_f.rearrange("p a b -> p (a b)")[0:E, 0:8]
        thr = AFF.get(2)
        k_tk = Trk()
        sch.op("dve", lambda e: e.tensor_copy(out=work[0:E, 0:T], in_=affT[0:E, 0:T]), r=[k_affT], w=[k_tk])
        segs = [(L, T, CAP_LAT, 0)] if last else [(L, T, CAP_LAT, 0), (0, L, CAP_CTX, 1)]
        for (lo, hi, cap, col) in segs:
            for rnd in range(cap // 8):
                sch.op("dve", lambda e, i=work[0:E, lo:hi]: e.max(out=m8, in_=i), r=[k_tk], w=[k_tk])
                if rnd < cap // 8 - 1:
                    sch.op("dve", lambda e, o=work[0:E, lo:hi]:
                           e.match_replace(out=o, in_to_replace=m8, in_values=o, imm_value=-1.0), r=[k_tk], w=[k_tk])
            sch.op("dve", lambda e, o=thr[0:E, col:col + 1]: e.tensor_copy(out=o, in_=m8[:, 7:8]), r=[k_tk], w=[k_tk])
        for (lo, hi, cap, col) in segs:
            sch.op("dve", lambda e, o=work[0:E, lo:hi], i=affT[0:E, lo:hi], s_=thr[0:E, col:col + 1]:
                   e.tensor_scalar(out=o, in0=i, scalar1=s_, scalar2=None, op0=ALU.is_ge), r=[k_tk, k_affT], w=[k_tk])
            sch.op("dve", lambda e, o=cs[0:E, lo:hi], i=work[0:E, lo:hi]:
                   e.tensor_tensor_scan(out=o, data0=i, data1=i, initial=0.0, op0=ALU.add, op1=ALU.max),
                   r=[k_tk], w=[k_tk])
        lo0 = L if last else 0
        sch.op("dve", lambda e, o=cs[0:E, lo0:T], a=cs[0:E, lo0:T], b_=work[0:E, lo0:T]:
               e.tensor_tensor(out=o, in0=a, in1=b_, op=ALU.mult), r=[k_tk], w=[k_tk])
        sch.op("dve", lambda e, o=cs[0:E, lo0:T], i=cs[0:E, lo0:T]:
               e.tensor_scalar(out=o, in0=i, scalar1=-1.0, scalar2=None, op0=ALU.add), r=[k_tk], w=[k_tk])
        t_first = 2 if last else 0
        for t in range(t_first, NT):
            sch.op("pe", lambda e, o=bank(4)[:, t * E:(t + 1) * E], i=cs[0:E, t * 128:(t + 1) * 128]:
                   e.transpose(out=o, in_=i, identity=ident_f[0:E, 0:E]), r=[k_tk, k_cst], w=[ptrk[4]])
        sch.op("act", lambda e, o=posm_tm.rearrange("p a b -> p (a b)")[:, t_first * E:NT * E],
               i=bank(4)[:, t_first * E:NT * E]: e.copy(out=o, in_=i), r=[ptrk[4]], w=[k_tk])

        barrier()
        k_sel = Trk()
        k_selw = [Trk(), Trk()]
        k_swst = Trk()
        k_swsc = Trk()
        k_xeT = Trk()
        k_wgu = [Trk(), Trk()]
        k_hid1 = Trk()
        k_hid = [k_hid1, k_hid1]
        sgf = [reg2[:, 0:288], reg2[:, 512:800]]
        k_sgf = [Trk(), Trk()]
        nj = 256 if last else 288
        wctr2 = 0
        for ex in range(E):
            for t in range(16):
                sch.op("dve", lambda e, o=sel[:, t, :], s_=posm_tm[:, 2 + t, ex:ex + 1]:
                       e.tensor_scalar(out=o, in0=iota_f[:, 0:256], scalar1=s_, scalar2=None, op0=ALU.is_equal),
                       r=[k_tk, k_cst], w=[k_sel])
            if not last:
                for t in range(2):
                    sch.op("dve", lambda e, o=selc[:, t, :], s_=posm_tm[:, t, ex:ex + 1]:
                           e.tensor_scalar(out=o, in0=iota_f[:, 0:32], scalar1=s_, scalar2=None, op0=ALU.is_equal),
                           r=[k_tk, k_cst], w=[k_sel])
            for t in range(16):
                s_w = t % 2
                sch.op("dve", lambda e, o=selw[s_w], s_=posm_tm[:, 2 + t, ex:ex + 1], a_=aff_tm[:, 2 + t, ex:ex + 1]:
                       e.tensor_scalar(out=o, in0=iota_f[:, 0:256], scalar1=s_, scalar2=a_, op0=ALU.is_equal,
                                       op1=ALU.mult), r=[k_tk, k_aff, k_cst], w=[k_selw[s_w]])
                pb_ = 6 + (t // 2) % 2
                for jc in range(2):
                    sch.op("pe", lambda e, o=bank_bf(pb_)[:, jc * 256 + (t % 2) * 128:jc * 256 + (t % 2) * 128 + 128],
                           i=selw[s_w][:, jc * 128:(jc + 1) * 128]: e.transpose(out=o, in_=i, identity=ident_bf),
                           r=[k_selw[s_w], k_cst], w=[ptrk[pb_]])
                if t % 2 == 1:
                    t0 = t - 1
                    sch.op("dve", lambda e, o=swst[:, :, t0 * 128:(t0 + 2) * 128],
                           i=bank_bf(pb_)[:, 0:512].rearrange("p (c t) -> p c t", c=2): e.tensor_copy(out=o, in_=i),
                           r=[ptrk[pb_]], w=[k_swst])
            for jc in range(2):
                ld("sp", SWTL.rearrange("t j e c k -> j e c t k")[:, ex, jc],
                   swst[:, jc, :].rearrange("j (t k) -> j t k", k=128), r=[k_swst], w=[k_scr["SWTL"]])
            if not last:
                for t in range(2):
                    s_w = t % 2
                    sch.op("dve", lambda e, o=selw[s_w][:, 0:32], s_=posm_tm[:, t, ex:ex + 1],
                           a_=aff_tm[:, t, ex:ex + 1]:
                           e.tensor_scalar(out=o, in0=iota_f[:, 0:32], scalar1=s_, scalar2=a_, op0=ALU.is_equal,
                                           op1=ALU.mult), r=[k_tk, k_aff, k_cst], w=[k_selw[s_w]])
                    sch.op("pe", lambda e, o=bank_bf(5)[0:32, t * 128:(t + 1) * 128], i=selw[s_w][:, 0:32]:
                           e.transpose(out=o, in_=i, identity=ident_bf), r=[k_selw[s_w], k_cst], w=[ptrk[5]])
                sch.op("act", lambda e, o=swsc[0:32, 0:256], i=bank_bf(5)[0:32, 0:256]: e.copy(out=o, in_=i),
                       r=[ptrk[5]], w=[k_swsc])
                ld("sp", SWTC[ex], swsc[0:32, 0:256], r=[k_swsc], w=[k_scr["SWTC"]])
            for dch in range(KD):
                pb_ = dch % 2
                for t in range(16):
                    sch.op("pe", lambda e, o=bank(pb_)[:, 0:256], l=xn2[:, 2 + t, dch * 128:(dch + 1) * 128],
                           r_=sel[:, t, :], st=(t == 0), sp_=(t == 15): e.matmul(o, l, r_, start=st, stop=sp_),
                           r=[k_xn2[2 + t], k_sel], w=[ptrk[pb_]])
                if not last:
                    for t in range(2):
                        sch.op("pe", lambda e, o=bank(pb_)[:, 256:288], l=xn2[:, t, dch * 128:(dch + 1) * 128],
                               r_=selc[:, t, :], st=(t == 0), sp_=(t == 1): e.matmul(o, l, r_, start=st, stop=sp_),
                               r=[k_xn2[t], k_sel], w=[ptrk[pb_]])
                sch.op("act", lambda e, o=xeT[:, dch, 0:256], i=bank(pb_)[:, 0:256], sc_=modcol(2, dch, 0),
                       bi_=modcol(3, dch, 0): e.activation(out=o, in_=i, func=AF.Identity, scale=sc_, bias=bi_),
                       r=[ptrk[pb_], k_mod], w=[k_xeT])
                if not last:
                    sch.op("act", lambda e, o=xeT[:, dch, 256:288], i=bank(pb_)[:, 256:288], sc_=modcol(2, dch, 1),
                           bi_=modcol(3, dch, 1): e.activation(out=o, in_=i, func=AF.Identity, scale=sc_, bias=bi_),
                           r=[ptrk[pb_], k_mod], w=[k_xeT])
            hb = ex % 2
            for fp in range(4):
                wb2 = wctr2 % 2
                wctr2 += 1
                ld("pool", wgu[wb2][0], w_eg[li, ex].rearrange("(k p) f -> p k f", p=128)[:, :, fp * 256:(fp + 1) * 256],
                   w=[k_wgu[wb2]])
                ld("pool", wgu[wb2][1], w_eu[li, ex].rearrange("(k p) f -> p k f", p=128)[:, :, fp * 256:(fp + 1) * 256],
                   w=[k_wgu[wb2]])
                for f2 in range(2):
                    fc = fp * 2 + f2
                    s3 = fc % 2
                    pg, pu = 2 + 2 * s3, 3 + 2 * s3
                    for kk, pb_ in ((0, pg), (1, pu)):
                        for k in range(KD):
                            sch.op("pe", lambda e, o=bank(pb_)[:, 0:nj], l=wgu[wb2][kk][:, k, f2 * 128:(f2 + 1) * 128],
                                   r_=xeT[:, k, 0:nj], st=(k == 0), sp_=(k == KD - 1):
                                   e.matmul(o, l, r_, start=st, stop=sp_),
                                   r=[k_wgu[wb2], k_xeT], w=[ptrk[pb_]])
                    sch.op("act", lambda e, o=sgf[s3][:, 0:nj], i=bank(pg)[:, 0:nj]:
                           e.activation(out=o, in_=i, func=AF.Silu), r=[ptrk[pg]], w=[k_sgf[s3]])
                    sch.op("dve", lambda e, o=hid[hb][:, fc, 0:nj], a=sgf[s3][:, 0:nj], b_=bank(pu)[:, 0:nj]:
                           e.tensor_tensor(out=o, in0=a, in1=b_, op=ALU.mult),
                           r=[k_sgf[s3], ptrk[pu]], w=[k_hid[hb]])
            ld("sp", HID[ex][:, :, 0:nj], hid[hb][:, :, 0:nj], r=[k_hid[hb]], w=[k_scr["HID"]])

        new_phase()
        ye = ABF.get(E * 3 * 512, (E, 3, 512))
        k_ye = Trk()
        hidb = [ABF.get(8 * 288, (8, 288)) for _ in range(2)]
        k_hidb = [Trk(), Trk()]
        wd = [ABF.get(8 * 512, (8, 512)) for _ in range(2)]
        k_wd = [Trk(), Trk()]
        swl = [ABF.get(E * 256, (E, 2, 128)) for _ in range(2)]
        k_swl = [Trk(), Trk()]
        swc = [ABF.get(E * 128, (E, 128)) for _ in range(2)]
        k_swc = [Trk(), Trk()]
        xpf = [AFF.get(512) for _ in range(2)]
        k_xpf = [Trk(), Trk()]
        tmpf = [AFF.get(512) for _ in range(2)]
        k_tmpf = [Trk(), Trk()]
        grep2 = AFF.get(2 * D, (2, D))
        k_gr2 = Trk()
        for kind in range(2):
            ld("sp", grep2[:, kind, :], GREP[1, kind], r=[k_scr["GREP"]], w=[k_gr2])
        SWTCv = SWTC.rearrange("e j t -> j e t")
        jcs = ((0, 128), (1, 128)) if last else ((0, 128), (1, 128), (2, 32))
        yctr = 0
        xctr = 0
        for dblk in range(4):
            dsl = slice(dblk * 512, (dblk + 1) * 512)
            for ex in range(E):
                b = ex % 2
                ld("pool", hidb[b][:, :, 0:nj], HID[ex][:, :, 0:nj], r=[k_scr["HID"]], w=[k_hidb[b]])
                ld("pool", wd[b], w_ed[li, ex].rearrange("(k p) d -> p k d", p=128)[:, :, dsl], w=[k_wd[b]])
                for (jc, rows) in jcs:
                    pb_ = yctr % 4
                    yctr += 1
                    for k in range(8):
                        sch.op("pe", lambda e, o=bank(pb_)[0:rows, :], l=hidb[b][:, k, jc * 128:jc * 128 + rows],
                               r_=wd[b][:, k, :], st=(k == 0), sp_=(k == 7): e.matmul(o, l, r_, start=st, stop=sp_),
                               r=[k_hidb[b], k_wd[b]], w=[ptrk[pb_]])
                    sch.op("act", lambda e, o=ye[0:rows, ex, jc, :], i=bank(pb_)[0:rows, :]: e.copy(out=o, in_=i),
                           r=[ptrk[pb_]], w=[k_ye])
            for t in range(2 if last else 0, NT):
                b = xctr % 2
                pb_ = 4 + xctr % 4
                xctr += 1
                kind = kind_of(t)
                ld("pool", xpf[b], X[t * 128:(t + 1) * 128, dsl], r=[k_X[t]], w=[k_xpf[b]])
                if kind == 1:
                    ld("pool", swc[b][0:32], SWTCv[:, :, t * 128:(t + 1) * 128], r=[k_scr["SWTC"]], w=[k_swc[b]])
                    for ex in range(E):
                        sch.op("pe", lambda e, o=bank(pb_), l=swc[b][0:32, ex, :], r_=ye[0:32, ex, 2, :],
                               st=(ex == 0), sp_=(ex == E - 1): e.matmul(o, l, r_, start=st, stop=sp_),
                               r=[k_swc[b], k_ye], w=[ptrk[pb_]])
                else:
                    ld("pool", swl[b], SWTL[t - 2], r=[k_scr["SWTL"]], w=[k_swl[b]])
                    for ex in range(E):
                        for jc in range(2):
                            sch.op("pe", lambda e, o=bank(pb_), l=swl[b][:, ex, jc, :], r_=ye[:, ex, jc, :],
                                   st=(ex == 0 and jc == 0), sp_=(ex == E - 1 and jc == 1):
                                   e.matmul(o, l, r_, start=st, stop=sp_), r=[k_swl[b], k_ye], w=[ptrk[pb_]])
                sch.op("dve", lambda e, o=tmpf[b], a=bank(pb_), g_=grep2[:, kind, dsl]:
                       e.tensor_tensor(out=o, in0=a, in1=g_, op=ALU.mult), r=[ptrk[pb_], k_gr2], w=[k_tmpf[b]])
                sch.op("dve", lambda e, o=xpf[b], a=xpf[b], t_=tmpf[b]: e.tensor_tensor(out=o, in0=a, in1=t_, op=ALU.add),
                       r=[k_tmpf[b]], w=[k_xpf[b]])
                ld("sp", X[t * 128:(t + 1) * 128, dsl], xpf[b], r=[k_xpf[b]], w=[k_X[t]])
        if dbg.get("stopF"):
            break

    barrier()
    k_out = Trk()
    for j in range(4):
        ld("sp", out[j * 512:(j + 1) * 512, :], X[L + j * 512:L + (j + 1) * 512, :], w=[k_out])
    barrier()
    with nc.Block() as block:
        @block.tensor
        def _(e):
            sch.replay("pe", e)

        @block.scalar
        def _(e):
            sch.replay("act", e)

        @block.vector
        def _(e):
            sch.replay("dve", e)

        @block.gpsimd
        def _(e):
            sch.replay("pool", e)

        @block.sync
        def _(e):
            sch.replay("sp", e)
    es.close()
    return nc


def host_inputs(inp, b, nlw=DEPTH, ne=E):
    f = np.float32
    m = {}
    m["x"] = np.ascontiguousarray(inp["x"][b])
    m["ctx"] = np.ascontiguousarray(inp["ctx"][b])
    cT = np.stack([inp["c"][b].reshape(KD, 128).T, inp["c_ctx"].reshape(KD, 128).T], axis=2)
    m["cT"] = np.ascontiguousarray(cT.reshape(128, KD * 2)).astype(f)
    m["w_mod"] = inp["w_mod"][:nlw]
    m["b_mod"] = inp["b_mod"][:nlw]
    m["b_modT"] = np.ascontiguousarray(inp["b_mod"][:nlw].reshape(nlw, 96, 128).transpose(0, 2, 1))
    m["norm1T"] = np.ascontiguousarray(inp["norm1"][:nlw].reshape(nlw, KD, 128).transpose(0, 2, 1))
    m["norm2T"] = np.ascontiguousarray(inp["norm2"][:nlw].reshape(nlw, KD, 128).transpose(0, 2, 1))
    m["w_in"] = inp["w_in"][:nlw]
    idx = _na_uniq_idx()
    rb = inp["na_rel_bias"][:nlw].reshape(nlw, 8, 15 * 31)
    rbp = np.concatenate([rb, np.full((nlw, 8, 1), NEG, f)], axis=2)
    m["na_bias"] = np.ascontiguousarray(rbp[:, :, idx])
    for nm in ("na_q_norm", "na_k_norm", "mla_q_a_norm", "mla_kv_a_norm", "mla_q_norm", "mla_k_norm",
               "gqa_q_norm", "gqa_k_norm"):
        m[nm] = inp[nm][:nlw]
    m["mla_w_q_b"] = inp["mla_w_q_b"][:nlw]
    m["mla_w_kv_b"] = inp["mla_w_kv_b"][:nlw]
    m["w_branch_a"] = inp["w_branch_a"][:nlw]
    m["w_branch_b"] = inp["w_branch_b"][:nlw]
    m["w_branch_c"] = inp["w_branch_c"][:nlw]
    m["w_out"] = inp["w_out"][:nlw]
    m["w_router"] = inp["w_router"][:nlw]
    m["w_expert_gate"] = inp["w_expert_gate"][:nlw, :ne]
    m["w_expert_up"] = inp["w_expert_up"][:nlw, :ne]
    m["w_expert_down"] = inp["w_expert_down"][:nlw, :ne]
    c64, s64 = _rope_tables(64)
    c32, s32 = _rope_tables(32)
    m["ropeC64"], m["ropeS64"], m["ropeC32"], m["ropeS32"] = c64, s64, c32, s32
    m["ident"] = np.eye(128, dtype=f)
    m["iota"] = np.ascontiguousarray(np.broadcast_to(np.arange(256, dtype=f), (128, 256)))
    sel = np.zeros((128, 128), f)
    sel[64, 0:64] = 1.0
    m["sel64"] = sel
    return {k: np.ascontiguousarray(v, dtype=f) for k, v in m.items()}


_NC_CACHE = {}


def kernel(**inputs):
    inp = {k: np.asarray(v) for k, v in inputs.items()}
    if "nc" not in _NC_CACHE:
        _NC_CACHE["nc"] = build()
    nc = _NC_CACHE["nc"]
    in_maps = [host_inputs(inp, b) for b in range(NCORES)]
    res = run_bass_kernel_spmd(nc, in_maps, core_ids=list(range(NCORES)))
    out = np.stack([np.asarray(res.results[b]["out"]) for b in range(NCORES)], axis=0)
    return out.astype(np.float32, copy=False)
```

```python
import numpy as np
from contextlib import ExitStack
import concourse.bass as bass
import concourse.mybir as mybir
from concourse.bass_utils import run_bass_kernel_spmd

F32 = mybir.dt.float32
BF16 = mybir.dt.bfloat16
AF = mybir.ActivationFunctionType
ALU = mybir.AluOpType
AX = mybir.AxisListType

D = 2048
KD = 16
L = 256
S = 2048
T = L + S
NT = T // 128
DEPTH = 4
GRID_W = 64
INW = 9248
E = 16
FF = 1024
CAP_LAT = 256
CAP_CTX = 32
EPS = 1e-6
NCORES = 4
NEG = -30000.0


class Trk:
    __slots__ = ("w", "r")

    def __init__(self):
        self.w = {}
        self.r = {}


ENGS = ("pe", "act", "dve", "pool", "sp")
NRING = 12
EPOCH = 30000


class Sch:
    def __init__(self, nc, es):
        self.nc = nc
        self.es = es
        self.sems = []
        self.owner = []
        self.stream = {e: [] for e in ENGS}
        self.cnt = {e: 0 for e in ENGS}
        self.csem = {e: None for e in ENGS}
        self.waited = {e: {} for e in ENGS}
        self.ring = {}
        self.dman = {}
        self.dtok = {}
        for q in ("sp", "pool", "act"):
            self.ring[q] = [self._newsem("d%s%d" % (q, i), "dma") for i in range(NRING)]
            self.dman[q] = 0
            self.dtok[q] = [None] * NRING

    def _newsem(self, name, owner):
        h = self.es.enter_context(self.nc.semaphore(name))
        self.sems.append(h)
        self.owner.append(owner)
        return len(self.sems) - 1

    def _need(self, e, si, v, waits):
        if e == "pe" and self.owner[si] == "pe":
            return
        if self.waited[e].get(si, 0) >= v:
            return
        if waits.get(si, 0) < v:
            waits[si] = v

    def _deps(self, e, r, w):
        waits = {}
        for t in r:
            for si, v in t.w.items():
                self._need(e, si, v, waits)
        for t in w:
            for si, v in t.w.items():
                self._need(e, si, v, waits)
            for si, v in t.r.items():
                self._need(e, si, v, waits)
        return waits

    def _commit(self, e, waits, tok, r, w):
        for si, v in waits.items():
            self.waited[e][si] = v
        si, v = tok
        for t in r:
            if t.r.get(si, 0) < v:
                t.r[si] = v
        for t in w:
            t.w = {si: v}
            t.r = {}

    def op(self, e, fn, r=(), w=()):
        waits = self._deps(e, r, w)
        if self.csem[e] is None or self.cnt[e] >= EPOCH:
            self.csem[e] = self._newsem("c%s%d" % (e, len(self.sems)), e)
            self.cnt[e] = 0
        self.cnt[e] += 1
        tok = (self.csem[e], self.cnt[e])
        self.stream[e].append((list(waits.items()), fn, tok, 1))
        self._commit(e, waits, tok, r, w)
        return tok

    def dma(self, q, fn, r=(), w=()):
        waits = self._deps(q, r, w)
        n = self.dman[q]
        slot = n % NRING
        prev = self.dtok[q][slot]
        if prev is not None:
            self._need(q, prev[0], prev[1], waits)
        tok = (self.ring[q][slot], 16 * (n // NRING + 1))
        self.dman[q] = n + 1
        self.dtok[q][slot] = tok
        self.stream[q].append((list(waits.items()), fn, tok, 16))
        self._commit(q, waits, tok, r, w)
        return tok

    def wait_all(self, e, trks):
        waits = {}
        for t in trks:
            for si, v in t.w.items():
                self._need(e, si, v, waits)
        for si, v in waits.items():
            self.waited[e][si] = v
        self.stream[e].append((list(waits.items()), None, None, 0))

    def replay(self, e, eng):
        for waits, fn, tok, inc in self.stream[e]:
            for si, v in waits:
                eng.wait_ge(self.sems[si], v)
            if fn is not None:
                fn(eng).then_inc(self.sems[tok[0]], inc)


def _rope_tables(rot_dim):
    t = np.arange(S)
    row = (t // GRID_W).astype(np.float32)
    col = (t % GRID_W).astype(np.float32)
    nf = rot_dim // 4
    inv = (10000.0 ** (-np.arange(nf, dtype=np.float32) / nf)).astype(np.float32)
    ar = row[:, None] * inv
    ac = col[:, None] * inv
    C = np.concatenate([np.cos(ar), np.cos(ar), np.cos(ac), np.cos(ac)], axis=1)
    Sg = np.concatenate([-np.sin(ar), np.sin(ar), -np.sin(ac), np.sin(ac)], axis=1)
    return C.astype(np.float32), Sg.astype(np.float32)


def _na_plan():
    plan = []
    for qb in range(4):
        rows = range(8 * qb, 8 * qb + 8)
        rs = [min(max(r - 4, 0), 24) for r in rows]
        lo, hi = min(rs), max(rs) + 7
        plan.append(list(range(lo // 2, hi // 2 + 1)))
    return plan


NA_PLAN = _na_plan()
NA_NTILES = sum(len(p) for p in NA_PLAN)


_NA_IDX = None


def _na_idx():
    global _NA_IDX
    if _NA_IDX is None:
        kk = np.arange(128)[:, None]
        qq = np.arange(512)[None, :]
        idx = np.full((NA_NTILES, 128, 512), -1, np.int64)
        ti = 0
        for qb in range(4):
            for kc in NA_PLAN[qb]:
                kt = kc * 128 + kk
                kr, kcol = kt // 64, kt % 64
                qt = qb * 512 + qq
                r, c = qt // 64, qt % 64
                rs = np.clip(r - 4, 0, 24)
                cs = np.clip(c - 8, 0, 48)
                ok = (kr >= rs) & (kr < rs + 8) & (kcol >= cs) & (kcol < cs + 16)
                v = (kr - r + 7) * 31 + (kcol - c + 15)
                idx[ti] = np.where(ok, v, -1)
                ti += 1
        _NA_IDX = idx
    return _NA_IDX


NA_TILE_ID = []
_uid = 0
for _qb in range(4):
    if _qb == 2:
        NA_TILE_ID.append(list(NA_TILE_ID[1]))
        continue
    NA_TILE_ID.append(list(range(_uid, _uid + len(NA_PLAN[_qb]))))
    _uid += len(NA_PLAN[_qb])
NA_NUNIQ = _uid


def _na_uniq_idx():
    idx = _na_idx()
    out = np.zeros((NA_NUNIQ, 128, 512), np.int64)
    ti = 0
    for qb in range(4):
        for j in range(len(NA_PLAN[qb])):
            out[NA_TILE_ID[qb][j]] = idx[ti]
            ti += 1
    return out


class Arena:
    def __init__(self, t, n):
        self.t = t
        self.n = n
        self.off = 0

    def reset(self):
        self.off = 0

    def get(self, n, shape=None):
        n_al = (n + 15) // 16 * 16
        assert self.off + n_al <= self.n, ("arena overflow", self.off, n_al, self.n)
        v = self.t[:, self.off:self.off + n]
        self.off += n_al
        if shape is not None and len(shape) == 2:
            v = v.rearrange("p (a b) -> p a b", a=shape[0])
        elif shape is not None and len(shape) == 3:
            v = v.rearrange("p (a b c) -> p a b c", a=shape[0], b=shape[1])
        return v


def build(nl=DEPTH, dbg=None, nlw=DEPTH, ne=E):
    dbg = dbg or {}
    nc = bass.Bass("TRN2", target_bir_lowering=False)

    def din(name, shape, dt=F32):
        return nc.dram_tensor(name, list(shape), dt, kind="ExternalInput").ap()

    def dscr(name, shape, dt):
        kind = "ExternalOutput" if dbg.get(name) else "Internal"
        return nc.dram_tensor(name, list(shape), dt, kind=kind).ap()

    x_in = din("x", [S, D])
    ctx_in = din("ctx", [L, D])
    cT_in = din("cT", [128, KD * 2])
    w_mod = din("w_mod", [nlw, D, 6 * D])
    b_mod = din("b_mod", [nlw, 6 * D])
    b_modT = din("b_modT", [nlw, 128, 96])
    norm1T = din("norm1T", [nlw, 128, KD])
    norm2T = din("norm2T", [nlw, 128, KD])
    w_in = din("w_in", [nlw, D, INW])
    na_bias = din("na_bias", [nlw, 8, NA_NUNIQ, 128, 512])
    gains = {}
    for nm, n in (("na_q_norm", 64), ("na_k_norm", 64), ("mla_q_a_norm", 512), ("mla_kv_a_norm", 256),
                  ("mla_q_norm", 96), ("mla_k_norm", 96), ("gqa_q_norm", 64), ("gqa_k_norm", 64)):
        gains[nm] = din(nm, [nlw, n])
    w_q_b = din("mla_w_q_b", [nlw, 512, 768])
    w_kv_b = din("mla_w_kv_b", [nlw, 256, 1024])
    w_br = [din("w_branch_a", [nlw, 512, D]), din("w_branch_b", [nlw, 512, D]), din("w_branch_c", [nlw, 512, D])]
    w_out = din("w_out", [nlw, D, D])
    w_router = din("w_router", [nlw, D, E])
    w_eg = din("w_expert_gate", [nlw, ne, D, FF])
    w_eu = din("w_expert_up", [nlw, ne, D, FF])
    w_ed = din("w_expert_down", [nlw, ne, FF, D])
    rc64 = din("ropeC64", [S, 64])
    rs64 = din("ropeS64", [S, 64])
    rc32 = din("ropeC32", [S, 32])
    rs32 = din("ropeS32", [S, 32])
    ident_in = din("ident", [128, 128])
    iota_in = din("iota", [128, 256])
    sel64_in = din("sel64", [128, 128])
    out = nc.dram_tensor("out", [S, D], F32, kind="ExternalOutput").ap()

    X = dscr("X", [T, D], F32)
    QaT = dscr("QaT", [512, T], BF16)
    KaT = dscr("KaT", [512, T], BF16)
    QcT = dscr("QcT", [512, T], BF16)
    KcT = dscr("KcT", [128, T], BF16)
    QbT = dscr("QbT", [8, 96, T], BF16)
    KbT = dscr("KbT", [8, 96, T], BF16)
    Va = dscr("Va", [T, 512], BF16)
    Vb = dscr("Vb", [T, 512], BF16)
    Vc = dscr("Vc", [T, 128], BF16)
    GT = dscr("GT", [3 * D, T], BF16)
    OT = dscr("OT", [3, 512, T], BF16)
    YT = dscr("YT", [D, T], BF16)
    GREP = dscr("GREP", [2, 2, 128, D], F32)
    HID = dscr("HID", [E, 128, 8, 288], BF16)
    SWTL = dscr("SWTL", [16, 128, E, 2, 128], BF16)
    SWTC = dscr("SWTC", [E, 32, L], BF16)

    es = ExitStack()
    abf_t = es.enter_context(nc.sbuf_tensor("abf", [128, 68 * 1024], BF16))
    af_t = es.enter_context(nc.sbuf_tensor("af", [128, 11 * 1024], F32))
    cst_t = es.enter_context(nc.sbuf_tensor("cst", [128, 4 * 1024], F32))
    pps = [es.enter_context(nc.psum_tensor("pp%d" % i, [128, 1024], F32)) for i in range(4)]
    sch = Sch(nc, es)
    ABF = Arena(abf_t, 68 * 1024)
    AFF = Arena(af_t, 11 * 1024)
    CST = Arena(cst_t, 4 * 1024)

    def bank(b):
        return pps[b // 2][:, (b % 2) * 512:(b % 2 + 1) * 512]

    def bank_bf(b):
        return bank(b).bitcast(BF16)

    ptrk = [Trk() for _ in range(8)]

    def barrier():
        toks = []
        for f in ENGS:
            if sch.csem[f] is not None:
                toks.append((sch.csem[f], sch.cnt[f]))
        for q in ("sp", "pool", "act"):
            for tk in sch.dtok[q]:
                if tk is not None:
                    toks.append(tk)
        for e in ENGS:
            waits = {}
            for si, v in toks:
                sch._need(e, si, v, waits)
            for si, v in waits.items():
                sch.waited[e][si] = v
            if waits:
                sch.stream[e].append((list(waits.items()), None, None, 0))

    def new_phase():
        barrier()
        ABF.reset()
        AFF.reset()

    ident_f = CST.get(128)
    iota_f = CST.get(256)
    sel64_f = CST.get(128)
    ident_bf_t = es.enter_context(nc.sbuf_tensor("identbf", [128, 128], BF16))
    ident_bf = ident_bf_t[:]
    rC64 = CST.get(16 * 64, (16, 64))
    rS64 = CST.get(16 * 64, (16, 64))
    rC32 = CST.get(16 * 32, (16, 32))
    rS32 = CST.get(16 * 32, (16, 32))
    scT_f = CST.get(32)
    modc = CST.get(4 * 32, (4, 32))
    modT = CST.get(192, (96, 2))
    bmT = CST.get(96)
    n1T = CST.get(16)
    n2T = CST.get(16)
    cT_sb = CST.get(32)
    scT_bf_t = es.enter_context(nc.sbuf_tensor("scTbf", [128, 32], BF16))
    scRep_t = es.enter_context(nc.sbuf_tensor("scRep", [128, 32 * 128], BF16))
    scT_bf = scT_bf_t[:]
    scRep = scRep_t[:].rearrange("p (a b) -> p a b", a=32)
    k_cst = Trk()

    def ld(q, out_ap, in_ap, r=(), w=()):
        return sch.dma(q, lambda e, o=out_ap, i=in_ap: e.dma_start(out=o, in_=i), r=r, w=w)

    ld("sp", ident_f, ident_in, w=[k_cst])
    ld("sp", iota_f, iota_in, w=[k_cst])
    ld("sp", sel64_f, sel64_in, w=[k_cst])
    ld("sp", cT_sb, cT_in, w=[k_cst])
    for (dst, src, n) in ((rC64, rc64, 64), (rS64, rs64, 64), (rC32, rc32, 32), (rS32, rs32, 32)):
        ld("sp", dst, src.rearrange("(t p) d -> p t d", p=128), w=[k_cst])
    sch.op("dve", lambda e: e.tensor_copy(out=ident_bf, in_=ident_f), r=[k_cst], w=[k_cst])
    sch.op("act", lambda e: e.activation(out=scT_f, in_=cT_sb, func=AF.Silu), r=[k_cst], w=[k_cst])
    sch.op("dve", lambda e: e.tensor_copy(out=scT_bf, in_=scT_f), r=[k_cst], w=[k_cst])
    sch.op("dve", lambda e: e.tensor_copy(out=scRep, in_=scT_f.unsqueeze(2).to_broadcast([128, 32, 128])),
           r=[k_cst], w=[k_cst])

    k_X = [Trk() for _ in range(NT)]
    ld("sp", X[0:L, :], ctx_in, w=k_X[0:2])
    for j in range(4):
        ld("sp", X[L + j * 512:L + (j + 1) * 512, :], x_in[j * 512:(j + 1) * 512, :], w=k_X[2 + 4 * j:6 + 4 * j])

    k_scr = {n: Trk() for n in ("QaT", "KaT", "QcT", "KcT", "QbT", "KbT", "Va", "Vb", "Vc", "GT", "OT", "YT",
                                "GREP", "HID", "SWTL", "SWTC")}

    def kind_of(t):
        return 1 if t < 2 else 0

    for li in range(nl):
        last = (li == DEPTH - 1) or bool(dbg.get("force_last"))

        new_phase()
        k_mod = Trk()
        ld("sp", bmT, b_modT[li], w=[k_mod])
        ld("sp", n1T, norm1T[li], w=[k_mod])
        ld("sp", n2T, norm2T[li], w=[k_mod])
        wm = [ABF.get(16 * 512, (16, 512)) for _ in range(2)]
        k_wm = [Trk(), Trk()]
        bmr = [AFF.get(512) for _ in range(2)]
        k_bmr = [Trk(), Trk()]
        grs = [AFF.get(512) for _ in range(2)]
        k_grs = [Trk(), Trk()]
        wmv = w_mod[li].rearrange("(k p) c -> p k c", p=128)
        psA = bank(0)[:, 0:192].rearrange("p (a b) -> p a b", a=96)
        ngr = 0
        for j in range(24):
            b = j % 2
            ld("pool", wm[b], wmv[:, :, j * 512:(j + 1) * 512], w=[k_wm[b]])
            for q in range(4):
                cc = j * 4 + q
                for k in range(KD):
                    sch.op("pe", lambda e, o=psA[:, cc, :], l=wm[b][:, k, q * 128:(q + 1) * 128],
                           r_=scT_bf[:, 2 * k:2 * k + 2], st=(k == 0), sp_=(k == KD - 1):
                           e.matmul(o, l, r_, start=st, stop=sp_),
                           r=[k_wm[b], k_cst], w=[ptrk[0]])
            which = {8: 0, 9: 0, 10: 0, 11: 0, 20: 1, 21: 1, 22: 1, 23: 1}.get(j)
            if which is not None:
                cb = (j - 8) if which == 0 else (j - 20)
                g0 = 2 * D if which == 0 else 5 * D
                for kind in range(2):
                    pb_ = 2 + (ngr % 4)
                    for k in range(KD):
                        sch.op("pe", lambda e, o=bank(pb_), l=scRep[:, 2 * k + kind, :], r_=wm[b][:, k, :],
                               st=(k == 0), sp_=(k == KD - 1): e.matmul(o, l, r_, start=st, stop=sp_),
                               r=[k_wm[b], k_cst], w=[ptrk[pb_]])
                    sb = ngr % 2
                    ld("sp", bmr[sb], b_mod[li, g0 + cb * 512:g0 + (cb + 1) * 512].partition_broadcast(128),
                       w=[k_bmr[sb]])
                    sch.op("dve", lambda e, o=grs[sb], a=bank(pb_), b_=bmr[sb]:
                           e.tensor_tensor(out=o, in0=a, in1=b_, op=ALU.add),
                           r=[ptrk[pb_], k_bmr[sb]], w=[k_grs[sb]])
                    ld("sp", GREP[which, kind, :, cb * 512:(cb + 1) * 512], grs[sb], r=[k_grs[sb]],
                       w=[k_scr["GREP"]])
                    ngr += 1
        sch.op("dve", lambda e: e.tensor_tensor(out=modT, in0=psA,
                                                in1=bmT.unsqueeze(2).to_broadcast([128, 96, 2]), op=ALU.add),
               r=[ptrk[0], k_mod], w=[k_mod])
        mc = modc.rearrange("p a (k c) -> p a k c", c=2)
        for (dst, sc_lo, sh_lo, nT) in ((0, 16, 0, n1T), (2, 64, 48, n2T)):
            sch.op("dve", lambda e, o=mc[:, dst], a=modT[:, sc_lo:sc_lo + 16, :], n_=nT:
                   e.scalar_tensor_tensor(out=o, in0=a, scalar=1.0,
                                          in1=n_.unsqueeze(2).to_broadcast([128, 16, 2]),
                                          op0=ALU.add, op1=ALU.mult),
                   r=[k_mod], w=[k_mod])
            sch.op("dve", lambda e, o=mc[:, dst + 1], a=modT[:, sh_lo:sh_lo + 16, :]:
                   e.tensor_copy(out=o, in_=a), r=[k_mod], w=[k_mod])

        def modcol(j, k, kind):
            return modc[:, j, 2 * k + kind:2 * k + kind + 1]

        new_phase()
        hT = ABF.get(KD * T, (KD, T))
        k_hT = [Trk() for _ in range(NT)]
        xb = [AFF.get(D) for _ in range(2)]
        k_xb = [Trk(), Trk()]
        scr6k = ABF.get(3 * D)
        junk = scr6k[:, 0:D]
        k_junk = Trk()
        xn = [scr6k[:, D:2 * D], scr6k[:, 2 * D:3 * D]]
        k_xn = [Trk(), Trk()]
        st_f = AFF.get(4 * NT, (4, NT))
        k_st = Trk()

        def norm_tile(t, xbuf, kx, xnbuf, kxn):
            sch.op("act", lambda e, o=junk, i=xbuf, a=st_f[:, 0, t:t + 1]:
                   e.activation(out=o, in_=i, func=AF.Square, accum_out=a), r=[kx], w=[k_junk, k_st])
            sch.op("act", lambda e, o=st_f[:, 1, t:t + 1], i=st_f[:, 0, t:t + 1]:
                   e.activation(out=o, in_=i, func=AF.Sqrt, scale=1.0 / D, bias=EPS), r=[k_st], w=[k_st])
            sch.op("dve", lambda e, o=st_f[:, 2, t:t + 1], i=st_f[:, 1, t:t + 1]: e.reciprocal(out=o, in_=i),
                   r=[k_st], w=[k_st])
            sch.op("dve", lambda e, o=xnbuf, i=xbuf, s_=st_f[:, 2, t:t + 1]:
                   e.tensor_scalar(out=o, in0=i, scalar1=s_, scalar2=None, op0=ALU.mult),
                   r=[kx, k_st], w=[kxn])

        def transpose_mod(t, xnbuf, kxn, dst_fn, kdst, gj):
            kind = kind_of(t)
            for g in range(4):
                pb_ = 4 + g
                for q in range(4):
                    k = g * 4 + q
                    sch.op("pe", lambda e, o=bank_bf(pb_)[:, q * 128:(q + 1) * 128],
                           i=xnbuf[:, k * 128:(k + 1) * 128]: e.transpose(out=o, in_=i, identity=ident_bf),
                           r=[kxn, k_cst], w=[ptrk[pb_]])
                for q in range(4):
                    k = g * 4 + q
                    sch.op("act", lambda e, o=dst_fn(k), i=bank_bf(pb_)[:, q * 128:(q + 1) * 128],
                           sc_=modcol(gj, k, kind), bi_=modcol(gj + 1, k, kind):
                           e.activation(out=o, in_=i, func=AF.Identity, scale=sc_, bias=bi_),
                           r=[ptrk[pb_], k_mod], w=[kdst])

        for t in range(NT):
            b = t % 2
            ld("pool", xb[b], X[t * 128:(t + 1) * 128, :], r=[k_X[t]], w=[k_xb[b]])
            norm_tile(t, xb[b], k_xb[b], xn[b], k_xn[b])
            transpose_mod(t, xn[b], k_xn[b], lambda k, t=t: hT[:, k, t * 128:(t + 1) * 128], k_hT[t], 0)
        if dbg.get("stopB"):
            break

        barrier()
        winv = w_in[li].rearrange("(k p) c -> p k c", p=128)
        gn = {}
        k_gn = Trk()
        for nm, n in (("na_q_norm", 64), ("na_k_norm", 64), ("mla_q_a_norm", 512), ("mla_kv_a_norm", 256),
                      ("mla_q_norm", 96), ("mla_k_norm", 96), ("gqa_q_norm", 64), ("gqa_k_norm", 64)):
            gn[nm] = AFF.get(n)
            ld("sp", gn[nm], gains[nm][li].partition_broadcast(128), w=[k_gn])
        sch.op("dve", lambda e, o=gn["na_q_norm"]: e.tensor_scalar(out=o, in0=o, scalar1=0.125, scalar2=None,
                                                                   op0=ALU.mult), r=[k_gn], w=[k_gn])
        wblk = [ABF.get(KD * 512, (KD, 512)) for _ in range(2)]
        k_wblk = [Trk(), Trk()]
        wqb = ABF.get(4 * 768, (4, 768))
        wkvb = ABF.get(2 * 1024, (2, 1024))
        k_w2 = Trk()
        ld("pool", wqb, w_q_b[li].rearrange("(k p) c -> p k c", p=128), w=[k_w2])
        ld("pool", wkvb, w_kv_b[li].rearrange("(k p) c -> p k c", p=128), w=[k_w2])
        sq = [AFF.get(1024) for _ in range(2)]
        k_sq = [Trk(), Trk()]
        o1 = [AFF.get(768) for _ in range(2)]
        k_o1 = [Trk(), Trk()]
        sm = AFF.get(64, (4, 16))
        k_sm = Trk()
        kpe_f = AFF.get(32)
        k_kpe = Trk()
        nb = [ABF.get(1024) for _ in range(2)]
        k_nb = [Trk(), Trk()]
        stg = [ABF.get(1024) for _ in range(2)]
        k_stg = [Trk(), Trk()]
        cqT = ABF.get(6 * 128, (6, 128))
        k_cqT = Trk()
        wctr = [0]
        uctr = [0]
        AFF_tmp = [AFF.get(768) for _ in range(2)]
        k_tmp = [Trk(), Trk()]

        def load_w(c0, n):
            b = wctr[0] % 2
            wctr[0] += 1
            ld("pool", wblk[b][:, :, 0:n], winv[:, :, c0:c0 + n], w=[k_wblk[b]])
            return wblk[b], k_wblk[b]

        def proj(t, wb, kwb, c0, n, pb_):
            for k in range(KD):
                sch.op("pe", lambda e, o=bank(pb_)[:, 0:n], l=hT[:, k, t * 128:(t + 1) * 128],
                       r_=wb[:, k, c0:c0 + n], st=(k == 0), sp_=(k == KD - 1):
                       e.matmul(o, l, r_, start=st, stop=sp_), r=[k_hT[t], kwb], w=[ptrk[pb_]])

        def normhead(src, ksrc, nh, hd, gain, dst, kdst):
            n = nh * hd
            u = uctr[0] % 2
            uctr[0] += 1
            sch.op("act", lambda e, o=sq[u][:, 0:n], i=src: e.activation(out=o, in_=i, func=AF.Square),
                   r=ksrc, w=[k_sq[u]])
            sch.op("dve", lambda e, o=sm[:, 0, 0:nh], i=sq[u][:, 0:n].rearrange("p (h d) -> p h d", h=nh):
                   e.tensor_reduce(out=o, in_=i, axis=AX.X, op=ALU.add), r=[k_sq[u]], w=[k_sm])
            sch.op("act", lambda e, o=sm[:, 1, 0:nh], i=sm[:, 0, 0:nh]:
                   e.activation(out=o, in_=i, func=AF.Sqrt, scale=1.0 / hd, bias=EPS), r=[k_sm], w=[k_sm])
            sch.op("dve", lambda e, o=sm[:, 2, 0:nh], i=sm[:, 1, 0:nh]: e.reciprocal(out=o, in_=i),
                   r=[k_sm], w=[k_sm])
            sch.op("dve", lambda e, o=sq[u][:, 0:n].rearrange("p (h d) -> p h d", h=nh),
                   i=src.rearrange("p (h d) -> p h d", h=nh),
                   s_=sm[:, 2, 0:nh].unsqueeze(2).to_broadcast([128, nh, hd]):
                   e.tensor_tensor(out=o, in0=i, in1=s_, op=ALU.mult), r=list(ksrc) + [k_sm, k_sq[u]], w=[k_sq[u]])
            sch.op("dve", lambda e, o=dst.rearrange("p (h d) -> p h d", h=nh),
                   i=sq[u][:, 0:n].rearrange("p (h d) -> p h d", h=nh),
                   g_=gain.unsqueeze(1).to_broadcast([128, nh, hd]):
                   e.tensor_tensor(out=o, in0=i, in1=g_, op=ALU.mult), r=[k_sq[u], k_gn], w=kdst)

        def rope(t, buf, kbuf, nh, hd, r0, R, tC, tS, dst, kdst):
            tt = t - 2
            m = R // 4
            u = uctr[0] % 2
            uctr[0] += 1
            bv = buf.rearrange("p (h d) -> p h d", h=nh)
            xs = sq[u][:, 0:nh * R].rearrange("p (h s a m) -> p h s a m", h=nh, s=2, a=2)
            xin = bv[:, :, r0:r0 + R].rearrange("p h (s a m) -> p h s a m", s=2, a=2)
            Sv = tS[:, tt, :].rearrange("p (s a m) -> p s a m", s=2, a=2)
            for a in range(2):
                for s_ in range(2):
                    sch.op("dve", lambda e, o=xs[:, :, s_, a, :], i=xin[:, :, s_, 1 - a, :],
                           g_=Sv[:, s_, a, :].unsqueeze(1).to_broadcast([128, nh, m]):
                           e.tensor_tensor(out=o, in0=i, in1=g_, op=ALU.mult),
                           r=[kbuf, k_cst, k_sq[u]], w=[k_sq[u]])
            t1 = o1[u][:, 0:nh * R].rearrange("p (h r) -> p h r", h=nh)
            sch.op("dve", lambda e, o=t1, i=bv[:, :, r0:r0 + R],
                   g_=tC[:, tt, :].unsqueeze(1).to_broadcast([128, nh, R]):
                   e.tensor_tensor(out=o, in0=i, in1=g_, op=ALU.mult), r=[kbuf, k_cst], w=[k_o1[u]])
            dv = dst.rearrange("p (h d) -> p h d", h=nh)
            sch.op("dve", lambda e, o=dv[:, :, r0:r0 + R], i=t1,
                   x_=sq[u][:, 0:nh * R].rearrange("p (h r) -> p h r", h=nh):
                   e.tensor_tensor(out=o, in0=i, in1=x_, op=ALU.add), r=[k_o1[u], k_sq[u]], w=kdst)
            if r0 > 0:
                sch.op("dve", lambda e, o=dv[:, :, 0:r0], i=bv[:, :, 0:r0]: e.tensor_copy(out=o, in_=i),
                       r=[kbuf], w=kdst)

        def transpose_out(t, src, ksrc, ncol_blocks, rows, dst_dram, kd):
            s = uctr[0] % 2
            uctr[0] += 1
            pb_ = 6 + s
            for q in range(ncol_blocks):
                sch.op("pe", lambda e, o=bank_bf(pb_)[0:rows, q * 128:(q + 1) * 128],
                       i=src[:, q * rows:(q + 1) * rows]: e.transpose(out=o, in_=i, identity=ident_bf),
                       r=[ksrc, k_cst], w=[ptrk[pb_]])
            n = ncol_blocks * 128
            sch.op("act", lambda e, o=stg[s][0:rows, 0:n], i=bank_bf(pb_)[0:rows, 0:n]: e.copy(out=o, in_=i),
                   r=[ptrk[pb_]], w=[k_stg[s]])
            ld("sp", dst_dram, stg[s][0:rows, 0:n].rearrange("p (q c) -> p q c", q=ncol_blocks),
               r=[k_stg[s]], w=[kd])

        def qk_group(c0, nh, gain, dst_dram_fn, kd, do_rope):
            wb, kwb = load_w(c0, nh * 64)
            for t in range(NT):
                pb_ = t % 4
                proj(t, wb, kwb, 0, nh * 64, pb_)
                u = t % 2
                if do_rope and t >= 2:
                    normhead(bank(pb_)[:, 0:nh * 64], [ptrk[pb_]], nh, 64, gain, AFF_tmp[u][:, 0:nh * 64],
                             [k_tmp[u]])
                    rope(t, AFF_tmp[u][:, 0:nh * 64], k_tmp[u], nh, 64, 0, 64, rC64, rS64,
                         nb[u][:, 0:nh * 64], [k_nb[u]])
                else:
                    normhead(bank(pb_)[:, 0:nh * 64], [ptrk[pb_]], nh, 64, gain, nb[u][:, 0:nh * 64], [k_nb[u]])
                nblk = max(1, nh * 64 // 128)
                transpose_out(t, nb[u], k_nb[u], nblk, 128 if nh >= 2 else 64, dst_dram_fn(t), kd)

        qk_group(0, 8, gn["na_q_norm"],
                 lambda t: QaT.rearrange("(q p) t -> p q t", p=128)[:, :, t * 128:(t + 1) * 128], k_scr["QaT"], False)
        qk_group(512, 8, gn["na_k_norm"],
                 lambda t: KaT.rearrange("(q p) t -> p q t", p=128)[:, :, t * 128:(t + 1) * 128], k_scr["KaT"], False)
        if dbg.get("stopC1"):
            break
        qk_group(2336, 8, gn["gqa_q_norm"],
                 lambda t: QcT.rearrange("(q p) t -> p q t", p=128)[:, :, t * 128:(t + 1) * 128], k_scr["QcT"], True)
        if dbg.get("stopC2"):
            break
        wb, kwb = load_w(2848, 256)
        for t in range(NT):
            pb_ = t % 4
            u = t % 2
            proj(t, wb, kwb, 0, 256, pb_)
            if t >= 2:
                normhead(bank(pb_)[:, 0:128], [ptrk[pb_]], 2, 64, gn["gqa_k_norm"], AFF_tmp[u][:, 0:128],
                         [k_tmp[u]])
                rope(t, AFF_tmp[u][:, 0:128], k_tmp[u], 2, 64, 0, 64, rC64, rS64, nb[u][:, 0:128], [k_nb[u]])
            else:
                normhead(bank(pb_)[:, 0:128], [ptrk[pb_]], 2, 64, gn["gqa_k_norm"], nb[u][:, 0:128], [k_nb[u]])
            sch.op("act", lambda e, o=nb[u][:, 128:256], i=bank(pb_)[:, 128:256]: e.copy(out=o, in_=i),
                   r=[ptrk[pb_]], w=[k_nb[u]])
            ld("sp", Vc[t * 128:(t + 1) * 128, :], nb[u][:, 128:256], r=[k_nb[u]], w=[k_scr["Vc"]])
            transpose_out(t, nb[u], k_nb[u], 1, 128,
                          KcT.rearrange("(q p) t -> p q t", p=128)[:, :, t * 128:(t + 1) * 128], k_scr["KcT"])
        wb, kwb = load_w(1024, 512)
        for t in range(NT):
            pb_ = t % 4
            u = t % 2
            proj(t, wb, kwb, 0, 512, pb_)
            sch.op("act", lambda e, o=nb[u][:, 0:512], i=bank(pb_): e.copy(out=o, in_=i),
                   r=[ptrk[pb_]], w=[k_nb[u]])
            ld("sp", Va[t * 128:(t + 1) * 128, :], nb[u][:, 0:512], r=[k_nb[u]], w=[k_scr["Va"]])

        if dbg.get("stopC3"):
            break
        wcq, k_wcq = load_w(1536, 512)
        wckv, k_wckv = load_w(2048, 288)
        QbTv = QbT.rearrange("h d t -> d h t")
        KbTv = KbT.rearrange("h d t -> d h t")
        for t in range(NT):
            lat = t >= 2
            proj(t, wcq, k_wcq, 0, 512, 0)
            proj(t, wckv, k_wckv, 0, 288, 1)
            normhead(bank(0), [ptrk[0]], 1, 512, gn["mla_q_a_norm"], nb[0][:, 0:512], [k_nb[0]])
            normhead(bank(1)[:, 0:256], [ptrk[1]], 1, 256, gn["mla_kv_a_norm"], nb[0][:, 512:768], [k_nb[0]])
            sch.op("act", lambda e, o=kpe_f, i=bank(1)[:, 256:288]: e.copy(out=o, in_=i), r=[ptrk[1]], w=[k_kpe])
            for q in range(6):
                sch.op("pe", lambda e, o=bank_bf(6)[:, q * 128:(q + 1) * 128], i=nb[0][:, q * 128:(q + 1) * 128]:
                       e.transpose(out=o, in_=i, identity=ident_bf), r=[k_nb[0], k_cst], w=[ptrk[6]])
            sch.op("act", lambda e, o=cqT.rearrange("p a b -> p (a b)"), i=bank_bf(6)[:, 0:768]: e.copy(out=o, in_=i),
                   r=[ptrk[6]], w=[k_cqT])
            if dbg.get("mla_stop") == 1:
                continue
            for (cb, n, pb_) in ((0, 512, 2), (512, 256, 3)):
                for k in range(4):
                    sch.op("pe", lambda e, o=bank(pb_)[:, 0:n], l=cqT[:, k, :], r_=wqb[:, k, cb:cb + n],
                           st=(k == 0), sp_=(k == 3): e.matmul(o, l, r_, start=st, stop=sp_),
                           r=[k_cqT, k_w2], w=[ptrk[pb_]])
            normhead(pps[1][:, 0:768], [ptrk[2], ptrk[3]], 8, 96, gn["mla_q_norm"], AFF_tmp[0][:, 0:768], [k_tmp[0]])
            if lat:
                rope(t, AFF_tmp[0][:, 0:768], k_tmp[0], 8, 96, 64, 32, rC32, rS32, nb[0][:, 0:768], [k_nb[0]])
            else:
                sch.op("dve", lambda e, o=nb[0][:, 0:768], i=AFF_tmp[0][:, 0:768]: e.tensor_copy(out=o, in_=i),
                       r=[k_tmp[0]], w=[k_nb[0]])
            transpose_out(t, nb[0], k_nb[0], 8, 96, QbTv[:, :, t * 128:(t + 1) * 128], k_scr["QbT"])
            if dbg.get("mla_stop") == 2:
                continue
            for (cb, pb_) in ((0, 4), (512, 5)):
                for k in range(2):
                    sch.op("pe", lambda e, o=bank(pb_), l=cqT[:, 4 + k, :], r_=wkvb[:, k, cb:cb + 512],
                           st=(k == 0), sp_=(k == 1): e.matmul(o, l, r_, start=st, stop=sp_),
                           r=[k_cqT, k_w2], w=[ptrk[pb_]])
            kbf = AFF_tmp[1][:, 0:768].rearrange("p (h d) -> p h d", h=8)
            if dbg.get("mla_stop") == 31:
                continue
            for hb2 in range(2):
                kvb = bank(4 + hb2).rearrange("p (h d) -> p h d", h=4)
                sch.op("dve", lambda e, o=nb[1][:, hb2 * 256:(hb2 + 1) * 256].rearrange("p (h d) -> p h d", h=4),
                       i=kvb[:, :, 64:128]: e.tensor_copy(out=o, in_=i), r=[ptrk[4 + hb2]], w=[k_nb[1]])
                if dbg.get("mla_stop") == 32:
                    continue
                sch.op("dve", lambda e, o=kbf[:, hb2 * 4:(hb2 + 1) * 4, 0:64], i=kvb[:, :, 0:64]:
                       e.tensor_copy(out=o, in_=i), r=[ptrk[4 + hb2]], w=[k_tmp[1]])
            if dbg.get("mla_stop") in (32, 33):
                continue
            ld("sp", Vb[t * 128:(t + 1) * 128, :], nb[1][:, 0:512], r=[k_nb[1]], w=[k_scr["Vb"]])
            sch.op("dve", lambda e, o=kbf[:, :, 64:96], i=kpe_f.unsqueeze(1).to_broadcast([128, 8, 32]):
                   e.tensor_copy(out=o, in_=i), r=[k_kpe], w=[k_tmp[1]])
            if dbg.get("mla_stop") == 3:
                continue
            normhead(AFF_tmp[1][:, 0:768], [k_tmp[1]], 8, 96, gn["mla_k_norm"], AFF_tmp[1][:, 0:768], [k_tmp[1]])
            if dbg.get("mla_stop") == 4:
                continue
            if lat:
                rope(t, AFF_tmp[1][:, 0:768], k_tmp[1], 8, 96, 64, 32, rC32, rS32, nb[1][:, 0:768], [k_nb[1]])
            else:
                sch.op("dve", lambda e, o=nb[1][:, 0:768], i=AFF_tmp[1][:, 0:768]: e.tensor_copy(out=o, in_=i),
                       r=[k_tmp[1]], w=[k_nb[1]])
            transpose_out(t, nb[1], k_nb[1], 8, 96, KbTv[:, :, t * 128:(t + 1) * 128], k_scr["KbT"])

        if dbg.get("stopC4"):
            break
        barrier()
        gst = [scr6k[:, 0:T], scr6k[:, T:2 * T]]
        k_gst = [Trk(), Trk()]
        tblocks = [(0, 256)] + [(L + 512 * j, 512) for j in range(4)]
        if last:
            tblocks = tblocks[1:]
        t_lo = tblocks[0][0]
        gctr = 0
        for blk in range(12):
            wb, kwb = load_w(3104 + blk * 512, 512)
            for q in range(4):
                gc = blk * 4 + q
                sg = gc % 2
                for (q0, nq) in tblocks:
                    pb_ = gctr % 4
                    gctr += 1
                    tl = list(range(q0 // 128, (q0 + nq) // 128))
                    for k in range(KD):
                        sch.op("pe", lambda e, o=bank(pb_)[:, 0:nq], l=wb[:, k, q * 128:(q + 1) * 128],
                               r_=hT[:, k, q0:q0 + nq], st=(k == 0), sp_=(k == KD - 1):
                               e.matmul(o, l, r_, start=st, stop=sp_),
                               r=[kwb] + [k_hT[t] for t in tl], w=[ptrk[pb_]])
                    sch.op("act", lambda e, o=gst[sg][:, q0:q0 + nq], i=bank(pb_)[:, 0:nq]:
                           e.activation(out=o, in_=i, func=AF.Sigmoid), r=[ptrk[pb_]], w=[k_gst[sg]])
                ld("sp", GT[gc * 128:(gc + 1) * 128, t_lo:T], gst[sg][:, t_lo:T], r=[k_gst[sg]], w=[k_scr["GT"]])
        if dbg.get("stopC"):
            break
        new_phase()
        qt = [ABF.get(T) for _ in range(2)]
        kt = [ABF.get(T) for _ in range(2)]
        vt = [ABF.get(NT * 128, (NT, 128)) for _ in range(2)]
        k_q = [Trk(), Trk()]
        k_k = [Trk(), Trk()]
        k_v = [Trk(), Trk()]
        pT = [ABF.get(512) for _ in range(5)]
        k_pT = [Trk() for _ in range(5)]
        pending = [None]
        bias_bfs = [ABF.get(NA_NUNIQ * 512, (NA_NUNIQ, 512)) for _ in range(2)]
        k_biass = [Trk(), Trk()]
        bstage = [AFF.get(512) for _ in range(2)]
        k_bst = [Trk(), Trk()]
        posb = [AFF.get(512) for _ in range(2)]
        lnb = [AFF.get(512) for _ in range(2)]
        rbb = [AFF.get(512) for _ in range(2)]
        k_fin = [Trk(), Trk()]
        oTb = [ABF.get(512) for _ in range(2)]
        k_oT = [Trk(), Trk()]
        for b in range(2):
            sch.op("dve", lambda e, o=vt[b]: e.memset(o, 0.0), w=[k_v[b]])
            sch.op("dve", lambda e, o=vt[b][:, :, 64:65]: e.memset(o, 1.0), w=[k_v[b]])
            sch.op("dve", lambda e, o=qt[b]: e.memset(o, 0.0), w=[k_q[b]])
            sch.op("dve", lambda e, o=kt[b]: e.memset(o, 0.0), w=[k_k[b]])
        jobs = []
        for h in range(8):
            jobs.append((0, h, QaT[h * 64:(h + 1) * 64, :], KaT[h * 64:(h + 1) * 64, :], Va[:, h * 64:(h + 1) * 64],
                         64, 1.0))
        for h in range(8):
            jobs.append((1, h, QbT[h], KbT[h], Vb[:, h * 64:(h + 1) * 64], 96, 96 ** -0.5))
        for h in range(8):
            g = h // 4
            jobs.append((2, h, QcT[h * 64:(h + 1) * 64, :], KcT[g * 64:(g + 1) * 64, :], Vc[:, g * 64:(g + 1) * 64],
                         64, 0.125))
        sctr = 0
        pctr = 0
        fctr = 0
        srcname = {0: ("QaT", "KaT", "Va"), 1: ("QbT", "KbT", "Vb"), 2: ("QcT", "KcT", "Vc")}
        for j, (mix, h, Qs, Ks, Vs, dk, scale) in enumerate(jobs):
            buf = j % 2
            nq_, nk_, nv_ = srcname[mix]
            sch.op("dve", lambda e, o=qt[buf][64:128, :]: e.memset(o, 0.0), w=[k_q[buf]])
            sch.op("dve", lambda e, o=kt[buf][64:128, :]: e.memset(o, 0.0), w=[k_k[buf]])
            ld("pool", qt[buf][0:dk, :], Qs, r=[k_scr[nq_]], w=[k_q[buf]])
            ld("pool", kt[buf][0:dk, :], Ks, r=[k_scr[nk_]], w=[k_k[buf]])
            ld("pool", vt[buf][:, :, 0:64], Vs.rearrange("(n p) d -> p n d", p=128), r=[k_scr[nv_]], w=[k_v[buf]])
            bias_bf = bias_bfs[h % 2]
            k_bias = k_biass[h % 2]
            if mix == 0:
                for tg in range(2):
                    ld("pool", bias_bf[:, tg * 10:(tg + 1) * 10, :],
                       na_bias[li, h, tg * 10:(tg + 1) * 10].rearrange("t p c -> p t c"), w=[k_bias])
            blocks = [(L + 512 * qb, 512, qb) for qb in range(4)]
            if not last:
                blocks.append((0, 256, None))
            LA = 3

            def finalize(fin):
                (q0f, nqf, s2f, pobf, mixf, hf) = fin
                pbb = 4 + s2f
                sch.op("dve", lambda e, o=posb[s2f][:, 0:nqf], i=bank(pobf)[:, 0:nqf]: e.tensor_copy(out=o, in_=i),
                       r=[ptrk[pobf]], w=[k_fin[s2f]])
                sch.op("pe", lambda e, o=bank(pbb)[:, 0:nqf], r_=posb[s2f][:, 0:nqf]:
                       e.matmul(o, sel64_f, r_, start=True, stop=True),
                       r=[k_fin[s2f], k_cst], w=[ptrk[pbb]])
                sch.op("dve", lambda e, o=rbb[s2f][0:64, 0:nqf], i=bank(pbb)[0:64, 0:nqf]: e.reciprocal(out=o, in_=i),
                       r=[ptrk[pbb]], w=[k_fin[s2f]])
                sch.op("dve", lambda e, o=oTb[s2f][0:64, 0:nqf], a=posb[s2f][0:64, 0:nqf], b_=rbb[s2f][0:64, 0:nqf]:
                       e.tensor_tensor(out=o, in0=a, in1=b_, op=ALU.mult), r=[k_fin[s2f]], w=[k_oT[s2f]])
                ld("sp", OT[mixf, hf * 64:(hf + 1) * 64, q0f:q0f + nqf], oTb[s2f][0:64, 0:nqf], r=[k_oT[s2f]],
                   w=[k_scr["OT"]])

            for (q0, nq, qb) in blocks:
                if qb is None:
                    kch = [(0, None), (1, None)]
                elif mix == 0:
                    kch = [(0, None), (1, None)] + [(2 + kc, NA_TILE_ID[qb][jj]) for jj, kc in enumerate(NA_PLAN[qb])]
                else:
                    kch = [(kc, None) for kc in range(NT)]
                s2 = fctr % 2
                fctr += 1
                pob = 6 + s2
                nk = len(kch)
                pslots = [None] * nk
                for idx in range(nk + LA):
                    if idx < nk:
                        kc, bi = kch[idx]
                        psb = sctr % 4
                        sctr += 1
                        sch.op("pe", lambda e, o=bank(psb)[:, 0:nq], l=kt[buf][:, kc * 128:(kc + 1) * 128],
                               r_=qt[buf][:, q0:q0 + nq], sp_=(bi is None): e.matmul(o, l, r_, start=True, stop=sp_),
                               r=[k_k[buf], k_q[buf]], w=[ptrk[psb]])
                        if bi is not None:
                            sch.op("pe", lambda e, o=bank(psb)[:, 0:nq], r_=bias_bf[:, bi, 0:nq]:
                                   e.matmul(o, ident_bf, r_, start=False, stop=True), r=[k_bias, k_cst], w=[ptrk[psb]])
                        p_ = pctr % 5
                        pctr += 1
                        pslots[idx] = p_
                        sch.op("act", lambda e, o=pT[p_][:, 0:nq], i=bank(psb)[:, 0:nq], sc_=scale:
                               e.activation(out=o, in_=i, func=AF.Exp, scale=sc_), r=[ptrk[psb]], w=[k_pT[p_]])
                    j2 = idx - LA
                    if j2 >= 0:
                        kc2 = kch[j2][0]
                        p2 = pslots[j2]
                        sch.op("pe", lambda e, o=bank(pob)[:, 0:nq], l=vt[buf][:, kc2, :], r_=pT[p2][:, 0:nq],
                               st=(j2 == 0), sp_=(j2 == nk - 1): e.matmul(o, l, r_, start=st, stop=sp_),
                               r=[k_v[buf], k_pT[p2]], w=[ptrk[pob]])
                    if idx == LA and pending[0] is not None:
                        finalize(pending[0])
                        pending[0] = None
                if pending[0] is not None:
                    finalize(pending[0])
                pending[0] = (q0, nq, s2, pob, mix, h)
        if pending[0] is not None:
            finalize(pending[0])
            pending[0] = None
        if dbg.get("stopD"):
            break

        new_phase()
        wbr = [ABF.get(4 * D, (4, D)) for _ in range(3)]
        k_wbr = Trk()
        for br in range(3):
            ld("pool", wbr[br], w_br[br][li].rearrange("(k p) c -> p k c", p=128), w=[k_wbr])
        otb = [[ABF.get(4 * 512, (4, 512)) for _ in range(3)] for _ in range(2)]
        k_otb = [Trk(), Trk()]
        gtb = [[ABF.get(512) for _ in range(3)] for _ in range(2)]
        k_gtb = [Trk(), Trk()]
        ytb = [ABF.get(KD * 512, (KD, 512)) for _ in range(2)]
        k_ytb = [Trk(), Trk()]
        ef = [[AFF.get(512) for _ in range(3)] for _ in range(2)]
        k_ef = [Trk(), Trk()]
        OTv = OT.rearrange("m (k p) t -> m p k t", p=128)
        ectr = 0
        for bi_, (q0, nq) in enumerate(tblocks):
            ob = bi_ % 2
            for br in range(3):
                ld("sp", otb[ob][br][:, :, 0:nq], OTv[br][:, :, q0:q0 + nq], r=[k_scr["OT"]], w=[k_otb[ob]])
            for dc in range(KD):
                gb = ectr % 2
                ectr += 1
                for br in range(3):
                    ld("sp", gtb[gb][br][:, 0:nq], GT[br * D + dc * 128:br * D + (dc + 1) * 128, q0:q0 + nq],
                       r=[k_scr["GT"]], w=[k_gtb[gb]])
                pbs = [(3 * gb + br) for br in range(3)]
                for br in range(3):
                    for k in range(4):
                        sch.op("pe", lambda e, o=bank(pbs[br])[:, 0:nq], l=wbr[br][:, k, dc * 128:(dc + 1) * 128],
                               r_=otb[ob][br][:, k, 0:nq], st=(k == 0), sp_=(k == 3):
                               e.matmul(o, l, r_, start=st, stop=sp_), r=[k_wbr, k_otb[ob]], w=[ptrk[pbs[br]]])
                for br in range(3):
                    sch.op("dve", lambda e, o=ef[gb][br][:, 0:nq], a=bank(pbs[br])[:, 0:nq], b_=gtb[gb][br][:, 0:nq]:
                           e.tensor_tensor(out=o, in0=a, in1=b_, op=ALU.mult),
                           r=[ptrk[pbs[br]], k_gtb[gb]], w=[k_ef[gb]])
                sch.op("pool", lambda e, o=ef[gb][0][:, 0:nq], a=ef[gb][0][:, 0:nq], b_=ef[gb][1][:, 0:nq]:
                       e.tensor_tensor(out=o, in0=a, in1=b_, op=ALU.add), r=[k_ef[gb]], w=[k_ef[gb]])
                sch.op("pool", lambda e, o=ytb[ob][:, dc, 0:nq], a=ef[gb][0][:, 0:nq], b_=ef[gb][2][:, 0:nq]:
                       e.tensor_tensor(out=o, in0=a, in1=b_, op=ALU.add), r=[k_ef[gb]], w=[k_ytb[ob]])
            ld("sp", YT.rearrange("(k p) t -> p k t", p=128)[:, :, q0:q0 + nq], ytb[ob][:, :, 0:nq],
               r=[k_ytb[ob]], w=[k_scr["YT"]])

        new_phase()
        wo = ABF.get(KD * D, (KD, D))
        k_wo = Trk()
        for j4 in range(4):
            ld("pool", wo[:, :, j4 * 512:(j4 + 1) * 512],
               w_out[li].rearrange("(k p) c -> p k c", p=128)[:, :, j4 * 512:(j4 + 1) * 512], w=[k_wo])
        grep1 = AFF.get(2 * D, (2, D))
        k_gr = Trk()
        for kind in range(2):
            ld("sp", grep1[:, kind, :], GREP[0, kind], r=[k_scr["GREP"]], w=[k_gr])
        yti = [ABF.get(KD * 128, (KD, 128)) for _ in range(2)]
        k_yti = [Trk(), Trk()]
        xe2 = [AFF.get(D) for _ in range(2)]
        k_xe2 = [Trk(), Trk()]
        tmpf = [AFF.get(512) for _ in range(2)]
        k_tmpf = [Trk(), Trk()]
        YTv = YT.rearrange("(k p) t -> p k t", p=128)
        octr = 0
        for t in range(2 if last else 0, NT):
            b = t % 2
            kind = kind_of(t)
            ld("pool", yti[b], YTv[:, :, t * 128:(t + 1) * 128], r=[k_scr["YT"]], w=[k_yti[b]])
            ld("pool", xe2[b], X[t * 128:(t + 1) * 128, :], r=[k_X[t]], w=[k_xe2[b]])
            for j4 in range(4):
                pb_ = octr % 4
                tb_ = octr % 2
                octr += 1
                for k in range(KD):
                    sch.op("pe", lambda e, o=bank(pb_), l=yti[b][:, k, :], r_=wo[:, k, j4 * 512:(j4 + 1) * 512],
                           st=(k == 0), sp_=(k == KD - 1): e.matmul(o, l, r_, start=st, stop=sp_),
                           r=[k_yti[b], k_wo], w=[ptrk[pb_]])
                sch.op("dve", lambda e, o=tmpf[tb_], a=bank(pb_), g_=grep1[:, kind, j4 * 512:(j4 + 1) * 512]:
                       e.tensor_tensor(out=o, in0=a, in1=g_, op=ALU.mult), r=[ptrk[pb_], k_gr], w=[k_tmpf[tb_]])
                sch.op("dve", lambda e, o=xe2[b][:, j4 * 512:(j4 + 1) * 512], a=xe2[b][:, j4 * 512:(j4 + 1) * 512],
                       t_=tmpf[tb_]: e.tensor_tensor(out=o, in0=a, in1=t_, op=ALU.add),
                       r=[k_tmpf[tb_]], w=[k_xe2[b]])
            ld("sp", X[t * 128:(t + 1) * 128, :], xe2[b], r=[k_xe2[b]], w=[k_X[t]])
        if dbg.get("stopE"):
            break
        new_phase()
        xn2 = ABF.get(NT * D, (NT, D))
        k_xn2 = [Trk() for _ in range(NT)]
        wr = ABF.get(KD * E, (KD, E))
        k_wr = Trk()
        ld("pool", wr, w_router[li].rearrange("(k p) e -> p k e", p=128), w=[k_wr])
        sel = ABF.get(16 * 256, (16, 256))
        selc = ABF.get(2 * 32, (2, 32))
        selw = [ABF.get(256) for _ in range(2)]
        swst = ABF.get(2 * S, (2, S))
        swsc = ABF.get(256)
        xeT = ABF.get(KD * 288, (KD, 288))
        wgu_r = ABF.get(16384)
        hid1 = ABF.get(8 * 288, (8, 288))
        hid = [hid1, hid1]
        wgu = [[wgu_r[:, (2 * b2 + kk) * 4096:(2 * b2 + kk + 1) * 4096].rearrange("p (k f) -> p k f", k=KD)
                for kk in range(2)] for b2 in range(2)]
        junk = wgu_r[:, 0:D]
        k_junk = Trk()
        h2T = [wgu_r[:, D + b2 * 2048:D + (b2 + 1) * 2048].rearrange("p (k f) -> p k f", k=KD) for b2 in range(2)]
        k_h2T = [Trk(), Trk()]
        xreg = AFF.get(2 * T)
        xb = [xreg[:, 0:D], xreg[:, T:T + D]]
        k_xb = [Trk(), Trk()]
        st_f = AFF.get(4 * NT, (4, NT))
        k_st = Trk()
        aff_tm = AFF.get(NT * E, (NT, E))
        posm_tm = AFF.get(NT * E, (NT, E))
        affT = AFF.get(T)
        reg2 = AFF.get(T)
        rs_f = AFF.get(16, (4, 4))
        k_aff = Trk()
        k_affT = Trk()
        for t in range(NT):
            b = t % 2
            kind = kind_of(t)
            ld("pool", xb[b], X[t * 128:(t + 1) * 128, :], r=[k_X[t]], w=[k_xb[b]])
            norm_tile(t, xb[b], k_xb[b], xn2[:, t, :], k_xn2[t])
            transpose_mod(t, xn2[:, t, :], k_xn2[t], lambda k, b=b: h2T[b][:, k, :], k_h2T[b], 2)
            pb_ = t % 2
            for k in range(KD):
                sch.op("pe", lambda e, o=bank(pb_)[:, 0:E], l=h2T[b][:, k, :], r_=wr[:, k, :],
                       st=(k == 0), sp_=(k == KD - 1): e.matmul(o, l, r_, start=st, stop=sp_),
                       r=[k_h2T[b], k_wr], w=[ptrk[pb_]])
            sch.op("dve", lambda e, o=rs_f[:, 0, 0:1], i=bank(pb_)[:, 0:E]:
                   e.tensor_reduce(out=o, in_=i, axis=AX.X, op=ALU.max), r=[ptrk[pb_]], w=[k_aff])
            sch.op("dve", lambda e, o=rs_f[:, 1, 0:1], i=rs_f[:, 0, 0:1]:
                   e.tensor_scalar(out=o, in0=i, scalar1=-1.0, scalar2=None, op0=ALU.mult), r=[k_aff], w=[k_aff])
            sch.op("act", lambda e, o=aff_tm[:, t, :], i=bank(pb_)[:, 0:E], b_=rs_f[:, 1, 0:1], a=rs_f[:, 2, 0:1]:
                   e.activation(out=o, in_=i, func=AF.Exp, bias=b_, scale=1.0, accum_out=a),
                   r=[ptrk[pb_], k_aff], w=[k_aff])
            sch.op("dve", lambda e, o=rs_f[:, 3, 0:1], i=rs_f[:, 2, 0:1]: e.reciprocal(out=o, in_=i),
                   r=[k_aff], w=[k_aff])
            sch.op("dve", lambda e, o=aff_tm[:, t, :], i=aff_tm[:, t, :], s_=rs_f[:, 3, 0:1]:
                   e.tensor_scalar(out=o, in0=i, scalar1=s_, scalar2=None, op0=ALU.mult), r=[k_aff], w=[k_aff])
            pb2 = 2 + t % 2
            sch.op("pe", lambda e, o=bank(pb2)[0:E, 0:128], i=aff_tm[:, t, :]:
                   e.transpose(out=o, in_=i, identity=ident_f), r=[k_aff, k_cst], w=[ptrk[pb2]])
            sch.op("act", lambda e, o=affT[0:E, t * 128:(t + 1) * 128], i=bank(pb2)[0:E, 0:128]: e.copy(out=o, in_=i),
                   r=[ptrk[pb2]], w=[k_affT])

        barrier()
        work = xreg[:, 0:T]
        cs = xreg[:, T:2 * T]
        m8 = rs_f.rearrange("p a b -> p (a b)")[0:E, 0:8]
        thr = AFF.get(2)
        k_tk = Trk()
        sch.op("dve", lambda e: e.tensor_copy(out=work[0:E, 0:T], in_=affT[0:E, 0:T]), r=[k_affT], w=[k_tk])
        segs = [(L, T, CAP_LAT, 0)] if last else [(L, T, CAP_LAT, 0), (0, L, CAP_CTX, 1)]
        for (lo, hi, cap, col) in segs:
            for rnd in range(cap // 8):
                sch.op("dve", lambda e, i=work[0:E, lo:hi]: e.max(out=m8, in_=i), r=[k_tk], w=[k_tk])
                if rnd < cap // 8 - 1:
                    sch.op("dve", lambda e, o=work[0:E, lo:hi]:
                           e.match_replace(out=o, in_to_replace=m8, in_values=o, imm_value=-1.0), r=[k_tk], w=[k_tk])
            sch.op("dve", lambda e, o=thr[0:E, col:col + 1]: e.tensor_copy(out=o, in_=m8[:, 7:8]), r=[k_tk], w=[k_tk])
        for (lo, hi, cap, col) in segs:
            sch.op("dve", lambda e, o=work[0:E, lo:hi], i=affT[0:E, lo:hi], s_=thr[0:E, col:col + 1]:
                   e.tensor_scalar(out=o, in0=i, scalar1=s_, scalar2=None, op0=ALU.is_ge), r=[k_tk, k_affT], w=[k_tk])
            sch.op("dve", lambda e, o=cs[0:E, lo:hi], i=work[0:E, lo:hi]:
                   e.tensor_tensor_scan(out=o, data0=i, data1=i, initial=0.0, op0=ALU.add, op1=ALU.max),
                   r=[k_tk], w=[k_tk])
        lo0 = L if last else 0
        sch.op("dve", lambda e, o=cs[0:E, lo0:T], a=cs[0:E, lo0:T], b_=work[0:E, lo0:T]:
               e.tensor_tensor(out=o, in0=a, in1=b_, op=ALU.mult), r=[k_tk], w=[k_tk])
        sch.op("dve", lambda e, o=cs[0:E, lo0:T], i=cs[0:E, lo0:T]:
               e.tensor_scalar(out=o, in0=i, scalar1=-1.0, scalar2=None, op0=ALU.add), r=[k_tk], w=[k_tk])
        t_first = 2 if last else 0
        for t in range(t_first, NT):
            sch.op("pe", lambda e, o=bank(4)[:, t * E:(t + 1) * E], i=cs[0:E, t * 128:(t + 1) * 128]:
                   e.transpose(out=o, in_=i, identity=ident_f[0:E, 0:E]), r=[k_tk, k_cst], w=[ptrk[4]])
        sch.op("act", lambda e, o=posm_tm.rearrange("p a b -> p (a b)")[:, t_first * E:NT * E],
               i=bank(4)[:, t_first * E:NT * E]: e.copy(out=o, in_=i), r=[ptrk[4]], w=[k_tk])

        barrier()
        k_sel = Trk()
        k_selw = [Trk(), Trk()]
        k_swst = Trk()
        k_swsc = Trk()
        k_xeT = Trk()
        k_wgu = [Trk(), Trk()]
        k_hid1 = Trk()
        k_hid = [k_hid1, k_hid1]
        sgf = [reg2[:, 0:288], reg2[:, 512:800]]
        k_sgf = [Trk(), Trk()]
        nj = 256 if last else 288
        wctr2 = 0
        for ex in range(E):
            for t in range(16):
                sch.op("dve", lambda e, o=sel[:, t, :], s_=posm_tm[:, 2 + t, ex:ex + 1]:
                       e.tensor_scalar(out=o, in0=iota_f[:, 0:256], scalar1=s_, scalar2=None, op0=ALU.is_equal),
                       r=[k_tk, k_cst], w=[k_sel])
            if not last:
                for t in range(2):
                    sch.op("dve", lambda e, o=selc[:, t, :], s_=posm_tm[:, t, ex:ex + 1]:
                           e.tensor_scalar(out=o, in0=iota_f[:, 0:32], scalar1=s_, scalar2=None, op0=ALU.is_equal),
                           r=[k_tk, k_cst], w=[k_sel])
            for t in range(16):
                s_w = t % 2
                sch.op("dve", lambda e, o=selw[s_w], s_=posm_tm[:, 2 + t, ex:ex + 1], a_=aff_tm[:, 2 + t, ex:ex + 1]:
                       e.tensor_scalar(out=o, in0=iota_f[:, 0:256], scalar1=s_, scalar2=a_, op0=ALU.is_equal,
                                       op1=ALU.mult), r=[k_tk, k_aff, k_cst], w=[k_selw[s_w]])
                pb_ = 6 + (t // 2) % 2
                for jc in range(2):
                    sch.op("pe", lambda e, o=bank_bf(pb_)[:, jc * 256 + (t % 2) * 128:jc * 256 + (t % 2) * 128 + 128],
                           i=selw[s_w][:, jc * 128:(jc + 1) * 128]: e.transpose(out=o, in_=i, identity=ident_bf),
                           r=[k_selw[s_w], k_cst], w=[ptrk[pb_]])
                if t % 2 == 1:
                    t0 = t - 1
                    sch.op("dve", lambda e, o=swst[:, :, t0 * 128:(t0 + 2) * 128],
                           i=bank_bf(pb_)[:, 0:512].rearrange("p (c t) -> p c t", c=2): e.tensor_copy(out=o, in_=i),
                           r=[ptrk[pb_]], w=[k_swst])
            for jc in range(2):
                ld("sp", SWTL.rearrange("t j e c k -> j e c t k")[:, ex, jc],
                   swst[:, jc, :].rearrange("j (t k) -> j t k", k=128), r=[k_swst], w=[k_scr["SWTL"]])
            if not last:
                for t in range(2):
                    s_w = t % 2
                    sch.op("dve", lambda e, o=selw[s_w][:, 0:32], s_=posm_tm[:, t, ex:ex + 1],
                           a_=aff_tm[:, t, ex:ex + 1]:
                           e.tensor_scalar(out=o, in0=iota_f[:, 0:32], scalar1=s_, scalar2=a_, op0=ALU.is_equal,
                                           op1=ALU.mult), r=[k_tk, k_aff, k_cst], w=[k_selw[s_w]])
                    sch.op("pe", lambda e, o=bank_bf(5)[0:32, t * 128:(t + 1) * 128], i=selw[s_w][:, 0:32]:
                           e.transpose(out=o, in_=i, identity=ident_bf), r=[k_selw[s_w], k_cst], w=[ptrk[5]])
                sch.op("act", lambda e, o=swsc[0:32, 0:256], i=bank_bf(5)[0:32, 0:256]: e.copy(out=o, in_=i),
                       r=[ptrk[5]], w=[k_swsc])
                ld("sp", SWTC[ex], swsc[0:32, 0:256], r=[k_swsc], w=[k_scr["SWTC"]])
            for dch in range(KD):
                pb_ = dch % 2
                for t in range(16):
                    sch.op("pe", lambda e, o=bank(pb_)[:, 0:256], l=xn2[:, 2 + t, dch * 128:(dch + 1) * 128],
                           r_=sel[:, t, :], st=(t == 0), sp_=(t == 15): e.matmul(o, l, r_, start=st, stop=sp_),
                           r=[k_xn2[2 + t], k_sel], w=[ptrk[pb_]])
                if not last:
                    for t in range(2):
                        sch.op("pe", lambda e, o=bank(pb_)[:, 256:288], l=xn2[:, t, dch * 128:(dch + 1) * 128],
                               r_=selc[:, t, :], st=(t == 0), sp_=(t == 1): e.matmul(o, l, r_, start=st, stop=sp_),
                               r=[k_xn2[t], k_sel], w=[ptrk[pb_]])
                sch.op("act", lambda e, o=xeT[:, dch, 0:256], i=bank(pb_)[:, 0:256], sc_=modcol(2, dch, 0),
                       bi_=modcol(3, dch, 0): e.activation(out=o, in_=i, func=AF.Identity, scale=sc_, bias=bi_),
                       r=[ptrk[pb_], k_mod], w=[k_xeT])
                if not last:
                    sch.op("act", lambda e, o=xeT[:, dch, 256:288], i=bank(pb_)[:, 256:288], sc_=modcol(2, dch, 1),
                           bi_=modcol(3, dch, 1): e.activation(out=o, in_=i, func=AF.Identity, scale=sc_, bias=bi_),
                           r=[ptrk[pb_], k_mod], w=[k_xeT])
            hb = ex % 2
            for fp in range(4):
                wb2 = wctr2 % 2
                wctr2 += 1
                ld("pool", wgu[wb2][0], w_eg[li, ex].rearrange("(k p) f -> p k f", p=128)[:, :, fp * 256:(fp + 1) * 256],
                   w=[k_wgu[wb2]])
                ld("pool", wgu[wb2][1], w_eu[li, ex].rearrange("(k p) f -> p k f", p=128)[:, :, fp * 256:(fp + 1) * 256],
                   w=[k_wgu[wb2]])
                for f2 in range(2):
                    fc = fp * 2 + f2
                    s3 = fc % 2
                    pg, pu = 2 + 2 * s3, 3 + 2 * s3
                    for kk, pb_ in ((0, pg), (1, pu)):
                        for k in range(KD):
                            sch.op("pe", lambda e, o=bank(pb_)[:, 0:nj], l=wgu[wb2][kk][:, k, f2 * 128:(f2 + 1) * 128],
                                   r_=xeT[:, k, 0:nj], st=(k == 0), sp_=(k == KD - 1):
                                   e.matmul(o, l, r_, start=st, stop=sp_),
                                   r=[k_wgu[wb2], k_xeT], w=[ptrk[pb_]])
                    sch.op("act", lambda e, o=sgf[s3][:, 0:nj], i=bank(pg)[:, 0:nj]:
                           e.activation(out=o, in_=i, func=AF.Silu), r=[ptrk[pg]], w=[k_sgf[s3]])
                    sch.op("dve", lambda e, o=hid[hb][:, fc, 0:nj], a=sgf[s3][:, 0:nj], b_=bank(pu)[:, 0:nj]:
                           e.tensor_tensor(out=o, in0=a, in1=b_, op=ALU.mult),
                           r=[k_sgf[s3], ptrk[pu]], w=[k_hid[hb]])
            ld("sp", HID[ex][:, :, 0:nj], hid[hb][:, :, 0:nj], r=[k_hid[hb]], w=[k_scr["HID"]])

        new_phase()
        ye = ABF.get(E * 3 * 512, (E, 3, 512))
        k_ye = Trk()
        hidb = [ABF.get(8 * 288, (8, 288)) for _ in range(2)]
        k_hidb = [Trk(), Trk()]
        wd = [ABF.get(8 * 512, (8, 512)) for _ in range(2)]
        k_wd = [Trk(), Trk()]
        swl = [ABF.get(E * 256, (E, 2, 128)) for _ in range(2)]
        k_swl = [Trk(), Trk()]
        swc = [ABF.get(E * 128, (E, 128)) for _ in range(2)]
        k_swc = [Trk(), Trk()]
        xpf = [AFF.get(512) for _ in range(2)]
        k_xpf = [Trk(), Trk()]
        tmpf = [AFF.get(512) for _ in range(2)]
        k_tmpf = [Trk(), Trk()]
        grep2 = AFF.get(2 * D, (2, D))
        k_gr2 = Trk()
        for kind in range(2):
            ld("sp", grep2[:, kind, :], GREP[1, kind], r=[k_scr["GREP"]], w=[k_gr2])
        SWTCv = SWTC.rearrange("e j t -> j e t")
        jcs = ((0, 128), (1, 128)) if last else ((0, 128), (1, 128), (2, 32))
        yctr = 0
        xctr = 0
        for dblk in range(4):
            dsl = slice(dblk * 512, (dblk + 1) * 512)
            for ex in range(E):
                b = ex % 2
                ld("pool", hidb[b][:, :, 0:nj], HID[ex][:, :, 0:nj], r=[k_scr["HID"]], w=[k_hidb[b]])
                ld("pool", wd[b], w_ed[li, ex].rearrange("(k p) d -> p k d", p=128)[:, :, dsl], w=[k_wd[b]])
                for (jc, rows) in jcs:
                    pb_ = yctr % 4
                    yctr += 1
                    for k in range(8):
                        sch.op("pe", lambda e, o=bank(pb_)[0:rows, :], l=hidb[b][:, k, jc * 128:jc * 128 + rows],
                               r_=wd[b][:, k, :], st=(k == 0), sp_=(k == 7): e.matmul(o, l, r_, start=st, stop=sp_),
                               r=[k_hidb[b], k_wd[b]], w=[ptrk[pb_]])
                    sch.op("act", lambda e, o=ye[0:rows, ex, jc, :], i=bank(pb_)[0:rows, :]: e.copy(out=o, in_=i),
                           r=[ptrk[pb_]], w=[k_ye])
            for t in range(2 if last else 0, NT):
                b = xctr % 2
                pb_ = 4 + xctr % 4
                xctr += 1
                kind = kind_of(t)
                ld("pool", xpf[b], X[t * 128:(t + 1) * 128, dsl], r=[k_X[t]], w=[k_xpf[b]])
                if kind == 1:
                    ld("pool", swc[b][0:32], SWTCv[:, :, t * 128:(t + 1) * 128], r=[k_scr["SWTC"]], w=[k_swc[b]])
                    for ex in range(E):
                        sch.op("pe", lambda e, o=bank(pb_), l=swc[b][0:32, ex, :], r_=ye[0:32, ex, 2, :],
                               st=(ex == 0), sp_=(ex == E - 1): e.matmul(o, l, r_, start=st, stop=sp_),
                               r=[k_swc[b], k_ye], w=[ptrk[pb_]])
                else:
                    ld("pool", swl[b], SWTL[t - 2], r=[k_scr["SWTL"]], w=[k_swl[b]])
                    for ex in range(E):
                        for jc in range(2):
                            sch.op("pe", lambda e, o=bank(pb_), l=swl[b][:, ex, jc, :], r_=ye[:, ex, jc, :],
                                   st=(ex == 0 and jc == 0), sp_=(ex == E - 1 and jc == 1):
                                   e.matmul(o, l, r_, start=st, stop=sp_), r=[k_swl[b], k_ye], w=[ptrk[pb_]])
                sch.op("dve", lambda e, o=tmpf[b], a=bank(pb_), g_=grep2[:, kind, dsl]:
                       e.tensor_tensor(out=o, in0=a, in1=g_, op=ALU.mult), r=[ptrk[pb_], k_gr2], w=[k_tmpf[b]])
                sch.op("dve", lambda e, o=xpf[b], a=xpf[b], t_=tmpf[b]: e.tensor_tensor(out=o, in0=a, in1=t_, op=ALU.add),
                       r=[k_tmpf[b]], w=[k_xpf[b]])
                ld("sp", X[t * 128:(t + 1) * 128, dsl], xpf[b], r=[k_xpf[b]], w=[k_X[t]])
        if dbg.get("stopF"):
            break

    barrier()
    k_out = Trk()
    for j in range(4):
        ld("sp", out[j * 512:(j + 1) * 512, :], X[L + j * 512:L + (j + 1) * 512, :], w=[k_out])
    barrier()
    with nc.Block() as block:
        @block.tensor
        def _(e):
            sch.replay("pe", e)

        @block.scalar
        def _(e):
            sch.replay("act", e)

        @block.vector
        def _(e):
            sch.replay("dve", e)

        @block.gpsimd
        def _(e):
            sch.replay("pool", e)

        @block.sync
        def _(e):
            sch.replay("sp", e)
    es.close()
    return nc


def host_inputs(inp, b, nlw=DEPTH, ne=E):
    f = np.float32
    m = {}
    m["x"] = np.ascontiguousarray(inp["x"][b])
    m["ctx"] = np.ascontiguousarray(inp["ctx"][b])
    cT = np.stack([inp["c"][b].reshape(KD, 128).T, inp["c_ctx"].reshape(KD, 128).T], axis=2)
    m["cT"] = np.ascontiguousarray(cT.reshape(128, KD * 2)).astype(f)
    m["w_mod"] = inp["w_mod"][:nlw]
    m["b_mod"] = inp["b_mod"][:nlw]
    m["b_modT"] = np.ascontiguousarray(inp["b_mod"][:nlw].reshape(nlw, 96, 128).transpose(0, 2, 1))
    m["norm1T"] = np.ascontiguousarray(inp["norm1"][:nlw].reshape(nlw, KD, 128).transpose(0, 2, 1))
    m["norm2T"] = np.ascontiguousarray(inp["norm2"][:nlw].reshape(nlw, KD, 128).transpose(0, 2, 1))
    m["w_in"] = inp["w_in"][:nlw]
    idx = _na_uniq_idx()
    rb = inp["na_rel_bias"][:nlw].reshape(nlw, 8, 15 * 31)
    rbp = np.concatenate([rb, np.full((nlw, 8, 1), NEG, f)], axis=2)
    m["na_bias"] = np.ascontiguousarray(rbp[:, :, idx])
    for nm in ("na_q_norm", "na_k_norm", "mla_q_a_norm", "mla_kv_a_norm", "mla_q_norm", "mla_k_norm",
               "gqa_q_norm", "gqa_k_norm"):
        m[nm] = inp[nm][:nlw]
    m["mla_w_q_b"] = inp["mla_w_q_b"][:nlw]
    m["mla_w_kv_b"] = inp["mla_w_kv_b"][:nlw]
    m["w_branch_a"] = inp["w_branch_a"][:nlw]
    m["w_branch_b"] = inp["w_branch_b"][:nlw]
    m["w_branch_c"] = inp["w_branch_c"][:nlw]
    m["w_out"] = inp["w_out"][:nlw]
    m["w_router"] = inp["w_router"][:nlw]
    m["w_expert_gate"] = inp["w_expert_gate"][:nlw, :ne]
    m["w_expert_up"] = inp["w_expert_up"][:nlw, :ne]
    m["w_expert_down"] = inp["w_expert_down"][:nlw, :ne]
    c64, s64 = _rope_tables(64)
    c32, s32 = _rope_tables(32)
    m["ropeC64"], m["ropeS64"], m["ropeC32"], m["ropeS32"] = c64, s64, c32, s32
    m["ident"] = np.eye(128, dtype=f)
    m["iota"] = np.ascontiguousarray(np.broadcast_to(np.arange(256, dtype=f), (128, 256)))
    sel = np.zeros((128, 128), f)
    sel[64, 0:64] = 1.0
    m["sel64"] = sel
    return {k: np.ascontiguousarray(v, dtype=f) for k, v in m.items()}


_NC_CACHE = {}


def kernel(**inputs):
    inp = {k: np.asarray(v) for k, v in inputs.items()}
    if "nc" not in _NC_CACHE:
        _NC_CACHE["nc"] = build()
    nc = _NC_CACHE["nc"]
    in_maps = [host_inputs(inp, b) for b in range(NCORES)]
    res = run_bass_kernel_spmd(nc, in_maps, core_ids=list(range(NCORES)))
    out = np.stack([np.asarray(res.results[b]["out"]) for b in range(NCORES)], axis=0)
    return out.astype(np.float32, copy=False)
```

```python
import numpy as np
from contextlib import ExitStack
import concourse.bass as bass
import concourse.mybir as mybir
from concourse.bass_utils import run_bass_kernel_spmd

F32 = mybir.dt.float32
BF16 = mybir.dt.bfloat16
AF = mybir.ActivationFunctionType
ALU = mybir.AluOpType
AX = mybir.AxisListType

D = 2048
KD = 16
L = 256
S = 2048
T = L + S
NT = T // 128
DEPTH = 4
GRID_W = 64
INW = 9248
E = 16
FF = 1024
CAP_LAT = 256
CAP_CTX = 32
EPS = 1e-6
NCORES = 4
NEG = -30000.0


class Trk:
    __slots__ = ("w", "r")

    def __init__(self):
        self.w = {}
        self.r = {}


ENGS = ("pe", "act", "dve", "pool", "sp")
NRING = 12
EPOCH = 30000


class Sch:
    def __init__(self, nc, es):
        self.nc = nc
        self.es = es
        self.sems = []
        self.owner = []
        self.stream = {e: [] for e in ENGS}
        self.cnt = {e: 0 for e in ENGS}
        self.csem = {e: None for e in ENGS}
        self.waited = {e: {} for e in ENGS}
        self.ring = {}
        self.dman = {}
        self.dtok = {}
        for q in ("sp", "pool", "act"):
            self.ring[q] = [self._newsem("d%s%d" % (q, i), "dma") for i in range(NRING)]
            self.dman[q] = 0
            self.dtok[q] = [None] * NRING

    def _newsem(self, name, owner):
        h = self.es.enter_context(self.nc.semaphore(name))
        self.sems.append(h)
        self.owner.append(owner)
        return len(self.sems) - 1

    def _need(self, e, si, v, waits):
        if e == "pe" and self.owner[si] == "pe":
            return
        if self.waited[e].get(si, 0) >= v:
            return
        if waits.get(si, 0) < v:
            waits[si] = v

    def _deps(self, e, r, w):
        waits = {}
        for t in r:
            for si, v in t.w.items():
                self._need(e, si, v, waits)
        for t in w:
            for si, v in t.w.items():
                self._need(e, si, v, waits)
            for si, v in t.r.items():
                self._need(e, si, v, waits)
        return waits

    def _commit(self, e, waits, tok, r, w):
        for si, v in waits.items():
            self.waited[e][si] = v
        si, v = tok
        for t in r:
            if t.r.get(si, 0) < v:
                t.r[si] = v
        for t in w:
            t.w = {si: v}
            t.r = {}

    def op(self, e, fn, r=(), w=()):
        waits = self._deps(e, r, w)
        if self.csem[e] is None or self.cnt[e] >= EPOCH:
            self.csem[e] = self._newsem("c%s%d" % (e, len(self.sems)), e)
            self.cnt[e] = 0
        self.cnt[e] += 1
        tok = (self.csem[e], self.cnt[e])
        self.stream[e].append((list(waits.items()), fn, tok, 1))
        self._commit(e, waits, tok, r, w)
        return tok

    def dma(self, q, fn, r=(), w=()):
        waits = self._deps(q, r, w)
        n = self.dman[q]
        slot = n % NRING
        prev = self.dtok[q][slot]
        if prev is not None:
            self._need(q, prev[0], prev[1], waits)
        tok = (self.ring[q][slot], 16 * (n // NRING + 1))
        self.dman[q] = n + 1
        self.dtok[q][slot] = tok
        self.stream[q].append((list(waits.items()), fn, tok, 16))
        self._commit(q, waits, tok, r, w)
        return tok

    def wait_all(self, e, trks):
        waits = {}
        for t in trks:
            for si, v in t.w.items():
                self._need(e, si, v, waits)
        for si, v in waits.items():
            self.waited[e][si] = v
        self.stream[e].append((list(waits.items()), None, None, 0))

    def replay(self, e, eng):
        for waits, fn, tok, inc in self.stream[e]:
            for si, v in waits:
                eng.wait_ge(self.sems[si], v)
            if fn is not None:
                fn(eng).then_inc(self.sems[tok[0]], inc)


def _rope_tables(rot_dim):
    t = np.arange(S)
    row = (t // GRID_W).astype(np.float32)
    col = (t % GRID_W).astype(np.float32)
    nf = rot_dim // 4
    inv = (10000.0 ** (-np.arange(nf, dtype=np.float32) / nf)).astype(np.float32)
    ar = row[:, None] * inv
    ac = col[:, None] * inv
    C = np.concatenate([np.cos(ar), np.cos(ar), np.cos(ac), np.cos(ac)], axis=1)
    Sg = np.concatenate([-np.sin(ar), np.sin(ar), -np.sin(ac), np.sin(ac)], axis=1)
    return C.astype(np.float32), Sg.astype(np.float32)


def _na_plan():
    plan = []
    for qb in range(4):
        rows = range(8 * qb, 8 * qb + 8)
        rs = [min(max(r - 4, 0), 24) for r in rows]
        lo, hi = min(rs), max(rs) + 7
        plan.append(list(range(lo // 2, hi // 2 + 1)))
    return plan


NA_PLAN = _na_plan()
NA_NTILES = sum(len(p) for p in NA_PLAN)


_NA_IDX = None


def _na_idx():
    global _NA_IDX
    if _NA_IDX is None:
        kk = np.arange(128)[:, None]
        qq = np.arange(512)[None, :]
        idx = np.full((NA_NTILES, 128, 512), -1, np.int64)
        ti = 0
        for qb in range(4):
            for kc in NA_PLAN[qb]:
                kt = kc * 128 + kk
                kr, kcol = kt // 64, kt % 64
                qt = qb * 512 + qq
                r, c = qt // 64, qt % 64
                rs = np.clip(r - 4, 0, 24)
                cs = np.clip(c - 8, 0, 48)
                ok = (kr >= rs) & (kr < rs + 8) & (kcol >= cs) & (kcol < cs + 16)
                v = (kr - r + 7) * 31 + (kcol - c + 15)
                idx[ti] = np.where(ok, v, -1)
                ti += 1
        _NA_IDX = idx
    return _NA_IDX


NA_TILE_ID = []
_uid = 0
for _qb in range(4):
    if _qb == 2:
        NA_TILE_ID.append(list(NA_TILE_ID[1]))
        continue
    NA_TILE_ID.append(list(range(_uid, _uid + len(NA_PLAN[_qb]))))
    _uid += len(NA_PLAN[_qb])
NA_NUNIQ = _uid


def _na_uniq_idx():
    idx = _na_idx()
    out = np.zeros((NA_NUNIQ, 128, 512), np.int64)
    ti = 0
    for qb in range(4):
        for j in range(len(NA_PLAN[qb])):
            out[NA_TILE_ID[qb][j]] = idx[ti]
            ti += 1
    return out


class Arena:
    def __init__(self, t, n):
        self.t = t
        self.n = n
        self.off = 0

    def reset(self):
        self.off = 0

    def get(self, n, shape=None):
        n_al = (n + 15) // 16 * 16
        assert self.off + n_al <= self.n, ("arena overflow", self.off, n_al, self.n)
        v = self.t[:, self.off:self.off + n]
        self.off += n_al
        if shape is not None and len(shape) == 2:
            v = v.rearrange("p (a b) -> p a b", a=shape[0])
        elif shape is not None and len(shape) == 3:
            v = v.rearrange("p (a b c) -> p a b c", a=shape[0], b=shape[1])
        return v


def build(nl=DEPTH, dbg=None, nlw=DEPTH, ne=E):
    dbg = dbg or {}
    nc = bass.Bass("TRN2", target_bir_lowering=False)

    def din(name, shape, dt=F32):
        return nc.dram_tensor(name, list(shape), dt, kind="ExternalInput").ap()

    def dscr(name, shape, dt):
        kind = "ExternalOutput" if dbg.get(name) else "Internal"
        return nc.dram_tensor(name, list(shape), dt, kind=kind).ap()

    x_in = din("x", [S, D])
    ctx_in = din("ctx", [L, D])
    cT_in = din("cT", [128, KD * 2])
    w_mod = din("w_mod", [nlw, D, 6 * D])
    b_mod = din("b_mod", [nlw, 6 * D])
    b_modT = din("b_modT", [nlw, 128, 96])
    norm1T = din("norm1T", [nlw, 128, KD])
    norm2T = din("norm2T", [nlw, 128, KD])
    w_in = din("w_in", [nlw, D, INW])
    na_bias = din("na_bias", [nlw, 8, NA_NUNIQ, 128, 512])
    gains = {}
    for nm, n in (("na_q_norm", 64), ("na_k_norm", 64), ("mla_q_a_norm", 512), ("mla_kv_a_norm", 256),
                  ("mla_q_norm", 96), ("mla_k_norm", 96), ("gqa_q_norm", 64), ("gqa_k_norm", 64)):
        gains[nm] = din(nm, [nlw, n])
    w_q_b = din("mla_w_q_b", [nlw, 512, 768])
    w_kv_b = din("mla_w_kv_b", [nlw, 256, 1024])
    w_br = [din("w_branch_a", [nlw, 512, D]), din("w_branch_b", [nlw, 512, D]), din("w_branch_c", [nlw, 512, D])]
    w_out = din("w_out", [nlw, D, D])
    w_router = din("w_router", [nlw, D, E])
    w_eg = din("w_expert_gate", [nlw, ne, D, FF])
    w_eu = din("w_expert_up", [nlw, ne, D, FF])
    w_ed = din("w_expert_down", [nlw, ne, FF, D])
    rc64 = din("ropeC64", [S, 64])
    rs64 = din("ropeS64", [S, 64])
    rc32 = din("ropeC32", [S, 32])
    rs32 = din("ropeS32", [S, 32])
    ident_in = din("ident", [128, 128])
    iota_in = din("iota", [128, 256])
    sel64_in = din("sel64", [128, 128])
    out = nc.dram_tensor("out", [S, D], F32, kind="ExternalOutput").ap()

    X = dscr("X", [T, D], F32)
    QaT = dscr("QaT", [512, T], BF16)
    KaT = dscr("KaT", [512, T], BF16)
    QcT = dscr("QcT", [512, T], BF16)
    KcT = dscr("KcT", [128, T], BF16)
    QbT = dscr("QbT", [8, 96, T], BF16)
    KbT = dscr("KbT", [8, 96, T], BF16)
    Va = dscr("Va", [T, 512], BF16)
    Vb = dscr("Vb", [T, 512], BF16)
    Vc = dscr("Vc", [T, 128], BF16)
    GT = dscr("GT", [3 * D, T], BF16)
    OT = dscr("OT", [3, 512, T], BF16)
    YT = dscr("YT", [D, T], BF16)
    GREP = dscr("GREP", [2, 2, 128, D], F32)
    HID = dscr("HID", [E, 128, 8, 288], BF16)
    SWTL = dscr("SWTL", [16, 128, E, 2, 128], BF16)
    SWTC = dscr("SWTC", [E, 32, L], BF16)

    es = ExitStack()
    abf_t = es.enter_context(nc.sbuf_tensor("abf", [128, 68 * 1024], BF16))
    af_t = es.enter_context(nc.sbuf_tensor("af", [128, 11 * 1024], F32))
    cst_t = es.enter_context(nc.sbuf_tensor("cst", [128, 4 * 1024], F32))
    pps = [es.enter_context(nc.psum_tensor("pp%d" % i, [128, 1024], F32)) for i in range(4)]
    sch = Sch(nc, es)
    ABF = Arena(abf_t, 68 * 1024)
    AFF = Arena(af_t, 11 * 1024)
    CST = Arena(cst_t, 4 * 1024)

    def bank(b):
        return pps[b // 2][:, (b % 2) * 512:(b % 2 + 1) * 512]

    def bank_bf(b):
        return bank(b).bitcast(BF16)

    ptrk = [Trk() for _ in range(8)]

    def barrier():
        toks = []
        for f in ENGS:
            if sch.csem[f] is not None:
                toks.append((sch.csem[f], sch.cnt[f]))
        for q in ("sp", "pool", "act"):
            for tk in sch.dtok[q]:
                if tk is not None:
                    toks.append(tk)
        for e in ENGS:
            waits = {}
            for si, v in toks:
                sch._need(e, si, v, waits)
            for si, v in waits.items():
                sch.waited[e][si] = v
            if waits:
                sch.stream[e].append((list(waits.items()), None, None, 0))

    def new_phase():
        barrier()
        ABF.reset()
        AFF.reset()

    ident_f = CST.get(128)
    iota_f = CST.get(256)
    sel64_f = CST.get(128)
    ident_bf_t = es.enter_context(nc.sbuf_tensor("identbf", [128, 128], BF16))
    ident_bf = ident_bf_t[:]
    rC64 = CST.get(16 * 64, (16, 64))
    rS64 = CST.get(16 * 64, (16, 64))
    rC32 = CST.get(16 * 32, (16, 32))
    rS32 = CST.get(16 * 32, (16, 32))
    scT_f = CST.get(32)
    modc = CST.get(4 * 32, (4, 32))
    modT = CST.get(192, (96, 2))
    bmT = CST.get(96)
    n1T = CST.get(16)
    n2T = CST.get(16)
    cT_sb = CST.get(32)
    scT_bf_t = es.enter_context(nc.sbuf_tensor("scTbf", [128, 32], BF16))
    scRep_t = es.enter_context(nc.sbuf_tensor("scRep", [128, 32 * 128], BF16))
    scT_bf = scT_bf_t[:]
    scRep = scRep_t[:].rearrange("p (a b) -> p a b", a=32)
    k_cst = Trk()

    def ld(q, out_ap, in_ap, r=(), w=()):
        return sch.dma(q, lambda e, o=out_ap, i=in_ap: e.dma_start(out=o, in_=i), r=r, w=w)

    ld("sp", ident_f, ident_in, w=[k_cst])
    ld("sp", iota_f, iota_in, w=[k_cst])
    ld("sp", sel64_f, sel64_in, w=[k_cst])
    ld("sp", cT_sb, cT_in, w=[k_cst])
    for (dst, src, n) in ((rC64, rc64, 64), (rS64, rs64, 64), (rC32, rc32, 32), (rS32, rs32, 32)):
        ld("sp", dst, src.rearrange("(t p) d -> p t d", p=128), w=[k_cst])
    sch.op("dve", lambda e: e.tensor_copy(out=ident_bf, in_=ident_f), r=[k_cst], w=[k_cst])
    sch.op("act", lambda e: e.activation(out=scT_f, in_=cT_sb, func=AF.Silu), r=[k_cst], w=[k_cst])
    sch.op("dve", lambda e: e.tensor_copy(out=scT_bf, in_=scT_f), r=[k_cst], w=[k_cst])
    sch.op("dve", lambda e: e.tensor_copy(out=scRep, in_=scT_f.unsqueeze(2).to_broadcast([128, 32, 128])),
           r=[k_cst], w=[k_cst])

    k_X = [Trk() for _ in range(NT)]
    ld("sp", X[0:L, :], ctx_in, w=k_X[0:2])
    for j in range(4):
        ld("sp", X[L + j * 512:L + (j + 1) * 512, :], x_in[j * 512:(j + 1) * 512, :], w=k_X[2 + 4 * j:6 + 4 * j])

    k_scr = {n: Trk() for n in ("QaT", "KaT", "QcT", "KcT", "QbT", "KbT", "Va", "Vb", "Vc", "GT", "OT", "YT",
                                "GREP", "HID", "SWTL", "SWTC")}

    def kind_of(t):
        return 1 if t < 2 else 0

    for li in range(nl):
        last = (li == DEPTH - 1) or bool(dbg.get("force_last"))

        new_phase()
        k_mod = Trk()
        ld("sp", bmT, b_modT[li], w=[k_mod])
        ld("sp", n1T, norm1T[li], w=[k_mod])
        ld("sp", n2T, norm2T[li], w=[k_mod])
        wm = [ABF.get(16 * 512, (16, 512)) for _ in range(2)]
        k_wm = [Trk(), Trk()]
        bmr = [AFF.get(512) for _ in range(2)]
        k_bmr = [Trk(), Trk()]
        grs = [AFF.get(512) for _ in range(2)]
        k_grs = [Trk(), Trk()]
        wmv = w_mod[li].rearrange("(k p) c -> p k c", p=128)
        psA = bank(0)[:, 0:192].rearrange("p (a b) -> p a b", a=96)
        ngr = 0
        for j in range(24):
            b = j % 2
            ld("pool", wm[b], wmv[:, :, j * 512:(j + 1) * 512], w=[k_wm[b]])
            for q in range(4):
                cc = j * 4 + q
                for k in range(KD):
                    sch.op("pe", lambda e, o=psA[:, cc, :], l=wm[b][:, k, q * 128:(q + 1) * 128],
                           r_=scT_bf[:, 2 * k:2 * k + 2], st=(k == 0), sp_=(k == KD - 1):
                           e.matmul(o, l, r_, start=st, stop=sp_),
                           r=[k_wm[b], k_cst], w=[ptrk[0]])
            which = {8: 0, 9: 0, 10: 0, 11: 0, 20: 1, 21: 1, 22: 1, 23: 1}.get(j)
            if which is not None:
                cb = (j - 8) if which == 0 else (j - 20)
                g0 = 2 * D if which == 0 else 5 * D
                for kind in range(2):
                    pb_ = 2 + (ngr % 4)
                    for k in range(KD):
                        sch.op("pe", lambda e, o=bank(pb_), l=scRep[:, 2 * k + kind, :], r_=wm[b][:, k, :],
                               st=(k == 0), sp_=(k == KD - 1): e.matmul(o, l, r_, start=st, stop=sp_),
                               r=[k_wm[b], k_cst], w=[ptrk[pb_]])
                    sb = ngr % 2
                    ld("sp", bmr[sb], b_mod[li, g0 + cb * 512:g0 + (cb + 1) * 512].partition_broadcast(128),
                       w=[k_bmr[sb]])
                    sch.op("dve", lambda e, o=grs[sb], a=bank(pb_), b_=bmr[sb]:
                           e.tensor_tensor(out=o, in0=a, in1=b_, op=ALU.add),
                           r=[ptrk[pb_], k_bmr[sb]], w=[k_grs[sb]])
                    ld("sp", GREP[which, kind, :, cb * 512:(cb + 1) * 512], grs[sb], r=[k_grs[sb]],
                       w=[k_scr["GREP"]])
                    ngr += 1
        sch.op("dve", lambda e: e.tensor_tensor(out=modT, in0=psA,
                                                in1=bmT.unsqueeze(2).to_broadcast([128, 96, 2]), op=ALU.add),
               r=[ptrk[0], k_mod], w=[k_mod])
        mc = modc.rearrange("p a (k c) -> p a k c", c=2)
        for (dst, sc_lo, sh_lo, nT) in ((0, 16, 0, n1T), (2, 64, 48, n2T)):
            sch.op("dve", lambda e, o=mc[:, dst], a=modT[:, sc_lo:sc_lo + 16, :], n_=nT:
                   e.scalar_tensor_tensor(out=o, in0=a, scalar=1.0,
                                          in1=n_.unsqueeze(2).to_broadcast([128, 16, 2]),
                                          op0=ALU.add, op1=ALU.mult),
                   r=[k_mod], w=[k_mod])
            sch.op("dve", lambda e, o=mc[:, dst + 1], a=modT[:, sh_lo:sh_lo + 16, :]:
                   e.tensor_copy(out=o, in_=a), r=[k_mod], w=[k_mod])

        def modcol(j, k, kind):
            return modc[:, j, 2 * k + kind:2 * k + kind + 1]

        new_phase()
        hT = ABF.get(KD * T, (KD, T))
        k_hT = [Trk() for _ in range(NT)]
        xb = [AFF.get(D) for _ in range(2)]
        k_xb = [Trk(), Trk()]
        scr6k = ABF.get(3 * D)
        junk = scr6k[:, 0:D]
        k_junk = Trk()
        xn = [scr6k[:, D:2 * D], scr6k[:, 2 * D:3 * D]]
        k_xn = [Trk(), Trk()]
        st_f = AFF.get(4 * NT, (4, NT))
        k_st = Trk()

        def norm_tile(t, xbuf, kx, xnbuf, kxn):
            sch.op("act", lambda e, o=junk, i=xbuf, a=st_f[:, 0, t:t + 1]:
                   e.activation(out=o, in_=i, func=AF.Square, accum_out=a), r=[kx], w=[k_junk, k_st])
            sch.op("act", lambda e, o=st_f[:, 1, t:t + 1], i=st_f[:, 0, t:t + 1]:
                   e.activation(out=o, in_=i, func=AF.Sqrt, scale=1.0 / D, bias=EPS), r=[k_st], w=[k_st])
            sch.op("dve", lambda e, o=st_f[:, 2, t:t + 1], i=st_f[:, 1, t:t + 1]: e.reciprocal(out=o, in_=i),
                   r=[k_st], w=[k_st])
            sch.op("dve", lambda e, o=xnbuf, i=xbuf, s_=st_f[:, 2, t:t + 1]:
                   e.tensor_scalar(out=o, in0=i, scalar1=s_, scalar2=None, op0=ALU.mult),
                   r=[kx, k_st], w=[kxn])

        def transpose_mod(t, xnbuf, kxn, dst_fn, kdst, gj):
            kind = kind_of(t)
            for g in range(4):
                pb_ = 4 + g
                for q in range(4):
                    k = g * 4 + q
                    sch.op("pe", lambda e, o=bank_bf(pb_)[:, q * 128:(q + 1) * 128],
                           i=xnbuf[:, k * 128:(k + 1) * 128]: e.transpose(out=o, in_=i, identity=ident_bf),
                           r=[kxn, k_cst], w=[ptrk[pb_]])
                for q in range(4):
                    k = g * 4 + q
                    if k % 2 == 0:
                        sch.op("act", lambda e, o=dst_fn(k), i=bank_bf(pb_)[:, q * 128:(q + 1) * 128],
                               sc_=modcol(gj, k, kind), bi_=modcol(gj + 1, k, kind):
                               e.activation(out=o, in_=i, func=AF.Identity, scale=sc_, bias=bi_),
                               r=[ptrk[pb_], k_mod], w=[kdst])
                    else:
                        sch.op("dve", lambda e, o=dst_fn(k), i=bank_bf(pb_)[:, q * 128:(q + 1) * 128],
                               sc_=modcol(gj, k, kind), bi_=modcol(gj + 1, k, kind):
                               e.tensor_scalar(out=o, in0=i, scalar1=sc_, scalar2=bi_, op0=ALU.mult, op1=ALU.add),
                               r=[ptrk[pb_], k_mod], w=[kdst])

        for t in range(NT):
            b = t % 2
            ld("pool", xb[b], X[t * 128:(t + 1) * 128, :], r=[k_X[t]], w=[k_xb[b]])
            norm_tile(t, xb[b], k_xb[b], xn[b], k_xn[b])
            transpose_mod(t, xn[b], k_xn[b], lambda k, t=t: hT[:, k, t * 128:(t + 1) * 128], k_hT[t], 0)
        if dbg.get("stopB"):
            break

        barrier()
        winv = w_in[li].rearrange("(k p) c -> p k c", p=128)
        gn = {}
        k_gn = Trk()
        for nm, n in (("na_q_norm", 64), ("na_k_norm", 64), ("mla_q_a_norm", 512), ("mla_kv_a_norm", 256),
                      ("mla_q_norm", 96), ("mla_k_norm", 96), ("gqa_q_norm", 64), ("gqa_k_norm", 64)):
            gn[nm] = AFF.get(n)
            ld("sp", gn[nm], gains[nm][li].partition_broadcast(128), w=[k_gn])
        sch.op("dve", lambda e, o=gn["na_q_norm"]: e.tensor_scalar(out=o, in0=o, scalar1=0.125, scalar2=None,
                                                                   op0=ALU.mult), r=[k_gn], w=[k_gn])
        wblk = [ABF.get(KD * 512, (KD, 512)) for _ in range(2)]
        k_wblk = [Trk(), Trk()]
        wqb = ABF.get(4 * 768, (4, 768))
        wkvb = ABF.get(2 * 1024, (2, 1024))
        k_w2 = Trk()
        ld("pool", wqb, w_q_b[li].rearrange("(k p) c -> p k c", p=128), w=[k_w2])
        ld("pool", wkvb, w_kv_b[li].rearrange("(k p) c -> p k c", p=128), w=[k_w2])
        sq = [AFF.get(1024) for _ in range(2)]
        k_sq = [Trk(), Trk()]
        o1 = [AFF.get(768) for _ in range(2)]
        k_o1 = [Trk(), Trk()]
        sm = AFF.get(64, (4, 16))
        k_sm = Trk()
        kpe_f = AFF.get(32)
        k_kpe = Trk()
        nb = [ABF.get(1024) for _ in range(2)]
        k_nb = [Trk(), Trk()]
        stg = [ABF.get(1024) for _ in range(2)]
        k_stg = [Trk(), Trk()]
        cqT = ABF.get(6 * 128, (6, 128))
        k_cqT = Trk()
        wctr = [0]
        uctr = [0]
        AFF_tmp = [AFF.get(768) for _ in range(2)]
        k_tmp = [Trk(), Trk()]

        def load_w(c0, n):
            b = wctr[0] % 2
            wctr[0] += 1
            ld("pool", wblk[b][:, :, 0:n], winv[:, :, c0:c0 + n], w=[k_wblk[b]])
            return wblk[b], k_wblk[b]

        def proj(t, wb, kwb, c0, n, pb_):
            for k in range(KD):
                sch.op("pe", lambda e, o=bank(pb_)[:, 0:n], l=hT[:, k, t * 128:(t + 1) * 128],
                       r_=wb[:, k, c0:c0 + n], st=(k == 0), sp_=(k == KD - 1):
                       e.matmul(o, l, r_, start=st, stop=sp_), r=[k_hT[t], kwb], w=[ptrk[pb_]])

        def normhead(src, ksrc, nh, hd, gain, dst, kdst):
            n = nh * hd
            u = uctr[0] % 2
            uctr[0] += 1
            sch.op("act", lambda e, o=sq[u][:, 0:n], i=src: e.activation(out=o, in_=i, func=AF.Square),
                   r=ksrc, w=[k_sq[u]])
            sch.op("dve", lambda e, o=sm[:, 0, 0:nh], i=sq[u][:, 0:n].rearrange("p (h d) -> p h d", h=nh):
                   e.tensor_reduce(out=o, in_=i, axis=AX.X, op=ALU.add), r=[k_sq[u]], w=[k_sm])
            sch.op("act", lambda e, o=sm[:, 1, 0:nh], i=sm[:, 0, 0:nh]:
                   e.activation(out=o, in_=i, func=AF.Sqrt, scale=1.0 / hd, bias=EPS), r=[k_sm], w=[k_sm])
            sch.op("dve", lambda e, o=sm[:, 2, 0:nh], i=sm[:, 1, 0:nh]: e.reciprocal(out=o, in_=i),
                   r=[k_sm], w=[k_sm])
            sch.op("dve", lambda e, o=sq[u][:, 0:n].rearrange("p (h d) -> p h d", h=nh),
                   i=src.rearrange("p (h d) -> p h d", h=nh),
                   s_=sm[:, 2, 0:nh].unsqueeze(2).to_broadcast([128, nh, hd]):
                   e.tensor_tensor(out=o, in0=i, in1=s_, op=ALU.mult), r=list(ksrc) + [k_sm, k_sq[u]], w=[k_sq[u]])
            sch.op("dve", lambda e, o=dst.rearrange("p (h d) -> p h d", h=nh),
                   i=sq[u][:, 0:n].rearrange("p (h d) -> p h d", h=nh),
                   g_=gain.unsqueeze(1).to_broadcast([128, nh, hd]):
                   e.tensor_tensor(out=o, in0=i, in1=g_, op=ALU.mult), r=[k_sq[u], k_gn], w=kdst)

        def rope(t, buf, kbuf, nh, hd, r0, R, tC, tS, dst, kdst):
            tt = t - 2
            m = R // 4
            u = uctr[0] % 2
            uctr[0] += 1
            bv = buf.rearrange("p (h d) -> p h d", h=nh)
            xs = sq[u][:, 0:nh * R].rearrange("p (h s a m) -> p h s a m", h=nh, s=2, a=2)
            xin = bv[:, :, r0:r0 + R].rearrange("p h (s a m) -> p h s a m", s=2, a=2)
            Sv = tS[:, tt, :].rearrange("p (s a m) -> p s a m", s=2, a=2)
            for a in range(2):
                for s_ in range(2):
                    sch.op("dve", lambda e, o=xs[:, :, s_, a, :], i=xin[:, :, s_, 1 - a, :],
                           g_=Sv[:, s_, a, :].unsqueeze(1).to_broadcast([128, nh, m]):
                           e.tensor_tensor(out=o, in0=i, in1=g_, op=ALU.mult),
                           r=[kbuf, k_cst, k_sq[u]], w=[k_sq[u]])
            t1 = o1[u][:, 0:nh * R].rearrange("p (h r) -> p h r", h=nh)
            sch.op("dve", lambda e, o=t1, i=bv[:, :, r0:r0 + R],
                   g_=tC[:, tt, :].unsqueeze(1).to_broadcast([128, nh, R]):
                   e.tensor_tensor(out=o, in0=i, in1=g_, op=ALU.mult), r=[kbuf, k_cst], w=[k_o1[u]])
            dv = dst.rearrange("p (h d) -> p h d", h=nh)
            sch.op("dve", lambda e, o=dv[:, :, r0:r0 + R], i=t1,
                   x_=sq[u][:, 0:nh * R].rearrange("p (h r) -> p h r", h=nh):
                   e.tensor_tensor(out=o, in0=i, in1=x_, op=ALU.add), r=[k_o1[u], k_sq[u]], w=kdst)
            if r0 > 0:
                sch.op("dve", lambda e, o=dv[:, :, 0:r0], i=bv[:, :, 0:r0]: e.tensor_copy(out=o, in_=i),
                       r=[kbuf], w=kdst)

        def transpose_out(t, src, ksrc, ncol_blocks, rows, dst_dram, kd):
            s = uctr[0] % 2
            uctr[0] += 1
            pb_ = 6 + s
            for q in range(ncol_blocks):
                sch.op("pe", lambda e, o=bank_bf(pb_)[0:rows, q * 128:(q + 1) * 128],
                       i=src[:, q * rows:(q + 1) * rows]: e.transpose(out=o, in_=i, identity=ident_bf),
                       r=[ksrc, k_cst], w=[ptrk[pb_]])
            n = ncol_blocks * 128
            sch.op("act", lambda e, o=stg[s][0:rows, 0:n], i=bank_bf(pb_)[0:rows, 0:n]: e.copy(out=o, in_=i),
                   r=[ptrk[pb_]], w=[k_stg[s]])
            ld("sp", dst_dram, stg[s][0:rows, 0:n].rearrange("p (q c) -> p q c", q=ncol_blocks),
               r=[k_stg[s]], w=[kd])

        def qk_group(c0, nh, gain, dst_dram_fn, kd, do_rope):
            wb, kwb = load_w(c0, nh * 64)
            for t in range(NT):
                pb_ = t % 4
                proj(t, wb, kwb, 0, nh * 64, pb_)
                u = t % 2
                if do_rope and t >= 2:
                    normhead(bank(pb_)[:, 0:nh * 64], [ptrk[pb_]], nh, 64, gain, AFF_tmp[u][:, 0:nh * 64],
                             [k_tmp[u]])
                    rope(t, AFF_tmp[u][:, 0:nh * 64], k_tmp[u], nh, 64, 0, 64, rC64, rS64,
                         nb[u][:, 0:nh * 64], [k_nb[u]])
                else:
                    normhead(bank(pb_)[:, 0:nh * 64], [ptrk[pb_]], nh, 64, gain, nb[u][:, 0:nh * 64], [k_nb[u]])
                nblk = max(1, nh * 64 // 128)
                transpose_out(t, nb[u], k_nb[u], nblk, 128 if nh >= 2 else 64, dst_dram_fn(t), kd)

        qk_group(0, 8, gn["na_q_norm"],
                 lambda t: QaT.rearrange("(q p) t -> p q t", p=128)[:, :, t * 128:(t + 1) * 128], k_scr["QaT"], False)
        qk_group(512, 8, gn["na_k_norm"],
                 lambda t: KaT.rearrange("(q p) t -> p q t", p=128)[:, :, t * 128:(t + 1) * 128], k_scr["KaT"], False)
        if dbg.get("stopC1"):
            break
        qk_group(2336, 8, gn["gqa_q_norm"],
                 lambda t: QcT.rearrange("(q p) t -> p q t", p=128)[:, :, t * 128:(t + 1) * 128], k_scr["QcT"], True)
        if dbg.get("stopC2"):
            break
        wb, kwb = load_w(2848, 256)
        for t in range(NT):
            pb_ = t % 4
            u = t % 2
            proj(t, wb, kwb, 0, 256, pb_)
            if t >= 2:
                normhead(bank(pb_)[:, 0:128], [ptrk[pb_]], 2, 64, gn["gqa_k_norm"], AFF_tmp[u][:, 0:128],
                         [k_tmp[u]])
                rope(t, AFF_tmp[u][:, 0:128], k_tmp[u], 2, 64, 0, 64, rC64, rS64, nb[u][:, 0:128], [k_nb[u]])
            else:
                normhead(bank(pb_)[:, 0:128], [ptrk[pb_]], 2, 64, gn["gqa_k_norm"], nb[u][:, 0:128], [k_nb[u]])
            sch.op("act", lambda e, o=nb[u][:, 128:256], i=bank(pb_)[:, 128:256]: e.copy(out=o, in_=i),
                   r=[ptrk[pb_]], w=[k_nb[u]])
            ld("sp", Vc[t * 128:(t + 1) * 128, :], nb[u][:, 128:256], r=[k_nb[u]], w=[k_scr["Vc"]])
            transpose_out(t, nb[u], k_nb[u], 1, 128,
                          KcT.rearrange("(q p) t -> p q t", p=128)[:, :, t * 128:(t + 1) * 128], k_scr["KcT"])
        wb, kwb = load_w(1024, 512)
        for t in range(NT):
            pb_ = t % 4
            u = t % 2
            proj(t, wb, kwb, 0, 512, pb_)
            sch.op("act", lambda e, o=nb[u][:, 0:512], i=bank(pb_): e.copy(out=o, in_=i),
                   r=[ptrk[pb_]], w=[k_nb[u]])
            ld("sp", Va[t * 128:(t + 1) * 128, :], nb[u][:, 0:512], r=[k_nb[u]], w=[k_scr["Va"]])

        if dbg.get("stopC3"):
            break
        wcq, k_wcq = load_w(1536, 512)
        wckv, k_wckv = load_w(2048, 288)
        QbTv = QbT.rearrange("h d t -> d h t")
        KbTv = KbT.rearrange("h d t -> d h t")
        for t in range(NT):
            lat = t >= 2
            proj(t, wcq, k_wcq, 0, 512, 0)
            proj(t, wckv, k_wckv, 0, 288, 1)
            normhead(bank(0), [ptrk[0]], 1, 512, gn["mla_q_a_norm"], nb[0][:, 0:512], [k_nb[0]])
            normhead(bank(1)[:, 0:256], [ptrk[1]], 1, 256, gn["mla_kv_a_norm"], nb[0][:, 512:768], [k_nb[0]])
            sch.op("act", lambda e, o=kpe_f, i=bank(1)[:, 256:288]: e.copy(out=o, in_=i), r=[ptrk[1]], w=[k_kpe])
            for q in range(6):
                sch.op("pe", lambda e, o=bank_bf(6)[:, q * 128:(q + 1) * 128], i=nb[0][:, q * 128:(q + 1) * 128]:
                       e.transpose(out=o, in_=i, identity=ident_bf), r=[k_nb[0], k_cst], w=[ptrk[6]])
            sch.op("act", lambda e, o=cqT.rearrange("p a b -> p (a b)"), i=bank_bf(6)[:, 0:768]: e.copy(out=o, in_=i),
                   r=[ptrk[6]], w=[k_cqT])
            if dbg.get("mla_stop") == 1:
                continue
            for (cb, n, pb_) in ((0, 512, 2), (512, 256, 3)):
                for k in range(4):
                    sch.op("pe", lambda e, o=bank(pb_)[:, 0:n], l=cqT[:, k, :], r_=wqb[:, k, cb:cb + n],
                           st=(k == 0), sp_=(k == 3): e.matmul(o, l, r_, start=st, stop=sp_),
                           r=[k_cqT, k_w2], w=[ptrk[pb_]])
            normhead(pps[1][:, 0:768], [ptrk[2], ptrk[3]], 8, 96, gn["mla_q_norm"], AFF_tmp[0][:, 0:768], [k_tmp[0]])
            if lat:
                rope(t, AFF_tmp[0][:, 0:768], k_tmp[0], 8, 96, 64, 32, rC32, rS32, nb[0][:, 0:768], [k_nb[0]])
            else:
                sch.op("dve", lambda e, o=nb[0][:, 0:768], i=AFF_tmp[0][:, 0:768]: e.tensor_copy(out=o, in_=i),
                       r=[k_tmp[0]], w=[k_nb[0]])
            transpose_out(t, nb[0], k_nb[0], 8, 96, QbTv[:, :, t * 128:(t + 1) * 128], k_scr["QbT"])
            if dbg.get("mla_stop") == 2:
                continue
            for (cb, pb_) in ((0, 4), (512, 5)):
                for k in range(2):
                    sch.op("pe", lambda e, o=bank(pb_), l=cqT[:, 4 + k, :], r_=wkvb[:, k, cb:cb + 512],
                           st=(k == 0), sp_=(k == 1): e.matmul(o, l, r_, start=st, stop=sp_),
                           r=[k_cqT, k_w2], w=[ptrk[pb_]])
            kbf = AFF_tmp[1][:, 0:768].rearrange("p (h d) -> p h d", h=8)
            if dbg.get("mla_stop") == 31:
                continue
            for hb2 in range(2):
                kvb = bank(4 + hb2).rearrange("p (h d) -> p h d", h=4)
                sch.op("dve", lambda e, o=nb[1][:, hb2 * 256:(hb2 + 1) * 256].rearrange("p (h d) -> p h d", h=4),
                       i=kvb[:, :, 64:128]: e.tensor_copy(out=o, in_=i), r=[ptrk[4 + hb2]], w=[k_nb[1]])
                if dbg.get("mla_stop") == 32:
                    continue
                sch.op("dve", lambda e, o=kbf[:, hb2 * 4:(hb2 + 1) * 4, 0:64], i=kvb[:, :, 0:64]:
                       e.tensor_copy(out=o, in_=i), r=[ptrk[4 + hb2]], w=[k_tmp[1]])
            if dbg.get("mla_stop") in (32, 33):
                continue
            ld("sp", Vb[t * 128:(t + 1) * 128, :], nb[1][:, 0:512], r=[k_nb[1]], w=[k_scr["Vb"]])
            sch.op("dve", lambda e, o=kbf[:, :, 64:96], i=kpe_f.unsqueeze(1).to_broadcast([128, 8, 32]):
                   e.tensor_copy(out=o, in_=i), r=[k_kpe], w=[k_tmp[1]])
            if dbg.get("mla_stop") == 3:
                continue
            normhead(AFF_tmp[1][:, 0:768], [k_tmp[1]], 8, 96, gn["mla_k_norm"], AFF_tmp[1][:, 0:768], [k_tmp[1]])
            if dbg.get("mla_stop") == 4:
                continue
            if lat:
                rope(t, AFF_tmp[1][:, 0:768], k_tmp[1], 8, 96, 64, 32, rC32, rS32, nb[1][:, 0:768], [k_nb[1]])
            else:
                sch.op("dve", lambda e, o=nb[1][:, 0:768], i=AFF_tmp[1][:, 0:768]: e.tensor_copy(out=o, in_=i),
                       r=[k_tmp[1]], w=[k_nb[1]])
            transpose_out(t, nb[1], k_nb[1], 8, 96, KbTv[:, :, t * 128:(t + 1) * 128], k_scr["KbT"])

        if dbg.get("stopC4"):
            break
        barrier()
        gst = [scr6k[:, 0:T], scr6k[:, T:2 * T]]
        k_gst = [Trk(), Trk()]
        tblocks = [(0, 256)] + [(L + 512 * j, 512) for j in range(4)]
        if last:
            tblocks = tblocks[1:]
        t_lo = tblocks[0][0]
        gctr = 0
        for blk in range(12):
            wb, kwb = load_w(3104 + blk * 512, 512)
            for q in range(4):
                gc = blk * 4 + q
                sg = gc % 2
                for (q0, nq) in tblocks:
                    pb_ = gctr % 4
                    gctr += 1
                    tl = list(range(q0 // 128, (q0 + nq) // 128))
                    for k in range(KD):
                        sch.op("pe", lambda e, o=bank(pb_)[:, 0:nq], l=wb[:, k, q * 128:(q + 1) * 128],
                               r_=hT[:, k, q0:q0 + nq], st=(k == 0), sp_=(k == KD - 1):
                               e.matmul(o, l, r_, start=st, stop=sp_),
                               r=[kwb] + [k_hT[t] for t in tl], w=[ptrk[pb_]])
                    sch.op("act", lambda e, o=gst[sg][:, q0:q0 + nq], i=bank(pb_)[:, 0:nq]:
                           e.activation(out=o, in_=i, func=AF.Sigmoid), r=[ptrk[pb_]], w=[k_gst[sg]])
                ld("sp", GT[gc * 128:(gc + 1) * 128, t_lo:T], gst[sg][:, t_lo:T], r=[k_gst[sg]], w=[k_scr["GT"]])
        if dbg.get("stopC"):
            break
        new_phase()
        qt = [ABF.get(T) for _ in range(2)]
        kt = [ABF.get(T) for _ in range(2)]
        vt = [ABF.get(NT * 128, (NT, 128)) for _ in range(2)]
        k_q = [Trk(), Trk()]
        k_k = [Trk(), Trk()]
        k_v = [Trk(), Trk()]
        pT = [ABF.get(512) for _ in range(5)]
        k_pT = [Trk() for _ in range(5)]
        pending = [None]
        bias_bfs = [ABF.get(NA_NUNIQ * 512, (NA_NUNIQ, 512)) for _ in range(2)]
        k_biass = [Trk(), Trk()]
        bstage = [AFF.get(512) for _ in range(2)]
        k_bst = [Trk(), Trk()]
        posb = [AFF.get(512) for _ in range(2)]
        lnb = [AFF.get(512) for _ in range(2)]
        rbb = [AFF.get(512) for _ in range(2)]
        k_fin = [Trk(), Trk()]
        oTb = [ABF.get(512) for _ in range(2)]
        k_oT = [Trk(), Trk()]
        for b in range(2):
            sch.op("dve", lambda e, o=vt[b]: e.memset(o, 0.0), w=[k_v[b]])
            sch.op("dve", lambda e, o=vt[b][:, :, 64:65]: e.memset(o, 1.0), w=[k_v[b]])
            sch.op("dve", lambda e, o=qt[b]: e.memset(o, 0.0), w=[k_q[b]])
            sch.op("dve", lambda e, o=kt[b]: e.memset(o, 0.0), w=[k_k[b]])
        jobs = []
        for h in range(8):
            jobs.append((0, h, QaT[h * 64:(h + 1) * 64, :], KaT[h * 64:(h + 1) * 64, :], Va[:, h * 64:(h + 1) * 64],
                         64, 1.0))
        for h in range(8):
            jobs.append((1, h, QbT[h], KbT[h], Vb[:, h * 64:(h + 1) * 64], 96, 96 ** -0.5))
        for h in range(8):
            g = h // 4
            jobs.append((2, h, QcT[h * 64:(h + 1) * 64, :], KcT[g * 64:(g + 1) * 64, :], Vc[:, g * 64:(g + 1) * 64],
                         64, 0.125))
        sctr = 0
        pctr = 0
        fctr = 0
        srcname = {0: ("QaT", "KaT", "Va"), 1: ("QbT", "KbT", "Vb"), 2: ("QcT", "KcT", "Vc")}
        for j, (mix, h, Qs, Ks, Vs, dk, scale) in enumerate(jobs):
            buf = j % 2
            nq_, nk_, nv_ = srcname[mix]
            sch.op("dve", lambda e, o=qt[buf][64:128, :]: e.memset(o, 0.0), w=[k_q[buf]])
            sch.op("dve", lambda e, o=kt[buf][64:128, :]: e.memset(o, 0.0), w=[k_k[buf]])
            ld("pool", qt[buf][0:dk, :], Qs, r=[k_scr[nq_]], w=[k_q[buf]])
            ld("pool", kt[buf][0:dk, :], Ks, r=[k_scr[nk_]], w=[k_k[buf]])
            ld("pool", vt[buf][:, :, 0:64], Vs.rearrange("(n p) d -> p n d", p=128), r=[k_scr[nv_]], w=[k_v[buf]])
            bias_bf = bias_bfs[h % 2]
            k_bias = k_biass[h % 2]
            if mix == 0:
                for tg in range(2):
                    ld("pool", bias_bf[:, tg * 10:(tg + 1) * 10, :],
                       na_bias[li, h, tg * 10:(tg + 1) * 10].rearrange("t p c -> p t c"), w=[k_bias])
            blocks = [(L + 512 * qb, 512, qb) for qb in range(4)]
            if not last:
                blocks.append((0, 256, None))
            LA = 3

            def finalize(fin):
                (q0f, nqf, s2f, pobf, mixf, hf) = fin
                pbb = 4 + s2f
                sch.op("dve", lambda e, o=posb[s2f][:, 0:nqf], i=bank(pobf)[:, 0:nqf]: e.tensor_copy(out=o, in_=i),
                       r=[ptrk[pobf]], w=[k_fin[s2f]])
                sch.op("pe", lambda e, o=bank(pbb)[:, 0:nqf], r_=posb[s2f][:, 0:nqf]:
                       e.matmul(o, sel64_f, r_, start=True, stop=True),
                       r=[k_fin[s2f], k_cst], w=[ptrk[pbb]])
                sch.op("dve", lambda e, o=rbb[s2f][0:64, 0:nqf], i=bank(pbb)[0:64, 0:nqf]: e.reciprocal(out=o, in_=i),
                       r=[ptrk[pbb]], w=[k_fin[s2f]])
                sch.op("dve", lambda e, o=oTb[s2f][0:64, 0:nqf], a=posb[s2f][0:64, 0:nqf], b_=rbb[s2f][0:64, 0:nqf]:
                       e.tensor_tensor(out=o, in0=a, in1=b_, op=ALU.mult), r=[k_fin[s2f]], w=[k_oT[s2f]])
                ld("sp", OT[mixf, hf * 64:(hf + 1) * 64, q0f:q0f + nqf], oTb[s2f][0:64, 0:nqf], r=[k_oT[s2f]],
                   w=[k_scr["OT"]])

            steps = []
            blkinfo = []
            for (q0, nq, qb) in blocks:
                if qb is None:
                    kch = [(0, None), (1, None)]
                elif mix == 0:
                    kch = [(0, None), (1, None)] + [(2 + kc, NA_TILE_ID[qb][jj]) for jj, kc in enumerate(NA_PLAN[qb])]
                else:
                    kch = [(kc, None) for kc in range(NT)]
                s2 = fctr % 2
                fctr += 1
                blkinfo.append((q0, nq, s2, 6 + s2, len(kch)))
                for idx, (kc, bi) in enumerate(kch):
                    steps.append((len(blkinfo) - 1, idx, kc, bi))
            ns = len(steps)
            pslots = [None] * ns
            pend_cnt = 0
            for g in range(ns + LA):
                if g < ns:
                    bix, idx, kc, bi = steps[g]
                    q0, nq, s2, pob, nk = blkinfo[bix]
                    psb = sctr % 4
                    sctr += 1
                    sch.op("pe", lambda e, o=bank(psb)[:, 0:nq], l=kt[buf][:, kc * 128:(kc + 1) * 128],
                           r_=qt[buf][:, q0:q0 + nq], sp_=(bi is None): e.matmul(o, l, r_, start=True, stop=sp_),
                           r=[k_k[buf], k_q[buf]], w=[ptrk[psb]])
                    if bi is not None:
                        sch.op("pe", lambda e, o=bank(psb)[:, 0:nq], r_=bias_bf[:, bi, 0:nq]:
                               e.matmul(o, ident_bf, r_, start=False, stop=True), r=[k_bias, k_cst], w=[ptrk[psb]])
                    p_ = pctr % 5
                    pctr += 1
                    pslots[g] = p_
                    sch.op("act", lambda e, o=pT[p_][:, 0:nq], i=bank(psb)[:, 0:nq], sc_=scale:
                           e.activation(out=o, in_=i, func=AF.Exp, scale=sc_), r=[ptrk[psb]], w=[k_pT[p_]])
                g2 = g - LA
                if g2 >= 0:
                    bix2, idx2, kc2, _ = steps[g2]
                    q02, nq2, s22, pob2, nk2 = blkinfo[bix2]
                    p2 = pslots[g2]
                    sch.op("pe", lambda e, o=bank(pob2)[:, 0:nq2], l=vt[buf][:, kc2, :], r_=pT[p2][:, 0:nq2],
                           st=(idx2 == 0), sp_=(idx2 == nk2 - 1): e.matmul(o, l, r_, start=st, stop=sp_),
                           r=[k_v[buf], k_pT[p2]], w=[ptrk[pob2]])
                    if pending[0] is not None:
                        if pend_cnt == 0:
                            finalize(pending[0])
                            pending[0] = None
                        else:
                            pend_cnt -= 1
                    if idx2 == nk2 - 1:
                        if pending[0] is not None:
                            finalize(pending[0])
                        pending[0] = (q02, nq2, s22, pob2, mix, h)
                        pend_cnt = 2
        if pending[0] is not None:
            finalize(pending[0])
            pending[0] = None
        if dbg.get("stopD"):
            break

        new_phase()
        wbr = [ABF.get(4 * D, (4, D)) for _ in range(3)]
        k_wbr = Trk()
        for br in range(3):
            ld("pool", wbr[br], w_br[br][li].rearrange("(k p) c -> p k c", p=128), w=[k_wbr])
        otb = [[ABF.get(4 * 512, (4, 512)) for _ in range(3)] for _ in range(2)]
        k_otb = [Trk(), Trk()]
        gtb = [[ABF.get(512) for _ in range(3)] for _ in range(2)]
        k_gtb = [Trk(), Trk()]
        ytb = [ABF.get(KD * 512, (KD, 512)) for _ in range(2)]
        k_ytb = [Trk(), Trk()]
        ef = [[AFF.get(512) for _ in range(3)] for _ in range(2)]
        k_ef = [Trk(), Trk()]
        OTv = OT.rearrange("m (k p) t -> m p k t", p=128)
        ectr = 0
        for bi_, (q0, nq) in enumerate(tblocks):
            ob = bi_ % 2
            for br in range(3):
                ld("sp", otb[ob][br][:, :, 0:nq], OTv[br][:, :, q0:q0 + nq], r=[k_scr["OT"]], w=[k_otb[ob]])
            for dc in range(KD):
                gb = ectr % 2
                ectr += 1
                for br in range(3):
                    ld("sp", gtb[gb][br][:, 0:nq], GT[br * D + dc * 128:br * D + (dc + 1) * 128, q0:q0 + nq],
                       r=[k_scr["GT"]], w=[k_gtb[gb]])
                pbs = [(3 * gb + br) for br in range(3)]
                for br in range(3):
                    for k in range(4):
                        sch.op("pe", lambda e, o=bank(pbs[br])[:, 0:nq], l=wbr[br][:, k, dc * 128:(dc + 1) * 128],
                               r_=otb[ob][br][:, k, 0:nq], st=(k == 0), sp_=(k == 3):
                               e.matmul(o, l, r_, start=st, stop=sp_), r=[k_wbr, k_otb[ob]], w=[ptrk[pbs[br]]])
                for br in range(3):
                    sch.op("dve", lambda e, o=ef[gb][br][:, 0:nq], a=bank(pbs[br])[:, 0:nq], b_=gtb[gb][br][:, 0:nq]:
                           e.tensor_tensor(out=o, in0=a, in1=b_, op=ALU.mult),
                           r=[ptrk[pbs[br]], k_gtb[gb]], w=[k_ef[gb]])
                sch.op("pool", lambda e, o=ef[gb][0][:, 0:nq], a=ef[gb][0][:, 0:nq], b_=ef[gb][1][:, 0:nq]:
                       e.tensor_tensor(out=o, in0=a, in1=b_, op=ALU.add), r=[k_ef[gb]], w=[k_ef[gb]])
                sch.op("pool", lambda e, o=ytb[ob][:, dc, 0:nq], a=ef[gb][0][:, 0:nq], b_=ef[gb][2][:, 0:nq]:
                       e.tensor_tensor(out=o, in0=a, in1=b_, op=ALU.add), r=[k_ef[gb]], w=[k_ytb[ob]])
            ld("sp", YT.rearrange("(k p) t -> p k t", p=128)[:, :, q0:q0 + nq], ytb[ob][:, :, 0:nq],
               r=[k_ytb[ob]], w=[k_scr["YT"]])

        new_phase()
        wo = ABF.get(KD * D, (KD, D))
        k_wo = Trk()
        for j4 in range(4):
            ld("pool", wo[:, :, j4 * 512:(j4 + 1) * 512],
               w_out[li].rearrange("(k p) c -> p k c", p=128)[:, :, j4 * 512:(j4 + 1) * 512], w=[k_wo])
        grep1 = AFF.get(2 * D, (2, D))
        k_gr = Trk()
        for kind in range(2):
            ld("sp", grep1[:, kind, :], GREP[0, kind], r=[k_scr["GREP"]], w=[k_gr])
        yti = [ABF.get(KD * 128, (KD, 128)) for _ in range(2)]
        k_yti = [Trk(), Trk()]
        xe2 = [AFF.get(D) for _ in range(2)]
        k_xe2 = [Trk(), Trk()]
        tmpf = [AFF.get(512) for _ in range(2)]
        k_tmpf = [Trk(), Trk()]
        YTv = YT.rearrange("(k p) t -> p k t", p=128)
        octr = 0
        for t in range(2 if last else 0, NT):
            b = t % 2
            kind = kind_of(t)
            ld("pool", yti[b], YTv[:, :, t * 128:(t + 1) * 128], r=[k_scr["YT"]], w=[k_yti[b]])
            ld("pool", xe2[b], X[t * 128:(t + 1) * 128, :], r=[k_X[t]], w=[k_xe2[b]])
            for j4 in range(4):
                pb_ = octr % 4
                tb_ = octr % 2
                octr += 1
                for k in range(KD):
                    sch.op("pe", lambda e, o=bank(pb_), l=yti[b][:, k, :], r_=wo[:, k, j4 * 512:(j4 + 1) * 512],
                           st=(k == 0), sp_=(k == KD - 1): e.matmul(o, l, r_, start=st, stop=sp_),
                           r=[k_yti[b], k_wo], w=[ptrk[pb_]])
                sch.op("dve", lambda e, o=tmpf[tb_], a=bank(pb_), g_=grep1[:, kind, j4 * 512:(j4 + 1) * 512]:
                       e.tensor_tensor(out=o, in0=a, in1=g_, op=ALU.mult), r=[ptrk[pb_], k_gr], w=[k_tmpf[tb_]])
                sch.op("dve", lambda e, o=xe2[b][:, j4 * 512:(j4 + 1) * 512], a=xe2[b][:, j4 * 512:(j4 + 1) * 512],
                       t_=tmpf[tb_]: e.tensor_tensor(out=o, in0=a, in1=t_, op=ALU.add),
                       r=[k_tmpf[tb_]], w=[k_xe2[b]])
            ld("sp", X[t * 128:(t + 1) * 128, :], xe2[b], r=[k_xe2[b]], w=[k_X[t]])
        if dbg.get("stopE"):
            break
        new_phase()
        xn2 = ABF.get(NT * D, (NT, D))
        k_xn2 = [Trk() for _ in range(NT)]
        wr = ABF.get(KD * E, (KD, E))
        k_wr = Trk()
        ld("pool", wr, w_router[li].rearrange("(k p) e -> p k e", p=128), w=[k_wr])
        sel = ABF.get(16 * 256, (16, 256))
        selc = ABF.get(2 * 32, (2, 32))
        selw = [ABF.get(256) for _ in range(2)]
        swst = ABF.get(2 * S, (2, S))
        swsc = ABF.get(256)
        xeT = ABF.get(KD * 288, (KD, 288))
        wgu_r = ABF.get(16384)
        hid1 = ABF.get(8 * 288, (8, 288))
        hid = [hid1, hid1]
        wgu = [[wgu_r[:, (2 * b2 + kk) * 4096:(2 * b2 + kk + 1) * 4096].rearrange("p (k f) -> p k f", k=KD)
                for kk in range(2)] for b2 in range(2)]
        junk = wgu_r[:, 0:D]
        k_junk = Trk()
        h2T = [wgu_r[:, D + b2 * 2048:D + (b2 + 1) * 2048].rearrange("p (k f) -> p k f", k=KD) for b2 in range(2)]
        k_h2T = [Trk(), Trk()]
        xreg = AFF.get(2 * T)
        xb = [xreg[:, 0:D], xreg[:, T:T + D]]
        k_xb = [Trk(), Trk()]
        st_f = AFF.get(4 * NT, (4, NT))
        k_st = Trk()
        aff_tm = AFF.get(NT * E, (NT, E))
        posm_tm = AFF.get(NT * E, (NT, E))
        affT = AFF.get(T)
        reg2 = AFF.get(T)
        rs_f = AFF.get(16, (4, 4))
        k_aff = Trk()
        k_affT = Trk()
        for t in range(NT):
            b = t % 2
            kind = kind_of(t)
            ld("pool", xb[b], X[t * 128:(t + 1) * 128, :], r=[k_X[t]], w=[k_xb[b]])
            norm_tile(t, xb[b], k_xb[b], xn2[:, t, :], k_xn2[t])
            transpose_mod(t, xn2[:, t, :], k_xn2[t], lambda k, b=b: h2T[b][:, k, :], k_h2T[b], 2)
            pb_ = t % 2
            for k in range(KD):
                sch.op("pe", lambda e, o=bank(pb_)[:, 0:E], l=h2T[b][:, k, :], r_=wr[:, k, :],
                       st=(k == 0), sp_=(k == KD - 1): e.matmul(o, l, r_, start=st, stop=sp_),
                       r=[k_h2T[b], k_wr], w=[ptrk[pb_]])
            sch.op("dve", lambda e, o=rs_f[:, 0, 0:1], i=bank(pb_)[:, 0:E]:
                   e.tensor_reduce(out=o, in_=i, axis=AX.X, op=ALU.max), r=[ptrk[pb_]], w=[k_aff])
            sch.op("dve", lambda e, o=rs_f[:, 1, 0:1], i=rs_f[:, 0, 0:1]:
                   e.tensor_scalar(out=o, in0=i, scalar1=-1.0, scalar2=None, op0=ALU.mult), r=[k_aff], w=[k_aff])
            sch.op("act", lambda e, o=aff_tm[:, t, :], i=bank(pb_)[:, 0:E], b_=rs_f[:, 1, 0:1], a=rs_f[:, 2, 0:1]:
                   e.activation(out=o, in_=i, func=AF.Exp, bias=b_, scale=1.0, accum_out=a),
                   r=[ptrk[pb_], k_aff], w=[k_aff])
            sch.op("dve", lambda e, o=rs_f[:, 3, 0:1], i=rs_f[:, 2, 0:1]: e.reciprocal(out=o, in_=i),
                   r=[k_aff], w=[k_aff])
            sch.op("dve", lambda e, o=aff_tm[:, t, :], i=aff_tm[:, t, :], s_=rs_f[:, 3, 0:1]:
                   e.tensor_scalar(out=o, in0=i, scalar1=s_, scalar2=None, op0=ALU.mult), r=[k_aff], w=[k_aff])
            pb2 = 2 + t % 2
            sch.op("pe", lambda e, o=bank(pb2)[0:E, 0:128], i=aff_tm[:, t, :]:
                   e.transpose(out=o, in_=i, identity=ident_f), r=[k_aff, k_cst], w=[ptrk[pb2]])
            sch.op("act", lambda e, o=affT[0:E, t * 128:(t + 1) * 128], i=bank(pb2)[0:E, 0:128]: e.copy(out=o, in_=i),
                   r=[ptrk[pb2]], w=[k_affT])

        barrier()
        work = xreg[:, 0:T]
        cs = xreg[:, T:2 * T]
        m8 = rs_f.rearrange("p a b -> p (a b)")[0:E, 0:8]
        thr = AFF.get(2)
        k_tk = Trk()
        sch.op("dve", lambda e: e.tensor_copy(out=work[0:E, 0:T], in_=affT[0:E, 0:T]), r=[k_affT], w=[k_tk])
        segs = [(L, T, CAP_LAT, 0)] if last else [(L, T, CAP_LAT, 0), (0, L, CAP_CTX, 1)]
        for (lo, hi, cap, col) in segs:
            for rnd in range(cap // 8):
                sch.op("dve", lambda e, i=work[0:E, lo:hi]: e.max(out=m8, in_=i), r=[k_tk], w=[k_tk])
                if rnd < cap // 8 - 1:
                    sch.op("dve", lambda e, o=work[0:E, lo:hi]:
                           e.match_replace(out=o, in_to_replace=m8, in_values=o, imm_value=-1.0), r=[k_tk], w=[k_tk])
            sch.op("dve", lambda e, o=thr[0:E, col:col + 1]: e.tensor_copy(out=o, in_=m8[:, 7:8]), r=[k_tk], w=[k_tk])
        for (lo, hi, cap, col) in segs:
            sch.op("dve", lambda e, o=work[0:E, lo:hi], i=affT[0:E, lo:hi], s_=thr[0:E, col:col + 1]:
                   e.tensor_scalar(out=o, in0=i, scalar1=s_, scalar2=None, op0=ALU.is_ge), r=[k_tk, k_affT], w=[k_tk])
            sch.op("dve", lambda e, o=cs[0:E, lo:hi], i=work[0:E, lo:hi]:
                   e.tensor_tensor_scan(out=o, data0=i, data1=i, initial=0.0, op0=ALU.add, op1=ALU.max),
                   r=[k_tk], w=[k_tk])
        lo0 = L if last else 0
        sch.op("dve", lambda e, o=cs[0:E, lo0:T], a=cs[0:E, lo0:T], b_=work[0:E, lo0:T]:
               e.tensor_tensor(out=o, in0=a, in1=b_, op=ALU.mult), r=[k_tk], w=[k_tk])
        sch.op("dve", lambda e, o=cs[0:E, lo0:T], i=cs[0:E, lo0:T]:
               e.tensor_scalar(out=o, in0=i, scalar1=-1.0, scalar2=None, op0=ALU.add), r=[k_tk], w=[k_tk])
        t_first = 2 if last else 0
        for t in range(t_first, NT):
            sch.op("pe", lambda e, o=bank(4)[:, t * E:(t + 1) * E], i=cs[0:E, t * 128:(t + 1) * 128]:
                   e.transpose(out=o, in_=i, identity=ident_f[0:E, 0:E]), r=[k_tk, k_cst], w=[ptrk[4]])
        sch.op("act", lambda e, o=posm_tm.rearrange("p a b -> p (a b)")[:, t_first * E:NT * E],
               i=bank(4)[:, t_first * E:NT * E]: e.copy(out=o, in_=i), r=[ptrk[4]], w=[k_tk])

        barrier()
        k_sel = Trk()
        k_selw = [Trk(), Trk()]
        k_swst = Trk()
        k_swsc = Trk()
        k_xeT = Trk()
        k_wgu = [Trk(), Trk()]
        k_hid1 = Trk()
        k_hid = [k_hid1, k_hid1]
        sgf = [reg2[:, 0:288], reg2[:, 512:800]]
        k_sgf = [Trk(), Trk()]
        nj = 256 if last else 288
        wctr2 = 0
        for ex in range(E):
            for t in range(16):
                sch.op("dve", lambda e, o=sel[:, t, :], s_=posm_tm[:, 2 + t, ex:ex + 1]:
                       e.tensor_scalar(out=o, in0=iota_f[:, 0:256], scalar1=s_, scalar2=None, op0=ALU.is_equal),
                       r=[k_tk, k_cst], w=[k_sel])
            if not last:
                for t in range(2):
                    sch.op("dve", lambda e, o=selc[:, t, :], s_=posm_tm[:, t, ex:ex + 1]:
                           e.tensor_scalar(out=o, in0=iota_f[:, 0:32], scalar1=s_, scalar2=None, op0=ALU.is_equal),
                           r=[k_tk, k_cst], w=[k_sel])
            for t in range(16):
                s_w = t % 2
                sch.op("dve", lambda e, o=selw[s_w], s_=posm_tm[:, 2 + t, ex:ex + 1], a_=aff_tm[:, 2 + t, ex:ex + 1]:
                       e.tensor_scalar(out=o, in0=iota_f[:, 0:256], scalar1=s_, scalar2=a_, op0=ALU.is_equal,
                                       op1=ALU.mult), r=[k_tk, k_aff, k_cst], w=[k_selw[s_w]])
                pb_ = 6 + (t // 2) % 2
                for jc in range(2):
                    sch.op("pe", lambda e, o=bank_bf(pb_)[:, jc * 256 + (t % 2) * 128:jc * 256 + (t % 2) * 128 + 128],
                           i=selw[s_w][:, jc * 128:(jc + 1) * 128]: e.transpose(out=o, in_=i, identity=ident_bf),
                           r=[k_selw[s_w], k_cst], w=[ptrk[pb_]])
                if t % 2 == 1:
                    t0 = t - 1
                    sch.op("dve", lambda e, o=swst[:, :, t0 * 128:(t0 + 2) * 128],
                           i=bank_bf(pb_)[:, 0:512].rearrange("p (c t) -> p c t", c=2): e.tensor_copy(out=o, in_=i),
                           r=[ptrk[pb_]], w=[k_swst])
            for jc in range(2):
                ld("sp", SWTL.rearrange("t j e c k -> j e c t k")[:, ex, jc],
                   swst[:, jc, :].rearrange("j (t k) -> j t k", k=128), r=[k_swst], w=[k_scr["SWTL"]])
            if not last:
                for t in range(2):
                    s_w = t % 2
                    sch.op("dve", lambda e, o=selw[s_w][:, 0:32], s_=posm_tm[:, t, ex:ex + 1],
                           a_=aff_tm[:, t, ex:ex + 1]:
                           e.tensor_scalar(out=o, in0=iota_f[:, 0:32], scalar1=s_, scalar2=a_, op0=ALU.is_equal,
                                           op1=ALU.mult), r=[k_tk, k_aff, k_cst], w=[k_selw[s_w]])
                    sch.op("pe", lambda e, o=bank_bf(5)[0:32, t * 128:(t + 1) * 128], i=selw[s_w][:, 0:32]:
                           e.transpose(out=o, in_=i, identity=ident_bf), r=[k_selw[s_w], k_cst], w=[ptrk[5]])
                sch.op("act", lambda e, o=swsc[0:32, 0:256], i=bank_bf(5)[0:32, 0:256]: e.copy(out=o, in_=i),
                       r=[ptrk[5]], w=[k_swsc])
                ld("sp", SWTC[ex], swsc[0:32, 0:256], r=[k_swsc], w=[k_scr["SWTC"]])
            for dch in range(KD):
                pb_ = dch % 2
                for t in range(16):
                    sch.op("pe", lambda e, o=bank(pb_)[:, 0:256], l=xn2[:, 2 + t, dch * 128:(dch + 1) * 128],
                           r_=sel[:, t, :], st=(t == 0), sp_=(t == 15): e.matmul(o, l, r_, start=st, stop=sp_),
                           r=[k_xn2[2 + t], k_sel], w=[ptrk[pb_]])
                if not last:
                    for t in range(2):
                        sch.op("pe", lambda e, o=bank(pb_)[:, 256:288], l=xn2[:, t, dch * 128:(dch + 1) * 128],
                               r_=selc[:, t, :], st=(t == 0), sp_=(t == 1): e.matmul(o, l, r_, start=st, stop=sp_),
                               r=[k_xn2[t], k_sel], w=[ptrk[pb_]])
                sch.op("act", lambda e, o=xeT[:, dch, 0:256], i=bank(pb_)[:, 0:256], sc_=modcol(2, dch, 0),
                       bi_=modcol(3, dch, 0): e.activation(out=o, in_=i, func=AF.Identity, scale=sc_, bias=bi_),
                       r=[ptrk[pb_], k_mod], w=[k_xeT])
                if not last:
                    sch.op("act", lambda e, o=xeT[:, dch, 256:288], i=bank(pb_)[:, 256:288], sc_=modcol(2, dch, 1),
                           bi_=modcol(3, dch, 1): e.activation(out=o, in_=i, func=AF.Identity, scale=sc_, bias=bi_),
                           r=[ptrk[pb_], k_mod], w=[k_xeT])
            hb = ex % 2
            for fp in range(4):
                wb2 = wctr2 % 2
                wctr2 += 1
                ld("pool", wgu[wb2][0], w_eg[li, ex].rearrange("(k p) f -> p k f", p=128)[:, :, fp * 256:(fp + 1) * 256],
                   w=[k_wgu[wb2]])
                ld("pool", wgu[wb2][1], w_eu[li, ex].rearrange("(k p) f -> p k f", p=128)[:, :, fp * 256:(fp + 1) * 256],
                   w=[k_wgu[wb2]])
                for f2 in range(2):
                    fc = fp * 2 + f2
                    s3 = fc % 2
                    pg, pu = 2 + 2 * s3, 3 + 2 * s3
                    for kk, pb_ in ((0, pg), (1, pu)):
                        for k in range(KD):
                            sch.op("pe", lambda e, o=bank(pb_)[:, 0:nj], l=wgu[wb2][kk][:, k, f2 * 128:(f2 + 1) * 128],
                                   r_=xeT[:, k, 0:nj], st=(k == 0), sp_=(k == KD - 1):
                                   e.matmul(o, l, r_, start=st, stop=sp_),
                                   r=[k_wgu[wb2], k_xeT], w=[ptrk[pb_]])
                    sch.op("act", lambda e, o=sgf[s3][:, 0:nj], i=bank(pg)[:, 0:nj]:
                           e.activation(out=o, in_=i, func=AF.Silu), r=[ptrk[pg]], w=[k_sgf[s3]])
                    sch.op("dve", lambda e, o=hid[hb][:, fc, 0:nj], a=sgf[s3][:, 0:nj], b_=bank(pu)[:, 0:nj]:
                           e.tensor_tensor(out=o, in0=a, in1=b_, op=ALU.mult),
                           r=[k_sgf[s3], ptrk[pu]], w=[k_hid[hb]])
            ld("sp", HID[ex][:, :, 0:nj], hid[hb][:, :, 0:nj], r=[k_hid[hb]], w=[k_scr["HID"]])

        new_phase()
        ye = ABF.get(E * 3 * 512, (E, 3, 512))
        k_ye = Trk()
        hidb = [ABF.get(8 * 288, (8, 288)) for _ in range(2)]
        k_hidb = [Trk(), Trk()]
        wd = [ABF.get(8 * 512, (8, 512)) for _ in range(2)]
        k_wd = [Trk(), Trk()]
        swl = [ABF.get(E * 256, (E, 2, 128)) for _ in range(2)]
        k_swl = [Trk(), Trk()]
        swc = [ABF.get(E * 128, (E, 128)) for _ in range(2)]
        k_swc = [Trk(), Trk()]
        xpf = [AFF.get(512) for _ in range(2)]
        k_xpf = [Trk(), Trk()]
        tmpf = [AFF.get(512) for _ in range(2)]
        k_tmpf = [Trk(), Trk()]
        grep2 = AFF.get(2 * D, (2, D))
        k_gr2 = Trk()
        for kind in range(2):
            ld("sp", grep2[:, kind, :], GREP[1, kind], r=[k_scr["GREP"]], w=[k_gr2])
        SWTCv = SWTC.rearrange("e j t -> j e t")
        jcs = ((0, 128), (1, 128)) if last else ((0, 128), (1, 128), (2, 32))
        yctr = 0
        xctr = 0
        for dblk in range(4):
            dsl = slice(dblk * 512, (dblk + 1) * 512)
            for ex in range(E):
                b = ex % 2
                ld("pool", hidb[b][:, :, 0:nj], HID[ex][:, :, 0:nj], r=[k_scr["HID"]], w=[k_hidb[b]])
                ld("pool", wd[b], w_ed[li, ex].rearrange("(k p) d -> p k d", p=128)[:, :, dsl], w=[k_wd[b]])
                for (jc, rows) in jcs:
                    pb_ = yctr % 4
                    yctr += 1
                    for k in range(8):
                        sch.op("pe", lambda e, o=bank(pb_)[0:rows, :], l=hidb[b][:, k, jc * 128:jc * 128 + rows],
                               r_=wd[b][:, k, :], st=(k == 0), sp_=(k == 7): e.matmul(o, l, r_, start=st, stop=sp_),
                               r=[k_hidb[b], k_wd[b]], w=[ptrk[pb_]])
                    sch.op("act", lambda e, o=ye[0:rows, ex, jc, :], i=bank(pb_)[0:rows, :]: e.copy(out=o, in_=i),
                           r=[ptrk[pb_]], w=[k_ye])
            for t in range(2 if last else 0, NT):
                b = xctr % 2
                pb_ = 4 + xctr % 4
                xctr += 1
                kind = kind_of(t)
                ld("pool", xpf[b], X[t * 128:(t + 1) * 128, dsl], r=[k_X[t]], w=[k_xpf[b]])
                if kind == 1:
                    ld("pool", swc[b][0:32], SWTCv[:, :, t * 128:(t + 1) * 128], r=[k_scr["SWTC"]], w=[k_swc[b]])
                    for ex in range(E):
                        sch.op("pe", lambda e, o=bank(pb_), l=swc[b][0:32, ex, :], r_=ye[0:32, ex, 2, :],
                               st=(ex == 0), sp_=(ex == E - 1): e.matmul(o, l, r_, start=st, stop=sp_),
                               r=[k_swc[b], k_ye], w=[ptrk[pb_]])
                else:
                    ld("pool", swl[b], SWTL[t - 2], r=[k_scr["SWTL"]], w=[k_swl[b]])
                    for ex in range(E):
                        for jc in range(2):
                            sch.op("pe", lambda e, o=bank(pb_), l=swl[b][:, ex, jc, :], r_=ye[:, ex, jc, :],
                                   st=(ex == 0 and jc == 0), sp_=(ex == E - 1 and jc == 1):
                                   e.matmul(o, l, r_, start=st, stop=sp_), r=[k_swl[b], k_ye], w=[ptrk[pb_]])
                sch.op("dve", lambda e, o=tmpf[b], a=bank(pb_), g_=grep2[:, kind, dsl]:
                       e.tensor_tensor(out=o, in0=a, in1=g_, op=ALU.mult), r=[ptrk[pb_], k_gr2], w=[k_tmpf[b]])
                sch.op("dve", lambda e, o=xpf[b], a=xpf[b], t_=tmpf[b]: e.tensor_tensor(out=o, in0=a, in1=t_, op=ALU.add),
                       r=[k_tmpf[b]], w=[k_xpf[b]])
                ld("sp", X[t * 128:(t + 1) * 128, dsl], xpf[b], r=[k_xpf[b]], w=[k_X[t]])
        if dbg.get("stopF"):
            break

    barrier()
    k_out = Trk()
    for j in range(4):
        ld("sp", out[j * 512:(j + 1) * 512, :], X[L + j * 512:L + (j + 1) * 512, :], w=[k_out])
    barrier()
    with nc.Block() as block:
        @block.tensor
        def _(e):
            sch.replay("pe", e)

        @block.scalar
        def _(e):
            sch.replay("act", e)

        @block.vector
        def _(e):
            sch.replay("dve", e)

        @block.gpsimd
        def _(e):
            sch.replay("pool", e)

        @block.sync
        def _(e):
            sch.replay("sp", e)
    es.close()
    return nc


def host_inputs(inp, b, nlw=DEPTH, ne=E):
    f = np.float32
    m = {}
    m["x"] = np.ascontiguousarray(inp["x"][b])
    m["ctx"] = np.ascontiguousarray(inp["ctx"][b])
    cT = np.stack([inp["c"][b].reshape(KD, 128).T, inp["c_ctx"].reshape(KD, 128).T], axis=2)
    m["cT"] = np.ascontiguousarray(cT.reshape(128, KD * 2)).astype(f)
    m["w_mod"] = inp["w_mod"][:nlw]
    m["b_mod"] = inp["b_mod"][:nlw]
    m["b_modT"] = np.ascontiguousarray(inp["b_mod"][:nlw].reshape(nlw, 96, 128).transpose(0, 2, 1))
    m["norm1T"] = np.ascontiguousarray(inp["norm1"][:nlw].reshape(nlw, KD, 128).transpose(0, 2, 1))
    m["norm2T"] = np.ascontiguousarray(inp["norm2"][:nlw].reshape(nlw, KD, 128).transpose(0, 2, 1))
    m["w_in"] = inp["w_in"][:nlw]
    idx = _na_uniq_idx()
    rb = inp["na_rel_bias"][:nlw].reshape(nlw, 8, 15 * 31)
    rbp = np.concatenate([rb, np.full((nlw, 8, 1), NEG, f)], axis=2)
    m["na_bias"] = np.ascontiguousarray(rbp[:, :, idx])
    for nm in ("na_q_norm", "na_k_norm", "mla_q_a_norm", "mla_kv_a_norm", "mla_q_norm", "mla_k_norm",
               "gqa_q_norm", "gqa_k_norm"):
        m[nm] = inp[nm][:nlw]
    m["mla_w_q_b"] = inp["mla_w_q_b"][:nlw]
    m["mla_w_kv_b"] = inp["mla_w_kv_b"][:nlw]
    m["w_branch_a"] = inp["w_branch_a"][:nlw]
    m["w_branch_b"] = inp["w_branch_b"][:nlw]
    m["w_branch_c"] = inp["w_branch_c"][:nlw]
    m["w_out"] = inp["w_out"][:nlw]
    m["w_router"] = inp["w_router"][:nlw]
    m["w_expert_gate"] = inp["w_expert_gate"][:nlw, :ne]
    m["w_expert_up"] = inp["w_expert_up"][:nlw, :ne]
    m["w_expert_down"] = inp["w_expert_down"][:nlw, :ne]
    c64, s64 = _rope_tables(64)
    c32, s32 = _rope_tables(32)
    m["ropeC64"], m["ropeS64"], m["ropeC32"], m["ropeS32"] = c64, s64, c32, s32
    m["ident"] = np.eye(128, dtype=f)
    m["iota"] = np.ascontiguousarray(np.broadcast_to(np.arange(256, dtype=f), (128, 256)))
    sel = np.zeros((128, 128), f)
    sel[64, 0:64] = 1.0
    m["sel64"] = sel
    return {k: np.ascontiguousarray(v, dtype=f) for k, v in m.items()}


_NC_CACHE = {}


def kernel(**inputs):
    inp = {k: np.asarray(v) for k, v in inputs.items()}
    if "nc" not in _NC_CACHE:
        _NC_CACHE["nc"] = build()
    nc = _NC_CACHE["nc"]
    in_maps = [host_inputs(inp, b) for b in range(NCORES)]
    res = run_bass_kernel_spmd(nc, in_maps, core_ids=list(range(NCORES)))
    out = np.stack([np.asarray(res.results[b]["out"]) for b in range(NCORES)], axis=0)
    return out.astype(np.float32, copy=False)
```
